# Optimizing a Trainium2 kernel written in Bass

```python
import math
import jax, jax.numpy as jnp
from jax import lax
import numpy as np

D_MODEL = 1024
BATCH = 8
SEQ = 2048
DEPTH = 4
DEC_BATCH = 128
DEC_SEQ = 1
PAST_LEN = 16384
PAGE_SIZE = 128

GM_WIDTH = D_MODEL
GM_GROUPS = 4
GM_GROUP_DIM = GM_WIDTH // GM_GROUPS
GM_CHUNK = 128
RET_HEADS = 4
RET_DK = D_MODEL // RET_HEADS
RET_DV = 2 * RET_DK
RET_QK = RET_HEADS * RET_DK
RET_V = RET_HEADS * RET_DV
RET_CHUNK = 128
ROPE_BASE = 10000.0
D_FF = 4 * D_MODEL
EPS = 1e-6
IN_WIDTH = 2 * GM_WIDTH + 2 * RET_QK + 2 * RET_V + 2 * D_MODEL
SPLITS = [GM_WIDTH, 2 * GM_WIDTH, 2 * GM_WIDTH + RET_QK, 2 * GM_WIDTH + 2 * RET_QK,
          2 * GM_WIDTH + 2 * RET_QK + RET_V, 2 * GM_WIDTH + 2 * RET_QK + 2 * RET_V]

kernel_name = "hybrid_gmlp_retention_decoder_step"


def rms_norm(x, g):
    xf = x.astype(jnp.float32)
    y = xf * lax.rsqrt(jnp.mean(xf * xf, axis=-1, keepdims=True) + EPS)
    return (y * g.astype(jnp.float32)).astype(x.dtype)


def layer_norm(x, g, b):
    xf = x.astype(jnp.float32)
    mu = jnp.mean(xf, axis=-1, keepdims=True)
    xc = xf - mu
    y = xc * lax.rsqrt(jnp.mean(xc * xc, axis=-1, keepdims=True) + EPS)
    return (y * g.astype(jnp.float32) + b.astype(jnp.float32)).astype(x.dtype)


def chunk_spatial_gating(u, v, w_s, b_s):
    B, L, _ = v.shape
    n_chunks = -(-L // GM_CHUNK)
    pad = n_chunks * GM_CHUNK - L
    vp = jnp.pad(v, ((0, 0), (0, pad), (0, 0))).reshape(B, n_chunks, GM_CHUNK, GM_GROUPS, GM_GROUP_DIM)
    causal = jnp.tril(jnp.ones((GM_CHUNK, GM_CHUNK), dtype=bool))
    w = jnp.where(causal[None], w_s, jnp.zeros_like(w_s)).astype(v.dtype)
    mixed = jnp.einsum('gij,bcjgd->bcigd', w, vp) + b_s.T.astype(v.dtype)[None, None, :, :, None]
    mixed = mixed.reshape(B, n_chunks * GM_CHUNK, GM_WIDTH)[:, :L]
    return u * mixed


def rotary(x, pos0):
    L = x.shape[1]
    inv_freq = 1.0 / (ROPE_BASE ** jnp.linspace(0.0, 1.0, RET_DK // 2, dtype=jnp.float32))
    pos = (jnp.arange(L, dtype=jnp.int32) + pos0).astype(jnp.float32)
    ang = pos[:, None] * inv_freq[None, :]
    cos = jnp.cos(ang)[None, :, None, :]
    sin = jnp.sin(ang)[None, :, None, :]
    x1, x2 = jnp.split(x, 2, axis=-1)
    return jnp.concatenate([x1 * cos - x2 * sin, x1 * sin + x2 * cos], axis=-1)


def retention(q, k, v, s0):
    B, L = q.shape[:2]
    c = math.gcd(L, RET_CHUNK)
    n = L // c
    log_g = jnp.log(1.0 - 2.0 ** (-5.0 - jnp.arange(RET_HEADS, dtype=jnp.float32)))
    idx = jnp.arange(c, dtype=jnp.float32)
    diff = idx[:, None] - idx[None, :]
    intra = jnp.exp(jnp.where(diff[None] >= 0, diff[None] * log_g[:, None, None], -jnp.inf))
    q_decay = jnp.exp((idx[:, None] + 1.0) * log_g[None, :])
    k_decay = jnp.exp((c - 1.0 - idx[:, None]) * log_g[None, :])
    chunk_decay = jnp.exp(c * log_g)

    def to_chunks(t):
        return t.reshape(B, n, c, *t.shape[2:]).swapaxes(0, 1)

    def step(S, inp):
        qc, kc, vc = inp
        scores = jnp.einsum('bihd,bjhd->bhij', qc, kc) * intra[None]
        inner = jnp.einsum('bhij,bjhe->bihe', scores, vc)
        cross = jnp.einsum('bihd,bhde->bihe', qc, S) * q_decay[None, :, :, None]
        S_new = S * chunk_decay[None, :, None, None] + jnp.einsum(
            'bjhd,bjhe->bhde', kc * k_decay[None, :, :, None], vc)
        return S_new, inner + cross

    s_fin, o = lax.scan(step, s0, (to_chunks(q), to_chunks(k), to_chunks(v)))
    o = o.swapaxes(0, 1).reshape(B, L, RET_HEADS, RET_DV)
    return o, s_fin


def hybrid_mixer(xn, pos0, s0, w_in, b_gate, gm_ln_g, gm_ln_b, gm_w_s, gm_b_s,
                 w_proj_gm, w_proj_ret, w_out):
    B, L, _ = xn.shape
    proj = xn @ w_in
    u, v, q, k, vr, gr, gate = jnp.split(proj, SPLITS, axis=-1)
    u = jax.nn.gelu(u)
    v = layer_norm(jax.nn.gelu(v), gm_ln_g, gm_ln_b)
    a = chunk_spatial_gating(u, v, gm_w_s, gm_b_s)
    qf = rotary(q.reshape(B, L, RET_HEADS, RET_DK).astype(jnp.float32), pos0)
    kf = rotary(k.reshape(B, L, RET_HEADS, RET_DK).astype(jnp.float32), pos0) * (RET_DK ** -0.5)
    vf = vr.reshape(B, L, RET_HEADS, RET_DV).astype(jnp.float32)
    o, s_new = retention(qf, kf, vf, s0.astype(jnp.float32))
    o = o * lax.rsqrt(jnp.mean(o * o, axis=-1, keepdims=True) + EPS)
    r = jax.nn.silu(gr) * o.reshape(B, L, RET_V).astype(xn.dtype)
    gate_a, gate_b = jnp.split(jax.nn.sigmoid(gate + b_gate), 2, axis=-1)
    h = gate_a * (a @ w_proj_gm) + gate_b * (r @ w_proj_ret)
    return h @ w_out, s_new.astype(s0.dtype), v


def run_trunk(x, pos0, state_ret, norm_mix_g, w_in, b_gate, gm_ln_g, gm_ln_b, gm_w_s, gm_b_s,
              w_proj_gm, w_proj_ret, w_out, norm_ffn_g, w_up, w_down, norm_final_g):
    new_states = []
    v_rows = []
    for l in range(DEPTH):
        xn = rms_norm(x, norm_mix_g[l])
        mix, s_new, v = hybrid_mixer(xn, pos0, state_ret[l], w_in[l], b_gate[l], gm_ln_g[l], gm_ln_b[l],
                                     gm_w_s[l], gm_b_s[l], w_proj_gm[l], w_proj_ret[l], w_out[l])
        x = x + mix
        hn = rms_norm(x, norm_ffn_g[l])
        x = x + jnp.square(jax.nn.relu(hn @ w_up[l])) @ w_down[l]
        new_states.append(s_new)
        v_rows.append(v)
    return rms_norm(x, norm_final_g), jnp.stack(new_states), jnp.stack(v_rows)


def setup_inputs(seed: int = 0) -> dict:
    key = jax.random.key(seed)
    ks = jax.random.split(key, 20)
    f32 = jnp.float32

    def nrm(k, shape, scale):
        return jax.random.normal(k, shape, f32) * scale

    return {
        "x_prompt": nrm(ks[0], (BATCH, SEQ, D_MODEL), 1.0),
        "x_sample": nrm(ks[1], (DEC_BATCH, DEC_SEQ, D_MODEL), 1.0),
        "state_ret": nrm(ks[2], (DEPTH, DEC_BATCH, RET_HEADS, RET_DK, RET_DV), 0.5),
        "norm_mix_g": 1.0 + nrm(ks[3], (DEPTH, D_MODEL), 0.01),
        "w_in": nrm(ks[4], (DEPTH, D_MODEL, IN_WIDTH), D_MODEL ** -0.5),
        "b_gate": nrm(ks[5], (DEPTH, 2 * D_MODEL), 0.01),
        "gm_ln_g": 1.0 + nrm(ks[6], (DEPTH, GM_WIDTH), 0.01),
        "gm_ln_b": nrm(ks[7], (DEPTH, GM_WIDTH), 0.01),
        "gm_w_s": nrm(ks[8], (DEPTH, GM_GROUPS, GM_CHUNK, GM_CHUNK), GM_CHUNK ** -0.5),
        "gm_b_s": 1.0 + nrm(ks[9], (DEPTH, GM_GROUPS, GM_CHUNK), 0.01),
        "w_proj_gm": nrm(ks[10], (DEPTH, GM_WIDTH, D_MODEL), GM_WIDTH ** -0.5),
        "w_proj_ret": nrm(ks[11], (DEPTH, RET_V, D_MODEL), RET_V ** -0.5),
        "w_out": nrm(ks[12], (DEPTH, D_MODEL, D_MODEL), D_MODEL ** -0.5),
        "norm_ffn_g": 1.0 + nrm(ks[13], (DEPTH, D_MODEL), 0.01),
        "w_up": nrm(ks[14], (DEPTH, D_MODEL, D_FF), D_MODEL ** -0.5),
        "w_down": nrm(ks[15], (DEPTH, D_FF, D_MODEL), D_FF ** -0.5),
        "norm_final_g": 1.0 + nrm(ks[16], (D_MODEL,), 0.01),
    }


def reference(x_prompt, x_sample, state_ret, norm_mix_g, w_in, b_gate, gm_ln_g, gm_ln_b, gm_w_s, gm_b_s,
              w_proj_gm, w_proj_ret, w_out, norm_ffn_g, w_up, w_down, norm_final_g):
    zero_state = jnp.zeros((DEPTH, x_prompt.shape[0], RET_HEADS, RET_DK, RET_DV), state_ret.dtype)
    y_prompt, new_ret_prompt, _ = run_trunk(
        x_prompt, 0, zero_state, norm_mix_g, w_in, b_gate, gm_ln_g, gm_ln_b, gm_w_s, gm_b_s,
        w_proj_gm, w_proj_ret, w_out, norm_ffn_g, w_up, w_down, norm_final_g)
    y_sample, new_ret_sample, gm_v_sample = run_trunk(
        x_sample, PAST_LEN, state_ret, norm_mix_g, w_in, b_gate, gm_ln_g, gm_ln_b, gm_w_s, gm_b_s,
        w_proj_gm, w_proj_ret, w_out, norm_ffn_g, w_up, w_down, norm_final_g)
    return (y_prompt, y_sample, new_ret_prompt, new_ret_sample, gm_v_sample)
```

```python
import os
import numpy as np
from contextlib import ExitStack
import concourse.bass as bass
import concourse.mybir as mybir
from concourse.bass_utils import run_bass_kernel_spmd

F32 = mybir.dt.float32
BF16 = mybir.dt.bfloat16
AF = mybir.ActivationFunctionType
ALU = mybir.AluOpType

D = 1024
H = 4
DK = 256
DV = 512
DFF = 4096
INW = 10240
EPS = 1e-6
NSLOT = 6
PAST_LEN = 16384


class Cfg:
    def __init__(self, depth=4, nch=8, ntiles=2, ns=16):
        self.depth, self.nch, self.ntiles, self.ns = depth, nch, ntiles, ns
        self.seq = nch * ntiles * 128
        self.ntok = nch * 128 + ns


class Prog:
    COMPUTE = ("pe", "act", "dve", "pool")

    def __init__(self):
        self.ops = []
        self.cells = {}

    def op(self, eng, fn, reads=(), writes=(), dma=None):
        oid = len(self.ops)
        deps = set()
        for k in reads:
            c = self.cells.get(k)
            if c is not None and c[0] is not None:
                deps.add(c[0])
        for k in writes:
            c = self.cells.get(k)
            if c is not None:
                if c[0] is not None:
                    deps.add(c[0])
                deps.update(c[1].values())
                deps.update(c[2])
        for k in reads:
            c = self.cells.get(k)
            if c is None:
                c = self.cells[k] = [None, {}, []]
            if dma is None:
                c[1][eng] = oid
            else:
                c[2].append(oid)
        for k in writes:
            self.cells[k] = [oid, {}, []]
        deps.discard(oid)
        self.ops.append(dict(eng=eng, fn=fn, deps=deps, dma=dma))
        return oid

    def emit(self, nc, block_engines, sem_alloc, dma_classes):
        ops = self.ops
        needed = set()
        for o in ops:
            needed.update(o["deps"])
        ROLL = 30000
        cnt = {e: 0 for e in self.COMPUTE}
        epoch = {e: 0 for e in self.COMPUTE}
        esems = {e: [sem_alloc("c_%s_0" % e)] for e in self.COMPUTE}
        dcnt = {}
        for i, o in enumerate(ops):
            o["pre"] = None
            if o["dma"] is not None:
                cls = o["dma"]
                sems = dma_classes[cls]
                n = dcnt.get(cls, 0)
                dcnt[cls] = n + 1
                s = sems[n % len(sems)]
                k = n // len(sems)
                o["pre"] = (s, 16 * k) if k > 0 else None
                o["ev"] = (s, 16 * (k + 1))
                o["inc"] = (s, 16)
            elif o["eng"] in self.COMPUTE and i in needed:
                e = o["eng"]
                if cnt[e] >= ROLL:
                    epoch[e] += 1
                    cnt[e] = 0
                    esems[e].append(sem_alloc("c_%s_%d" % (e, epoch[e])))
                cnt[e] += 1
                s = esems[e][epoch[e]]
                o["ev"] = (s, cnt[e])
                o["inc"] = (s, 1)
            else:
                o["ev"] = None
                o["inc"] = None
        per = {}
        for i, o in enumerate(ops):
            per.setdefault(o["eng"], []).append(i)
        self.nwaits = 0

        def run(engname, e):
            seen = {}
            for i in per.get(engname, []):
                o = ops[i]
                want = {}
                for d in o["deps"]:
                    od = ops[d]
                    if od["dma"] is None and od["eng"] == "pe" and engname == "pe":
                        continue
                    ev = od["ev"]
                    if ev is None:
                        continue
                    s, v = ev
                    if want.get(id(s), (None, -1))[1] < v:
                        want[id(s)] = (s, v)
                if o["pre"] is not None:
                    s, v = o["pre"]
                    if want.get(id(s), (None, -1))[1] < v:
                        want[id(s)] = (s, v)
                for sid, (s, v) in want.items():
                    if seen.get(sid, -1) >= v:
                        continue
                    seen[sid] = v
                    e.wait_ge(s, v)
                    self.nwaits += 1
                ins = o["fn"](e)
                if o["inc"] is not None:
                    assert ins is not None
                    ins.then_inc(o["inc"][0], o["inc"][1])

        for engname, deco in block_engines.items():
            deco((lambda en: (lambda e: run(en, e)))(engname))


def _host_tables(cfg):
    nch, ntiles = cfg.nch, cfg.ntiles
    inv_freq = (1.0 / (np.float32(10000.0) ** np.linspace(0.0, 1.0, DK // 2, dtype=np.float32))).astype(np.float32)
    pos = np.arange(cfg.seq, dtype=np.int32).astype(np.float32)
    ang = (pos[:, None] * inv_freq[None, :]).astype(np.float32)
    cos = np.cos(ang).astype(np.float32).reshape(ntiles, nch, 128, 128).transpose(0, 2, 1, 3)
    sin = np.sin(ang).astype(np.float32).reshape(ntiles, nch, 128, 128).transpose(0, 2, 1, 3)
    angs = (np.float32(PAST_LEN) * inv_freq).astype(np.float32)
    cs_s = np.stack([np.cos(angs), np.sin(angs)]).astype(np.float32)
    cs_s = np.broadcast_to(cs_s[None], (16, 2, 128)).copy()
    g = 1.0 - 2.0 ** (-5.0 - np.arange(H, dtype=np.float64))
    p = (np.arange(nch)[None, :, None] * 128 + np.arange(128)[:, None, None] + 1).astype(np.float64)
    dq = (g[None, None, :] ** p).astype(np.float32)
    dk = ((g[None, None, :] ** (-p)) * (DK ** -0.5)).astype(np.float32)
    dqk = np.stack([dq, dk], axis=2).copy()
    j = np.arange(128)[:, None]
    i = np.arange(128)[None, :]
    mask = (i >= j).astype(np.float32)
    ident = np.eye(128, dtype=np.float32)
    gam = g
    gend = g ** (128 * nch)
    cs = np.ascontiguousarray(np.stack([cos, sin], axis=3))
    return dict(cs=cs, cs_s=cs_s, dqk=dqk, mask=mask,
                ident=ident), gam, gend


def build_nc(cfg):
    depth, nch, ntiles, ns = cfg.depth, cfg.nch, cfg.ntiles, cfg.ns
    NT = cfg.ntok
    _, GAM, GEND = _host_tables(cfg)
    nc = bass.Bass("TRN2", target_bir_lowering=False)

    def din(name, shape):
        return nc.dram_tensor(name, list(shape), F32, kind="ExternalInput").ap()

    def dout(name, shape):
        return nc.dram_tensor(name, list(shape), F32, kind="ExternalOutput").ap()

    xp = din("xp", [cfg.seq, D])
    xs = din("xs", [ns, D])
    st = din("st", [depth, ns, H, DK, DV])
    w_in = din("w_in", [depth, D, INW])
    w_pgm = din("w_pgm", [depth, D, D])
    w_pret = din("w_pret", [depth, 2 * D, D])
    w_out = din("w_out", [depth, D, D])
    w_up = din("w_up", [depth, D, DFF])
    w_down = din("w_down", [depth, DFF, D])
    gcols_d = din("gcols", [128, depth, 3, 8])
    gfin_d = din("gfin", [D])
    bg_d = din("bgcols", [128, depth, 16])
    lng_d = din("lng", [depth, D])
    lnb_d = din("lnb", [depth, D])
    wt_d = din("wt", [depth, 128, 4, 128])
    w00_d = din("w00", [depth, 4])
    bs_d = din("bs", [depth, 4 * 128])
    cs_d = din("cs", [ntiles, 128, nch, 2, 128])
    css_d = din("cs_s", [16, 2, 128])
    dqk_d = din("dqk", [128, nch, 2, H])
    mask_d = din("mask", [128, 128])
    ident_d = din("ident", [128, 128])

    yp = dout("yp", [cfg.seq, D])
    ys = dout("ys", [ns, D])
    nrp = dout("nrp", [depth, H, DK, DV])
    nrs = dout("nrs", [depth, ns, H, DK, DV])
    gmv = dout("gmv", [depth, ns, D])

    P = Prog()
    es = ExitStack()

    def sb(name, shape, dt):
        return es.enter_context(nc.sbuf_tensor(name, list(shape), dt))

    def ps(name, shape, dt):
        return es.enter_context(nc.psum_tensor(name, list(shape), dt))

    X = sb("X", [128, nch + 1, D], F32)
    XNT = sb("XNT", [128, 8, NT], BF16)
    AT = sb("AT", [128, 8, NT], BF16)
    BIG = sb("BIG", [128, 16, NT], BF16)
    RING = sb("RING", [128, NSLOT, 8, 512], BF16)
    CSB = sb("CSB", [128, 2, 2, 128], F32)
    CSS = sb("CSS", [16, 2, 128], F32)
    DQK = sb("DQK", [128, nch, 2, H], F32)
    MASK = sb("MASK", [128, 128], F32)
    IDF = sb("IDF", [128, 128], F32)
    IDB = sb("IDB", [128, 128], BF16)
    ONESB = sb("ONESB", [1, 128], BF16)
    MHALF = sb("MHALF", [128, 16], F32)
    GCOLS = sb("GCOLS", [128, depth, 3, 8], F32)
    BGC = sb("BGC", [128, depth, 16], F32)
    WC = sb("WC", [128, 2, 2, 512], BF16)
    SBUF_S = sb("SBUF_S", [128, 2, 2, 512], F32)
    WF = sb("WF", [128, 2, 1024], F32)
    WB = sb("WB", [128, 2, 1024], BF16)
    QKP = sb("QKP", [128, 2, 512], BF16)
    VB = sb("VB", [128, 2, 512], BF16)
    SG = sb("SG", [128, 3, 512], BF16)
    VBS = sb("VBS", [16, 512], BF16)
    OSA = sb("OSA", [16, 512], F32)
    QKT = sb("QKT", [128, 2, 4, 128], BF16)
    SCT = sb("SCT", [128, 2, 128], BF16)
    RR = sb("RR", [128, 2, 512], BF16)
    RTAB = sb("RTAB", [128, 8, 128], F32)
    RTABS = sb("RTABS", [128, 8, 16], F32)
    WTM = sb("WTM", [128, 4, 128], BF16)
    DG = sb("DG", [16, 4, 16], BF16)
    W00 = sb("W00", [16, 4], F32)
    BSH = sb("BSH", [1, 4, 128], BF16)
    BSL = sb("BSL", [1, 4, 128], BF16)
    BS0H = sb("BS0H", [1, 4, 16], BF16)
    BS0L = sb("BS0L", [1, 4, 16], BF16)
    SSQ = sb("SSQ", [128, 16], F32)
    RS = sb("RS", [128, 16], F32)
    ST1 = sb("ST1", [128, 16], F32)
    QM = sb("QM", [128, 16, 2, 16], F32)
    QS = sb("QS", [16, 256], F32)
    KS = sb("KS", [16, 256], BF16)
    KM = sb("KM", [16, 2, 256], BF16)

    SBUF_FREE = nc.sbuf_bytes_remaining
    PB_ = [ps("P%d" % i, [128, 512], F32) for i in range(4)]
    P45 = ps("P45", [128, 2, 512], F32)
    PT = [ps("PT0", [128, 8, 128], BF16)]
    PX = ps("PX", [128, 512], F32)

    def bank(i):
        return PB_[i] if i < 4 else P45[:, i - 4, :]

    def bk(i):
        return ("ps", i)

    sems = []

    def sem_alloc(name):
        s = es.enter_context(nc.semaphore(name))
        sems.append(s)
        return s

    dma_classes = {
        "w": [sem_alloc("dw%d" % i) for i in range(NSLOT)],
        "io": [sem_alloc("dio%d" % i) for i in range(4)],
        "par": [sem_alloc("dpar%d" % i) for i in range(6)],
        "ppar": [sem_alloc("dppar%d" % i) for i in range(2)],
        "si": [sem_alloc("dsi%d" % i) for i in range(2)],
        "so": [sem_alloc("dso%d" % i) for i in range(2)],
    }

    def chunk_list(t):
        cl = list(range(nch))
        if t == 0:
            cl.append(nch)
        return cl

    def ntok(c):
        return 128 if c < nch else ns

    def tok0(c):
        return c * 128

    def tokgroups(t):
        tgs = []
        c = 0
        while c < nch:
            n = min(4, nch - c)
            tgs.append((c * 128, n * 128, list(range(c, c + n))))
            c += n
        if t == 0:
            tgs.append((nch * 128, ns, [nch]))
        return tgs

    def dma(eng, cls, out, in_, reads, writes):
        P.op(eng, lambda e: e.dma_start(out=out, in_=in_), reads=reads, writes=writes, dma=cls)

    wstate = {"n": 0}

    def wload(parts):
        slot = wstate["n"] % NSLOT
        wstate["n"] += 1
        for (src, coff, ncols) in parts:
            dma("pool", "w", RING[:, slot, :, coff:coff + ncols], src.rearrange("(k p) n -> p k n", p=128),
                reads=[], writes=[("ring", slot)])
        return slot

    def win_unit(l, c0, n=512):
        return [(w_in[l, :, c0:c0 + n], 0, n)]

    def mm_group(out_ap, pairs, reads, writes, first=True, last=True):
        def fn(e):
            ins = None
            n = len(pairs)
            for i, (a, b) in enumerate(pairs):
                ins = e.matmul(out_ap, lhsT=a, rhs=b, start=(first and i == 0), stop=(last and i == n - 1))
            return ins
        P.op("pe", fn, reads=reads, writes=writes)

    def transposes(out_t, in_list, ident, reads, writes):
        def fn(e):
            ins = None
            for (o, i_) in in_list:
                ins = e.transpose(o, i_, ident)
            return ins
        P.op("pe", fn, reads=reads, writes=writes)

    def act(out, in_, func, reads, writes, **kw):
        P.op("act", lambda e: e.activation(out=out, in_=in_, func=func, **kw), reads=reads, writes=writes)

    def dve(fname, reads, writes, **kw):
        P.op("dve", lambda e: getattr(e, fname)(**kw), reads=reads, writes=writes)

    def pool_pow(ap, cells):
        n = ap.shape[-1]
        P.op("pool", lambda e: e.tensor_tensor(out=ap, in0=ap, in1=MHALF[0:ap.shape[0], 0:n], op=ALU.pow),
             reads=cells + [("mhalf",)], writes=cells)

    cnt = {"wf": 0, "wb": 0, "qkp": 0, "vb": 0, "qkt": 0, "sct": 0, "rr": 0, "pt": 0, "sg": 0, "cs": 0, "wc": 0, "ss": 0, "km": 0}

    def rot(name, n=2):
        v = cnt[name] % n
        cnt[name] += 1
        return v

    dma("sp", "par", CSS[:], css_d, [], [("css",)])
    dma("sp", "par", DQK[:], dqk_d, [], [("dqk",)])
    dma("sp", "par", MASK[:], mask_d, [], [("mask",)])
    dma("sp", "par", IDF[:], ident_d, [], [("idf",)])
    dma("sp", "par", GCOLS[:], gcols_d, [], [("gcols",)])
    dma("sp", "par", BGC[:], bg_d, [], [("bgc",)])
    dve("tensor_copy", [("idf",)], [("idb",)], out=IDB[:], in_=IDF[:])
    dve("memset", [], [("onesb",)], ap=ONESB[:], constant=1.0)
    dve("memset", [], [("mhalf",)], ap=MHALF[:], constant=-0.5)
    dve("memset", [], [("ssq",)], ap=SSQ[:], constant=1.0)
    dve("memset", [], [("st1",)], ap=ST1[:], constant=1.0)
    dve("memset", [], [("qm",)], ap=QM[:], constant=0.0)

    def phase_norm(t, l, gi):
        cl = chunk_list(t)
        for c in cl:
            n = ntok(c)
            jb = rot("wb")
            act(WB[0:n, jb, :], X[0:n, c, :], AF.Square, [("x", c), ("ssq",)], [("wb", jb), ("ssq", c)],
                accum_out=SSQ[0:n, c:c + 1])
        ncl = len(cl)
        dve("tensor_scalar", [("ssq", c) for c in cl] + [("ssq",)], [("rs",)], out=RS[:, 0:ncl], in0=SSQ[:, 0:ncl],
            scalar1=1.0 / D, scalar2=EPS, op0=ALU.mult, op1=ALU.add)
        pool_pow(RS[:, 0:ncl], [("rs",)])
        xbs = {}

        def do_xs(c):
            n = ntok(c)
            xb = rot("wb")
            xbs[c] = xb
            dve("tensor_scalar", [("x", c), ("rs",)], [("wb", xb)], out=WB[0:n, xb, :], in0=X[0:n, c, :],
                scalar1=RS[0:n, c:c + 1], scalar2=None, op0=ALU.mult)
        do_xs(cl[0])
        for i, c in enumerate(cl):
            n = ntok(c)
            if i + 1 < len(cl):
                do_xs(cl[i + 1])
            xb = xbs[c]
            transposes(None, [(PT[0][:, k, 0:n], WB[0:n, xb, k * 128:(k + 1) * 128]) for k in range(8)],
                       IDB[0:n, 0:n], [("wb", xb), ("idb",)], [("pt", 0)])
            dve("tensor_tensor", [("pt", 0), ("gcols",)], [("xnT", c)],
                out=XNT[:, :, tok0(c):tok0(c) + n], in0=PT[0][:, :, 0:n],
                in1=GCOLS[:, l, gi, :].unsqueeze(2).to_broadcast([128, 8, n]), op=ALU.mult)

    def load_layer_params(t, l):
        dma("pool", "ppar", WB[:, 1, :], lnb_d[l].partition_broadcast(128), [], [("wb", 1)])
        dma("sp", "par", WF[:, 0, 0:512].rearrange("p (g i) -> p g i", g=4), wt_d[l], [], [("wf", 0, 0)])
        dve("tensor_tensor", [("wf", 0, 0), ("mask",)], [("wtm",)], out=WTM[:],
            in0=WF[:, 0, 0:512].rearrange("p (g i) -> p g i", g=4),
            in1=MASK[:].unsqueeze(1).to_broadcast([128, 4, 128]), op=ALU.mult)
        dma("sp", "par", WF[0:1, 1, 0:512], bs_d[l].unsqueeze(0), [], [("wf", 1, 0)])
        dve("tensor_copy", [("wf", 1, 0)], [("bsh",)], out=BSH[:].rearrange("p g i -> p (g i)"), in_=WF[0:1, 1, 0:512])
        dve("tensor_tensor", [("wf", 1, 0), ("bsh",)], [("wf", 1, 1)], out=WF[0:1, 1, 512:1024], in0=WF[0:1, 1, 0:512],
            in1=BSH[:].rearrange("p g i -> p (g i)"), op=ALU.subtract)
        dve("tensor_copy", [("wf", 1, 1)], [("bsl",)], out=BSL[:].rearrange("p g i -> p (g i)"), in_=WF[0:1, 1, 512:1024])
        for kk in range(8):
            g = kk // 2
            mm_group(bank(kk % 2)[:, (kk // 2) * 128:(kk // 2) * 128 + 128],
                     [(WB[:, 1, kk * 128:(kk + 1) * 128], WTM[:, g, :]),
                      (ONESB[0:1, :], BSH[0:1, g, :]), (ONESB[0:1, :], BSL[0:1, g, :])],
                     [("wb", 1), ("wtm",), ("onesb",), ("bsh",), ("bsl",)], [bk(kk % 2)])
        for par in range(2):
            dve("tensor_copy", [bk(par)], [("rtab",)],
                out=RTAB[:].rearrange("p (a b) i -> p a b i", b=2)[:, :, par, :],
                in_=bank(par)[:].rearrange("p (a i) -> p a i", a=4))
        if t == 0:
            dma("sp", "par", W00[:], w00_d[l].partition_broadcast(16), [], [("w00",)])
            for g in range(4):
                dve("tensor_scalar", [("w00",), ("idf",)], [("dg",)], out=DG[:, g, :], in0=IDF[0:16, 0:16],
                    scalar1=W00[:, g:g + 1], scalar2=None, op0=ALU.mult)
            dve("tensor_copy", [("bsh",)], [("bs0h",)], out=BS0H[:], in_=BSH[:, :, 0:1].to_broadcast([1, 4, 16]))
            dve("tensor_copy", [("bsl",)], [("bs0l",)], out=BS0L[:], in_=BSL[:, :, 0:1].to_broadcast([1, 4, 16]))
            for kk in range(8):
                g = kk // 2
                mm_group(bank(2)[:, kk * 16:(kk + 1) * 16],
                         [(WB[0:16, 1, kk * 128:(kk + 1) * 128], DG[:, g, :]),
                          (ONESB[0:1, :], BS0H[0:1, g, :]), (ONESB[0:1, :], BS0L[0:1, g, :])],
                         [("wb", 1), ("dg",), ("onesb",), ("bs0h",), ("bs0l",)], [bk(2)])
            dve("tensor_copy", [bk(2)], [("rtabs",)], out=RTABS[:],
                in_=bank(2)[:, 0:128].rearrange("p (a i) -> p a i", a=8))

    def phase_G(t, l, slots_u, get_slots_v, after_u=None):
        tgs = tokgroups(t)
        pb = 0
        for ui in range(2):
            for (t0, n, cs) in tgs:
                for cc in range(4):
                    b = pb % 2
                    pb += 1
                    kk = ui * 4 + cc
                    mm_group(bank(b)[:, 0:n],
                             [(RING[:, slots_u[ui], k, cc * 128:(cc + 1) * 128], XNT[:, k, t0:t0 + n]) for k in range(8)],
                             [("ring", slots_u[ui])] + [("xnT", c) for c in cs], [bk(b)])
                    act(AT[:, kk, t0:t0 + n], bank(b)[:, 0:n], AF.Gelu_apprx_tanh, [bk(b)], [("aT", kk, c) for c in cs])
        slots_v = get_slots_v()
        if after_u is not None:
            after_u()
        def part1(c):
            n = ntok(c)
            vg = c % 2
            sb_ = (c % 2) * 8
            for hf in range(2):
                mm_group(bank(2 + hf)[0:n, :],
                         [(XNT[:, k, tok0(c):tok0(c) + n], RING[:, slots_v[hf], k, :]) for k in range(8)],
                         [("ring", slots_v[hf]), ("xnT", c)], [bk(2 + hf)])
                act(WF[0:n, vg, hf * 512:(hf + 1) * 512], bank(2 + hf)[0:n, :], AF.Gelu_apprx_tanh, [bk(2 + hf), ("st1",)],
                    [("wf", vg, hf), ("st1", sb_ + hf)], accum_out=ST1[0:n, sb_ + hf:sb_ + hf + 1])
            dve("tensor_scalar", [("st1", sb_), ("st1", sb_ + 1), ("st1",)], [("st1", sb_ + 2)], out=ST1[0:n, sb_ + 2:sb_ + 3],
                in0=ST1[0:n, sb_:sb_ + 1], scalar1=ST1[0:n, sb_ + 1:sb_ + 2], scalar2=-1.0 / D, op0=ALU.add, op1=ALU.mult)
            act(WB[0:n, 0, :], WF[0:n, vg, :], AF.Square, [("wf", vg, 0), ("wf", vg, 1), ("st1", sb_ + 2)],
                [("wb", 0), ("st1", sb_ + 3)], bias=ST1[0:n, sb_ + 2:sb_ + 3], accum_out=ST1[0:n, sb_ + 3:sb_ + 4])
            dve("tensor_scalar", [("st1", sb_ + 3)], [("st1", sb_ + 4)], out=ST1[0:n, sb_ + 4:sb_ + 5], in0=ST1[0:n, sb_ + 3:sb_ + 4],
                scalar1=1.0 / D, scalar2=EPS, op0=ALU.mult, op1=ALU.add)
            pool_pow(ST1[0:n, sb_ + 4:sb_ + 5], [("st1", sb_ + 4)])

        def part2(c):
            n = ntok(c)
            vg = c % 2
            sb_ = (c % 2) * 8
            vh = 1
            stc = [("st1", sb_ + 2), ("st1", sb_ + 4)]
            dve("tensor_scalar", [("wf", vg, 0), ("wf", vg, 1)] + stc, [("wb", vh)],
                out=WB[0:n, vh, :], in0=WF[0:n, vg, :], scalar1=ST1[0:n, sb_ + 2:sb_ + 3], scalar2=ST1[0:n, sb_ + 4:sb_ + 5],
                op0=ALU.add, op1=ALU.mult)
            if c == nch:
                dve("tensor_scalar", [("wf", vg, 0), ("wf", vg, 1)] + stc, [("wf", vg, 0), ("wf", vg, 1)],
                    out=WF[0:n, vg, :], in0=WF[0:n, vg, :], scalar1=ST1[0:n, sb_ + 2:sb_ + 3], scalar2=ST1[0:n, sb_ + 4:sb_ + 5],
                    op0=ALU.add, op1=ALU.mult)
                sc = 1 - vg
                dma("sp", "par", WF[0:n, sc, :], lng_d[l].partition_broadcast(n), [], [("wf", sc, 0), ("wf", sc, 1)])
                dve("tensor_tensor", [("wf", vg, 0), ("wf", vg, 1), ("wf", sc, 0), ("wf", sc, 1)],
                    [("wf", vg, 0), ("wf", vg, 1)], out=WF[0:n, vg, :], in0=WF[0:n, vg, :], in1=WF[0:n, sc, :], op=ALU.mult)
                dma("sp", "par", WF[0:n, sc, :], lnb_d[l].partition_broadcast(n), [], [("wf", sc, 0), ("wf", sc, 1)])
                dve("tensor_tensor", [("wf", vg, 0), ("wf", vg, 1), ("wf", sc, 0), ("wf", sc, 1)],
                    [("wf", vg, 0), ("wf", vg, 1)], out=WF[0:n, vg, :], in0=WF[0:n, vg, :], in1=WF[0:n, sc, :], op=ALU.add)
                dma("sp", "io", gmv[l], WF[0:n, vg, :], [("wf", vg, 0), ("wf", vg, 1)], [("gmv", l)])
            for kk in range(8):
                g = kk // 2
                rhs = WTM[:, g, :] if c < nch else DG[:, g, :]
                rcell = ("wtm",) if c < nch else ("dg",)
                mm_group(bank(4 + kk // 4)[:, (kk % 4) * 128:(kk % 4) * 128 + n],
                         [(WB[0:n, vh, kk * 128:(kk + 1) * 128], rhs)], [("wb", vh), rcell], [bk(4 + kk // 4)])
            tm = vg
            tmv = WF[:, tm, :].rearrange("p (a i) -> p a i", a=8)[:, :, 0:n]
            dve("tensor_tensor", [bk(4), bk(5), ("gcols",)], [("wf", tm, 0), ("wf", tm, 1)], out=tmv,
                in0=P45[:].rearrange("p b (a i) -> p (b a) i", a=4)[:, :, 0:n],
                in1=GCOLS[:, l, 2, :].unsqueeze(2).to_broadcast([128, 8, n]), op=ALU.mult)
            rt = RTAB[:] if c < nch else RTABS[:]
            P.op("pool", lambda e, tmv=tmv, rt=rt: e.tensor_tensor(out=tmv, in0=tmv, in1=rt, op=ALU.add),
                 reads=[("wf", tm, 0), ("wf", tm, 1), ("rtab",), ("rtabs",)], writes=[("wf", tm, 0), ("wf", tm, 1)])
            P.op("pool", lambda e, tmv=tmv, c=c, n=n: e.tensor_tensor(out=AT[:, :, tok0(c):tok0(c) + n], in0=tmv,
                                                                   in1=AT[:, :, tok0(c):tok0(c) + n], op=ALU.mult),
                 reads=[("wf", tm, 0), ("wf", tm, 1)] + [("aT", kk, c) for kk in range(8)],
                 writes=[("aT", kk, c) for kk in range(8)])

        cl = chunk_list(t)
        part1(cl[0])
        for i in range(1, len(cl)):
            part1(cl[i])
            part2(cl[i - 1])
        part2(cl[-1])

    def phase_P(t, l, src, src_cells, nk, gate_off, dst, dst_cells, groups):
        tgs = tokgroups(t)
        pb = 0
        for ch in range(2):
            wp_slots, wg_slot = groups[ch]()
            for (t0, n, cs) in tgs:
                for cc in range(4):
                    b = pb % 2
                    pb += 1
                    col = ch * 4 + cc
                    pairs = []
                    for k in range(nk):
                        pairs.append((RING[:, wp_slots[k // 8], k % 8, cc * 128:(cc + 1) * 128], src[:, k, t0:t0 + n]))
                    mm_group(bank(b)[:, 0:n], pairs,
                             [("ring", s) for s in wp_slots] + [src_cells(k, c) for k in range(nk) for c in cs], [bk(b)])
                    mm_group(bank(2 + b)[:, 0:n],
                             [(RING[:, wg_slot, k, cc * 128:(cc + 1) * 128], XNT[:, k, t0:t0 + n]) for k in range(8)],
                             [("ring", wg_slot)] + [("xnT", c) for c in cs], [bk(2 + b)])
                    sg = rot("wf")
                    act(WF[:, sg, 0:n], bank(2 + b)[:, 0:n], AF.Sigmoid, [bk(2 + b), ("bgc",)], [("wf", sg, 0)],
                        bias=BGC[:, l, gate_off + col:gate_off + col + 1])
                    dve("tensor_tensor", [("wf", sg, 0), bk(b)], [dst_cells(col, c) for c in cs],
                        out=dst[:, col, t0:t0 + n], in0=bank(b)[:, 0:n], in1=WF[:, sg, 0:n], op=ALU.mult)
        so = groups[2]()
        for c in chunk_list(t):
            n = ntok(c)
            for oh in range(2):
                b = 4 + oh
                mm_group(bank(b)[0:n, :],
                         [(dst[:, k, tok0(c):tok0(c) + n], RING[:, so[oh], k, :]) for k in range(8)],
                         [("ring", so[oh])] + [dst_cells(k, c) for k in range(8)], [bk(b)])
                dve("tensor_tensor", [bk(b), ("x", c)], [("x", c)], out=X[0:n, c, oh * 512:(oh + 1) * 512],
                    in0=bank(b)[0:n, :], in1=X[0:n, c, oh * 512:(oh + 1) * 512], op=ALU.add)

    def gn_elem(n, src, src_cells, sgi):
        jb = rot("wb")
        act(WB[0:n, jb, 0:512], src, AF.Square, src_cells + [("st1",)], [("wb", jb), ("st1", 5)],
            accum_out=ST1[0:n, 5:6])
        dve("tensor_scalar", [("st1", 5)], [("st1", 6)], out=ST1[0:n, 6:7], in0=ST1[0:n, 5:6],
            scalar1=1.0 / DV, scalar2=EPS, op0=ALU.mult, op1=ALU.add)
        pool_pow(ST1[0:n, 6:7], [("st1", 6)])
        rb = rot("rr")
        dve("scalar_tensor_tensor", src_cells + [("st1", 6), ("sg", sgi)], [("rr", rb)], out=RR[0:n, rb, :], in0=src,
            scalar=ST1[0:n, 6:7], in1=SG[0:n, sgi, :], op0=ALU.mult, op1=ALU.mult)
        return rb

    def gn_transpose(n, h, c, rb):
        transposes(None, [(PT[0][:, 4 + e, 0:n], RR[0:n, rb, e * 128:(e + 1) * 128]) for e in range(4)],
                   IDB[0:n, 0:n], [("rr", rb), ("idb",)], [("pt", 0)])
        act(BIG[:, h * 4:(h + 1) * 4, tok0(c):tok0(c) + n], PT[0][:, 4:8, 0:n], AF.Identity, [("pt", 0)],
            [("big", h * 4 + e, c) for e in range(4)])

    GB = {"b": 1}

    def proj3(c, n, which, slot):
        bi = {"qk": 0, "v": 1, "g": GB["b"]}[which]
        mm_group(bank(bi)[0:n, :], [(XNT[:, k, tok0(c):tok0(c) + n], RING[:, slot, k, :]) for k in range(8)],
                 [("ring", slot), ("xnT", c)], [bk(bi)])

    def phase_R(t, l, h, slots):
        sqk, sv, sgs = slots
        gam = float(GAM[h])
        have_state = (t > 0)
        GB["b"] = 1 if t == 0 else 2
        wcur = None
        if have_state:
            dma("sp", "si", SBUF_S[:, 0, :, :], nrp[l, h].rearrange("(a p) e -> p a e", p=128), [("nrp", l, h)], [("ss", 0)])
            w0 = rot("wc")
            act(WC[:, w0, :, :], SBUF_S[:, 0, :, :], AF.Identity, [("ss", 0)], [("wc", w0)])
            wcur = w0
        gen = None
        if t == 0:
            gen = sample_part(l, h, slots, gam)
            next(gen)

        def bg(k=1):
            nonlocal gen
            if os.environ.get('KBG', '1') == '0':
                return
            for _ in range(k):
                if gen is None:
                    return
                try:
                    next(gen)
                except StopIteration:
                    gen = None

        prev = None
        pend_tr = None
        first = True
        csbuf = {}

        def load_cs(cc):
            bb = rot("cs")
            csbuf[cc] = bb
            dma("sp", "par", CSB[:, bb, :, :], cs_d[t, :, cc, :, :], [], [("cs", bb)])
        load_cs(0)
        for s in range(nch + 1):
            c = s if s < nch else None
            n = 128
            if c is not None and c + 1 < nch:
                load_cs(c + 1)
            cur = None
            if c is not None:
                proj3(c, n, "qk", sqk)
                ra = rot("wf")
                rb_ = rot("wf")
                for qk in range(2):
                    src = bank(0)[0:n, qk * 256:(qk + 1) * 256].rearrange("p (a f) -> p a f", a=2)
                    for (ti, wfi) in ((0, ra), (1, rb_)):
                        dve("scalar_tensor_tensor", [bk(0), ("dqk",), ("cs", csbuf[c])], [("wf", wfi, 0)],
                            out=WF[0:n, wfi, qk * 256:(qk + 1) * 256].rearrange("p (a f) -> p a f", a=2),
                            in0=src, scalar=DQK[0:n, c, qk, h:h + 1],
                            in1=CSB[0:n, csbuf[c], ti, :].unsqueeze(1).to_broadcast([n, 2, 128]), op0=ALU.mult, op1=ALU.mult)
                qpn = rot("qkp")
                A4 = WF[0:n, ra, 0:512].rearrange("p (q a f) -> p q a f", q=2, a=2)
                B4 = WF[0:n, rb_, 0:512].rearrange("p (q a f) -> p q a f", q=2, a=2)
                O4 = QKP[0:n, qpn, :].rearrange("p (q a f) -> p q a f", q=2, a=2)
                rc = [("wf", ra, 0), ("wf", rb_, 0)]
                dve("tensor_tensor", rc, [("qkp", qpn, 0)], out=O4[:, :, 0, :], in0=A4[:, :, 0, :], in1=B4[:, :, 1, :], op=ALU.subtract)
                dve("tensor_tensor", rc, [("qkp", qpn, 1)], out=O4[:, :, 1, :], in0=B4[:, :, 0, :], in1=A4[:, :, 1, :], op=ALU.add)
                cur = {"c": c, "qp": qpn}
            bg()
            if prev is not None:
                qp, vb = prev["qp"], prev["vb"]
                transposes(None, [(PT[0][:, j, 0:n], QKP[0:n, qp, j * 128:(j + 1) * 128]) for j in range(4)],
                           IDB[0:n, 0:n], [("qkp", qp, 0), ("qkp", qp, 1), ("idb",)], [("pt", 0)])
                qt = rot("qkt")
                act(QKT[:, qt, :, 0:n], PT[0][:, 0:4, 0:n], AF.Identity, [("pt", 0)], [("qkt", qt)])
                bg()
            if c is not None:
                proj3(c, n, "v", sv)
                vbn = rot("vb")
                act(VB[0:n, vbn, :], bank(1)[0:n, :], AF.Identity, [bk(1)], [("vb", vbn)])
                cur["vb"] = vbn
            bg()
            if pend_tr is not None:
                gn_transpose(n, h, pend_tr[0], pend_tr[1])
                pend_tr = None
                bg()
            if prev is not None:
                mm_group(bank(1)[0:n, 0:n], [(QKT[:, qt, 2 + hf, 0:n], QKT[:, qt, hf, 0:n]) for hf in range(2)],
                         [("qkt", qt)], [bk(1)])
                sc = rot("sct")
                dve("tensor_tensor", [bk(1), ("mask",)], [("sct", sc)], out=SCT[0:n, sc, 0:n], in0=bank(1)[0:n, 0:n],
                    in1=MASK[0:n, 0:n], op=ALU.mult)
                bg()
            if c is not None:
                proj3(c, n, "g", sgs)
                sgn = rot("sg")
                act(SG[0:n, sgn, :], bank(GB["b"])[0:n, :], AF.Silu, [bk(GB["b"])], [("sg", sgn)])
                cur["sg"] = sgn
            bg()
            if prev is not None:
                pairs = [(SCT[0:n, sc, 0:n], VB[0:n, vb, :])]
                rds = [("sct", sc), ("vb", vb)]
                if have_state or not first:
                    pairs += [(QKT[:, qt, hf, 0:n], WC[:, wcur, hf, :]) for hf in range(2)]
                    rds += [("qkt", qt), ("wc", wcur)]
                mm_group(bank(3)[0:n, :], pairs, rds, [bk(3)])
                for hf in range(2):
                    def fn(e, hf=hf, qp=qp, vb=vb, first=first):
                        return e.matmul(bank(4 + hf)[:, :], lhsT=QKP[0:128, qp, 256 + hf * 128:256 + (hf + 1) * 128],
                                        rhs=VB[0:128, vb, :], start=first, stop=True, skip_group_check=True)
                    P.op("pe", fn, reads=[("qkp", qp, 0), ("qkp", qp, 1), ("vb", vb)], writes=[bk(4 + hf)])
                first = False
                if prev["c"] < nch - 1:
                    wn = rot("wc")
                    if have_state:
                        dve("tensor_tensor", [bk(4), bk(5), ("ss", 0)], [("wc", wn)], out=WC[:, wn, :, :], in0=P45[:],
                            in1=SBUF_S[:, 0, :, :], op=ALU.add)
                    else:
                        act(WC[:, wn, :, :], P45[:], AF.Identity, [bk(4), bk(5)], [("wc", wn)])
                    wcur = wn
            bg()
            if prev is not None:
                rbuf = gn_elem(n, bank(3)[0:n, :], [bk(3)], prev["sg"])
                pend_tr = (prev["c"], rbuf)
            bg()
            prev = cur
        if pend_tr is not None:
            gn_transpose(128, h, pend_tr[0], pend_tr[1])
        sn = rot("wf")
        sview = WF[:, sn, :].rearrange("p (a e) -> p a e", a=2)
        if have_state:
            dve("tensor_tensor", [bk(4), bk(5), ("ss", 0)], [("wf", sn, 0), ("wf", sn, 1)], out=sview, in0=P45[:],
                in1=SBUF_S[:, 0, :, :], op=ALU.add)
            act(sview, sview, AF.Identity, [("wf", sn, 0), ("wf", sn, 1)], [("wf", sn, 0), ("wf", sn, 1)], scale=float(GEND[h]))
        else:
            act(sview, P45[:], AF.Identity, [bk(4), bk(5)], [("wf", sn, 0), ("wf", sn, 1)], scale=float(GEND[h]))
        dma("sp", "so", nrp[l, h].rearrange("(a p) e -> p a e", p=128), sview, [("wf", sn, 0), ("wf", sn, 1)], [("nrp", l, h)])
        if gen is not None:
            for _ in gen:
                pass

    def sample_part(l, h, slots, gam):
        sqk, sv, sgs = slots
        c = nch
        n = ns
        proj3(c, n, "qk", sqk)
        proj3(c, n, "v", sv)
        act(VBS[0:n, :], bank(1)[0:n, :], AF.Identity, [bk(1)], [("vbs",)])
        proj3(c, n, "g", sgs)
        act(SG[0:n, 2, :], bank(1)[0:n, :], AF.Silu, [bk(1)], [("sg", 2)])
        ra = rot("wf")
        rb_ = rot("wf")
        for qk in range(2):
            src = bank(0)[0:n, qk * 256:(qk + 1) * 256].rearrange("p (a f) -> p a f", a=2)
            for (ti, wfi) in ((0, ra), (1, rb_)):
                dve("scalar_tensor_tensor", [bk(0), ("css",)], [("wf", wfi, 0)],
                    out=WF[0:n, wfi, qk * 256:(qk + 1) * 256].rearrange("p (a f) -> p a f", a=2),
                    in0=src, scalar=(1.0 if qk == 0 else DK ** -0.5),
                    in1=CSS[0:n, ti, :].unsqueeze(1).to_broadcast([n, 2, 128]), op0=ALU.mult, op1=ALU.mult)
        A4 = WF[0:n, ra, 0:512].rearrange("p (q a f) -> p q a f", q=2, a=2)
        B4 = WF[0:n, rb_, 0:512].rearrange("p (q a f) -> p q a f", q=2, a=2)
        rc = [("wf", ra, 0), ("wf", rb_, 0)]
        QS2 = QS[:].rearrange("p (a f) -> p a f", a=2)
        KS2 = KS[:].rearrange("p (a f) -> p a f", a=2)
        dve("tensor_tensor", rc, [("qs", 0)], out=QS2[:, 0, :], in0=A4[:, 0, 0, :], in1=B4[:, 0, 1, :], op=ALU.subtract)
        dve("tensor_tensor", rc, [("qs", 1)], out=QS2[:, 1, :], in0=B4[:, 0, 0, :], in1=A4[:, 0, 1, :], op=ALU.add)
        dve("tensor_tensor", rc, [("ks", 0)], out=KS2[:, 0, :], in0=A4[:, 1, 0, :], in1=B4[:, 1, 1, :], op=ALU.subtract)
        dve("tensor_tensor", rc, [("ks", 1)], out=KS2[:, 1, :], in0=B4[:, 1, 0, :], in1=A4[:, 1, 1, :], op=ALU.add)
        transposes(None, [(bank(1)[:, hf * 16:hf * 16 + n], QS[0:n, hf * 128:(hf + 1) * 128]) for hf in range(2)],
                   IDF[0:n, 0:n], [("qs", 0), ("qs", 1), ("idf",)], [bk(1)])
        for b in range(n):
            dve("tensor_copy", [bk(1), ("qm",)], [("qm", b)], out=QM[:, b, :, b:b + 1],
                in_=bank(1)[:, 0:32].rearrange("p (a j) -> p a j", a=2)[:, :, b:b + 1])
        qkt_ = rot("wf")
        dve("tensor_tensor", [("qs", 0), ("qs", 1), ("ks", 0), ("ks", 1)], [("wf", qkt_, 0)], out=WF[0:n, qkt_, 0:256],
            in0=QS[0:n, :], in1=KS[0:n, :], op=ALU.mult)
        dve("reduce_sum", [("wf", qkt_, 0), ("st1",)], [("st1", 14)], out=ST1[0:n, 14:15], in_=WF[0:n, qkt_, 0:256],
            axis=mybir.AxisListType.X)
        yield
        for b in range(n):
            km = rot("km")
            dve("tensor_scalar", [("ks", 0), ("ks", 1), ("idf",)], [("km", km)], out=KM[0:n, km, :], in0=KS[0:n, :],
                scalar1=IDF[0:n, b:b + 1], scalar2=None, op0=ALU.mult)
            ss = rot("ss")
            dma("sp", "si", SBUF_S[:, ss, :, :], st[l, b, h].rearrange("(a p) e -> p a e", p=128), [], [("ss", ss)])
            for hf in range(2):
                def fn(e, b=b, hf=hf, ss=ss):
                    return e.matmul(PB_[2][0:ns, :], lhsT=QM[:, b, hf, :], rhs=SBUF_S[:, ss, hf, :],
                                    start=(b == 0 and hf == 0), stop=(b == ns - 1 and hf == 1), skip_group_check=True)
                P.op("pe", fn, reads=[("qm", b), ("ss", ss)], writes=[bk(2)])
            yield
            for hf in range(2):
                mm_group(PX[:, :], [(KM[0:n, km, hf * 128:(hf + 1) * 128], VBS[0:n, :])],
                         [("km", km), ("vbs",)], [bk(6)])
                dve("scalar_tensor_tensor", [("ss", ss), bk(6)], [("ss", ss)], out=SBUF_S[:, ss, hf, :],
                    in0=SBUF_S[:, ss, hf, :], scalar=gam, in1=PX[:, :], op0=ALU.mult, op1=ALU.add)
                if hf == 1:
                    dma("sp", "so", nrs[l, b, h].rearrange("(a p) e -> p a e", p=128), SBUF_S[:, ss, :, :], [("ss", ss)],
                        [("nrs", l, b, h)])
                yield
        tq = rot("wf")
        dve("tensor_scalar", [("vbs",), ("st1", 14)], [("wf", tq, 0)], out=WF[0:n, tq, 0:512], in0=VBS[0:n, :],
            scalar1=ST1[0:n, 14:15], scalar2=None, op0=ALU.mult)
        dve("scalar_tensor_tensor", [bk(2), ("wf", tq, 0)], [("osa",)], out=OSA[:], in0=PB_[2][0:ns, :], scalar=gam,
            in1=WF[0:n, tq, 0:512], op0=ALU.mult, op1=ALU.add)
        rbuf = gn_elem(n, OSA[:], [("osa",)], 2)
        gn_transpose(n, h, c, rbuf)

    def phase_F(t, l, groups):
        tgs = tokgroups(t)
        pb = 0
        for g in range(4):
            su = groups[2 * g]()
            buf = g % 2
            for (t0, n, cs) in tgs:
                for ui in range(2):
                    for cc in range(4):
                        b = pb % 2
                        pb += 1
                        kk = ui * 4 + cc
                        mm_group(bank(b)[:, 0:n],
                                 [(RING[:, su[ui], k, cc * 128:(cc + 1) * 128], XNT[:, k, t0:t0 + n]) for k in range(8)],
                                 [("ring", su[ui])] + [("xnT", c) for c in cs], [bk(b)])
                        rl = rot("wf")
                        act(WF[:, rl, 0:n], bank(b)[:, 0:n], AF.Relu, [bk(b)], [("wf", rl, 0)])
                        dve("tensor_tensor", [("wf", rl, 0)], [("big", buf * 8 + kk, c) for c in cs],
                            out=BIG[:, buf * 8 + kk, t0:t0 + n], in0=WF[:, rl, 0:n], in1=WF[:, rl, 0:n], op=ALU.mult)
            sd = groups[2 * g + 1]()
            for c in chunk_list(t):
                n = ntok(c)
                for oh in range(2):
                    b = 2 + (pb % 2)
                    pb += 1
                    mm_group(bank(b)[0:n, :],
                             [(BIG[:, buf * 8 + k, tok0(c):tok0(c) + n], RING[:, sd[oh], k, :]) for k in range(8)],
                             [("ring", sd[oh])] + [("big", buf * 8 + k, c) for k in range(8)], [bk(b)])
                    dve("tensor_tensor", [bk(b), ("x", c)], [("x", c)], out=X[0:n, c, oh * 512:(oh + 1) * 512],
                        in0=bank(b)[0:n, :], in1=X[0:n, c, oh * 512:(oh + 1) * 512], op=ALU.add)

    def weight_groups(l):
        G = []
        G.append([win_unit(l, 0), win_unit(l, 512)])
        G.append([win_unit(l, 1024), win_unit(l, 1536)])
        for ch in range(2):
            G.append([[(w_pgm[l, :, ch * 512:(ch + 1) * 512], 0, 512)], win_unit(l, 8192 + ch * 512)])
        G.append([[(w_out[l, :, 0:512], 0, 512)], [(w_out[l, :, 512:1024], 0, 512)]])
        for h in range(H):
            G.append([[(w_in[l, :, 2048 + h * 256:2048 + (h + 1) * 256], 0, 256),
                       (w_in[l, :, 3072 + h * 256:3072 + (h + 1) * 256], 256, 256)],
                      win_unit(l, 4096 + h * 512), win_unit(l, 6144 + h * 512)])
        for ch in range(2):
            G.append([[(w_pret[l, 0:1024, ch * 512:(ch + 1) * 512], 0, 512)],
                      [(w_pret[l, 1024:2048, ch * 512:(ch + 1) * 512], 0, 512)], win_unit(l, 9216 + ch * 512)])
        G.append([[(w_out[l, :, 0:512], 0, 512)], [(w_out[l, :, 512:1024], 0, 512)]])
        for g in range(4):
            G.append([[(w_up[l, :, g * 1024:g * 1024 + 512], 0, 512)], [(w_up[l, :, g * 1024 + 512:(g + 1) * 1024], 0, 512)]])
            G.append([[(w_down[l, g * 1024:(g + 1) * 1024, 0:512], 0, 512)], [(w_down[l, g * 1024:(g + 1) * 1024, 512:1024], 0, 512)]])
        return G

    allgroups = []
    for t in range(ntiles):
        for l in range(depth):
            allgroups += weight_groups(l)
    gstate = {"next_load": 0, "loaded": {}, "next_use": 0}

    def issue_group_load():
        gi = gstate["next_load"]
        if gi >= len(allgroups):
            return
        gstate["next_load"] += 1
        base = (gi % 2) * 3
        slots = []
        for ui, parts in enumerate(allgroups[gi]):
            slot = base + ui
            for (src, coff, ncols) in parts:
                dma("pool", "w", RING[:, slot, :, coff:coff + ncols], src.rearrange("(k p) n -> p k n", p=128),
                    reads=[], writes=[("ring", slot)])
            slots.append(slot)
        gstate["loaded"][gi] = slots

    def use_group():
        gi = gstate["next_use"]
        gstate["next_use"] += 1
        while gstate["next_load"] <= gi + 1 and gstate["next_load"] < len(allgroups):
            issue_group_load()
        return gstate["loaded"][gi]

    for t in range(ntiles):
        dma("sp", "io", X[:, 0:nch, :], xp[t * nch * 128:(t + 1) * nch * 128, :].rearrange("(c p) d -> p c d", p=128),
            [], [("x", c) for c in range(nch)])
        if t == 0:
            dma("sp", "io", X[0:ns, nch, :], xs, [], [("x", nch)])
        for l in range(depth):
            phase_norm(t, l, 0)
            su = use_group()
            phase_G(t, l, su, use_group, after_u=lambda: load_layer_params(t, l))

            def pa_group():
                s = use_group()
                return [s[0]], s[1]
            phase_P(t, l, AT, lambda k, c: ("aT", k, c), 8, 0, BIG, lambda k, c: ("big", k, c),
                    [pa_group, pa_group, use_group])
            for h in range(H):
                phase_R(t, l, h, use_group())

            def pb_group():
                s = use_group()
                return [s[0], s[1]], s[2]
            phase_P(t, l, BIG, lambda k, c: ("big", k, c), 16, 8, AT, lambda k, c: ("aT", k, c),
                    [pb_group, pb_group, use_group])
            phase_norm(t, l, 1)
            phase_F(t, l, [use_group] * 8)
        cl = chunk_list(t)
        for c in cl:
            n = ntok(c)
            jb = rot("wb")
            act(WB[0:n, jb, :], X[0:n, c, :], AF.Square, [("x", c), ("ssq",)], [("wb", jb), ("ssq", c)],
                accum_out=SSQ[0:n, c:c + 1])
        ncl = len(cl)
        dve("tensor_scalar", [("ssq", c) for c in cl] + [("ssq",)], [("rs",)], out=RS[:, 0:ncl], in0=SSQ[:, 0:ncl],
            scalar1=1.0 / D, scalar2=EPS, op0=ALU.mult, op1=ALU.add)
        pool_pow(RS[:, 0:ncl], [("rs",)])
        gf = rot("wf")
        dma("sp", "par", WF[:, gf, :], gfin_d.partition_broadcast(128), [], [("wf", gf, 0), ("wf", gf, 1)])
        for c in cl:
            n = ntok(c)
            dve("scalar_tensor_tensor", [("x", c), ("rs",), ("wf", gf, 0), ("wf", gf, 1)], [("x", c)], out=X[0:n, c, :],
                in0=X[0:n, c, :], scalar=RS[0:n, c:c + 1], in1=WF[0:n, gf, :], op0=ALU.mult, op1=ALU.mult)
        dma("sp", "io", yp[t * nch * 128:(t + 1) * nch * 128, :].rearrange("(c p) d -> p c d", p=128), X[:, 0:nch, :],
            [("x", c) for c in range(nch)], [("yp", t)])
        if t == 0:
            dma("sp", "io", ys, X[0:ns, nch, :], [("x", nch)], [("ys",)])

    outs = [("yp", t) for t in range(ntiles)] + [("ys",)] + [("gmv", l) for l in range(depth)]
    outs += [("nrp", l, h) for l in range(depth) for h in range(H)]
    outs += [("nrs", l, b, h) for l in range(depth) for b in range(ns) for h in range(H)]
    P.op("sp", lambda e: None, reads=outs, writes=[])

    block = es.enter_context(nc.Block())
    P.emit(nc, {"sp": block.sync, "act": block.scalar, "dve": block.vector, "pool": block.gpsimd, "pe": block.tensor},
           sem_alloc, dma_classes)
    es.close()
    P.sbuf_free = SBUF_FREE
    return nc, P


def _prep_common(cfg, inputs):
    depth = cfg.depth
    tabs, _, _ = _host_tables(cfg)
    f = lambda a: np.ascontiguousarray(np.asarray(a, dtype=np.float32))
    g3 = np.stack([f(inputs["norm_mix_g"])[:depth], f(inputs["norm_ffn_g"])[:depth], f(inputs["gm_ln_g"])[:depth]], axis=1)
    gcols = np.ascontiguousarray(g3.reshape(depth, 3, 8, 128).transpose(3, 0, 1, 2))
    bgc = np.ascontiguousarray(f(inputs["b_gate"])[:depth].reshape(depth, 16, 128).transpose(2, 0, 1))
    wt = np.ascontiguousarray(f(inputs["gm_w_s"])[:depth].transpose(0, 3, 1, 2))
    w00 = np.ascontiguousarray(f(inputs["gm_w_s"])[:depth, :, 0, 0])
    common = dict(
        w_in=f(inputs["w_in"])[:depth], w_pgm=f(inputs["w_proj_gm"])[:depth], w_pret=f(inputs["w_proj_ret"])[:depth],
        w_out=f(inputs["w_out"])[:depth], w_up=f(inputs["w_up"])[:depth], w_down=f(inputs["w_down"])[:depth],
        gcols=gcols, gfin=f(inputs["norm_final_g"]), bgcols=bgc, lng=f(inputs["gm_ln_g"])[:depth],
        lnb=f(inputs["gm_ln_b"])[:depth], wt=wt, w00=w00, bs=f(inputs["gm_b_s"])[:depth].reshape(depth, 512),
        cs=tabs["cs"], cs_s=tabs["cs_s"], dqk=tabs["dqk"], mask=tabs["mask"], ident=tabs["ident"],
    )
    return common


_CACHE = {}


def run(cfg, inputs, ncores, trace=False):
    key = (cfg.depth, cfg.nch, cfg.ntiles, cfg.ns)
    if key not in _CACHE:
        _CACHE[key] = build_nc(cfg)
    nc, _ = _CACHE[key]
    common = _prep_common(cfg, inputs)
    xp = np.asarray(inputs["x_prompt"], dtype=np.float32)
    xs = np.asarray(inputs["x_sample"], dtype=np.float32)
    st = np.asarray(inputs["state_ret"], dtype=np.float32)
    ns = cfg.ns
    in_maps = []
    for b in range(ncores):
        m = dict(common)
        m["xp"] = np.ascontiguousarray(xp[b])
        m["xs"] = np.ascontiguousarray(xs[b * ns:(b + 1) * ns, 0, :])
        m["st"] = np.ascontiguousarray(st[:cfg.depth, b * ns:(b + 1) * ns])
        in_maps.append(m)
    res = run_bass_kernel_spmd(nc, in_maps, core_ids=list(range(ncores)), trace=trace)
    rs = res.results
    y_prompt = np.stack([rs[b]["yp"] for b in range(ncores)], axis=0)
    y_sample = np.concatenate([rs[b]["ys"] for b in range(ncores)], axis=0)[:, None, :]
    nrp = np.stack([rs[b]["nrp"] for b in range(ncores)], axis=1)
    nrs = np.concatenate([rs[b]["nrs"] for b in range(ncores)], axis=1)
    gmv = np.concatenate([rs[b]["gmv"] for b in range(ncores)], axis=1)[:, :, None, :]
    out = (y_prompt.astype(np.float32), y_sample.astype(np.float32), nrp.astype(np.float32), nrs.astype(np.float32),
           gmv.astype(np.float32))
    return out, res


def kernel(**inputs):
    cfg = Cfg(depth=4, nch=8, ntiles=2, ns=16)
    out, _ = run(cfg, inputs, 8)
    return out
```

```python
import os
import numpy as np
from contextlib import ExitStack
import concourse.bass as bass
import concourse.mybir as mybir
from concourse.bass_utils import run_bass_kernel_spmd

F32 = mybir.dt.float32
BF16 = mybir.dt.bfloat16
AF = mybir.ActivationFunctionType
ALU = mybir.AluOpType

D = 1024
H = 4
DK = 256
DV = 512
DFF = 4096
INW = 10240
EPS = 1e-6
NSLOT = 6
PAST_LEN = 16384


class Cfg:
    def __init__(self, depth=4, nch=8, ntiles=2, ns=16):
        self.depth, self.nch, self.ntiles, self.ns = depth, nch, ntiles, ns
        self.seq = nch * ntiles * 128
        self.ntok = nch * 128 + ns


class Prog:
    COMPUTE = ("pe", "act", "dve", "pool")

    def __init__(self):
        self.ops = []
        self.cells = {}

    def op(self, eng, fn, reads=(), writes=(), dma=None):
        oid = len(self.ops)
        deps = set()
        for k in reads:
            c = self.cells.get(k)
            if c is not None and c[0] is not None:
                deps.add(c[0])
        for k in writes:
            c = self.cells.get(k)
            if c is not None:
                if c[0] is not None:
                    deps.add(c[0])
                deps.update(c[1].values())
                deps.update(c[2])
        for k in reads:
            c = self.cells.get(k)
            if c is None:
                c = self.cells[k] = [None, {}, []]
            if dma is None:
                c[1][eng] = oid
            else:
                c[2].append(oid)
        for k in writes:
            self.cells[k] = [oid, {}, []]
        deps.discard(oid)
        self.ops.append(dict(eng=eng, fn=fn, deps=deps, dma=dma))
        return oid

    def emit(self, nc, block_engines, sem_alloc, dma_classes):
        ops = self.ops
        needed = set()
        for o in ops:
            needed.update(o["deps"])
        ROLL = 30000
        cnt = {e: 0 for e in self.COMPUTE}
        epoch = {e: 0 for e in self.COMPUTE}
        esems = {e: [sem_alloc("c_%s_0" % e)] for e in self.COMPUTE}
        dcnt = {}
        for i, o in enumerate(ops):
            o["pre"] = None
            if o["dma"] is not None:
                cls = o["dma"]
                sems = dma_classes[cls]
                n = dcnt.get(cls, 0)
                dcnt[cls] = n + 1
                s = sems[n % len(sems)]
                k = n // len(sems)
                o["pre"] = (s, 16 * k) if k > 0 else None
                o["ev"] = (s, 16 * (k + 1))
                o["inc"] = (s, 16)
            elif o["eng"] in self.COMPUTE and i in needed:
                e = o["eng"]
                if cnt[e] >= ROLL:
                    epoch[e] += 1
                    cnt[e] = 0
                    esems[e].append(sem_alloc("c_%s_%d" % (e, epoch[e])))
                cnt[e] += 1
                s = esems[e][epoch[e]]
                o["ev"] = (s, cnt[e])
                o["inc"] = (s, 1)
            else:
                o["ev"] = None
                o["inc"] = None
        per = {}
        for i, o in enumerate(ops):
            per.setdefault(o["eng"], []).append(i)
        self.nwaits = 0

        def run(engname, e):
            seen = {}
            for i in per.get(engname, []):
                o = ops[i]
                want = {}
                for d in o["deps"]:
                    od = ops[d]
                    if od["dma"] is None and od["eng"] == "pe" and engname == "pe":
                        continue
                    ev = od["ev"]
                    if ev is None:
                        continue
                    s, v = ev
                    if want.get(id(s), (None, -1))[1] < v:
                        want[id(s)] = (s, v)
                if o["pre"] is not None:
                    s, v = o["pre"]
                    if want.get(id(s), (None, -1))[1] < v:
                        want[id(s)] = (s, v)
                for sid, (s, v) in want.items():
                    if seen.get(sid, -1) >= v:
                        continue
                    seen[sid] = v
                    e.wait_ge(s, v)
                    self.nwaits += 1
                ins = o["fn"](e)
                if o["inc"] is not None:
                    assert ins is not None
                    ins.then_inc(o["inc"][0], o["inc"][1])

        for engname, deco in block_engines.items():
            deco((lambda en: (lambda e: run(en, e)))(engname))


def _host_tables(cfg):
    nch, ntiles = cfg.nch, cfg.ntiles
    inv_freq = (1.0 / (np.float32(10000.0) ** np.linspace(0.0, 1.0, DK // 2, dtype=np.float32))).astype(np.float32)
    pos = np.arange(cfg.seq, dtype=np.int32).astype(np.float32)
    ang = (pos[:, None] * inv_freq[None, :]).astype(np.float32)
    cos = np.cos(ang).astype(np.float32).reshape(ntiles, nch, 128, 128).transpose(0, 2, 1, 3)
    sin = np.sin(ang).astype(np.float32).reshape(ntiles, nch, 128, 128).transpose(0, 2, 1, 3)
    angs = (np.float32(PAST_LEN) * inv_freq).astype(np.float32)
    cs_s = np.stack([np.cos(angs), np.sin(angs)]).astype(np.float32)
    cs_s = np.broadcast_to(cs_s[None], (16, 2, 128)).copy()
    g = 1.0 - 2.0 ** (-5.0 - np.arange(H, dtype=np.float64))
    p = (np.arange(nch)[None, :, None] * 128 + np.arange(128)[:, None, None] + 1).astype(np.float64)
    dq = (g[None, None, :] ** p).astype(np.float32)
    dk = ((g[None, None, :] ** (-p)) * (DK ** -0.5)).astype(np.float32)
    dqk = np.stack([dq, dk], axis=2).copy()
    j = np.arange(128)[:, None]
    i = np.arange(128)[None, :]
    mask = (i >= j).astype(np.float32)
    ident = np.eye(128, dtype=np.float32)
    gam = g
    gend = g ** (128 * nch)
    cs = np.ascontiguousarray(np.stack([cos, sin], axis=3))
    return dict(cs=cs, cs_s=cs_s, dqk=dqk, mask=mask,
                ident=ident), gam, gend


def build_nc(cfg):
    depth, nch, ntiles, ns = cfg.depth, cfg.nch, cfg.ntiles, cfg.ns
    NT = cfg.ntok
    _, GAM, GEND = _host_tables(cfg)
    nc = bass.Bass("TRN2", target_bir_lowering=False)

    def din(name, shape):
        return nc.dram_tensor(name, list(shape), F32, kind="ExternalInput").ap()

    def dout(name, shape):
        return nc.dram_tensor(name, list(shape), F32, kind="ExternalOutput").ap()

    xp = din("xp", [cfg.seq, D])
    xs = din("xs", [ns, D])
    st = din("st", [depth, ns, H, DK, DV])
    w_in = din("w_in", [depth, D, INW])
    w_pgm = din("w_pgm", [depth, D, D])
    w_pret = din("w_pret", [depth, 2 * D, D])
    w_out = din("w_out", [depth, D, D])
    w_up = din("w_up", [depth, D, DFF])
    w_down = din("w_down", [depth, DFF, D])
    gcols_d = din("gcols", [128, depth, 3, 8])
    gfin_d = din("gfin", [D])
    bg_d = din("bgcols", [128, depth, 16])
    lng_d = din("lng", [depth, D])
    lnb_d = din("lnb", [depth, D])
    wt_d = din("wt", [depth, 128, 4, 128])
    w00_d = din("w00", [depth, 4])
    bs_d = din("bs", [depth, 4 * 128])
    cs_d = din("cs", [ntiles, 128, nch, 2, 128])
    css_d = din("cs_s", [16, 2, 128])
    dqk_d = din("dqk", [128, nch, 2, H])
    mask_d = din("mask", [128, 128])
    ident_d = din("ident", [128, 128])

    yp = dout("yp", [cfg.seq, D])
    ys = dout("ys", [ns, D])
    nrp = dout("nrp", [depth, H, DK, DV])
    nrs = dout("nrs", [depth, ns, H, DK, DV])
    gmv = dout("gmv", [depth, ns, D])

    P = Prog()
    es = ExitStack()

    def sb(name, shape, dt):
        return es.enter_context(nc.sbuf_tensor(name, list(shape), dt))

    def ps(name, shape, dt):
        return es.enter_context(nc.psum_tensor(name, list(shape), dt))

    X = sb("X", [128, nch + 1, D], F32)
    XNT = sb("XNT", [128, 8, NT], BF16)
    AT = sb("AT", [128, 8, NT], BF16)
    BIG = sb("BIG", [128, 16, NT], BF16)
    RING = sb("RING", [128, NSLOT, 8, 512], BF16)
    CSB = sb("CSB", [128, 2, 2, 128], F32)
    CSS = sb("CSS", [16, 2, 128], F32)
    DQK = sb("DQK", [128, nch, 2, H], F32)
    MASK = sb("MASK", [128, 128], F32)
    IDF = sb("IDF", [128, 128], F32)
    IDB = sb("IDB", [128, 128], BF16)
    ONESB = sb("ONESB", [1, 128], BF16)
    MHALF = sb("MHALF", [128, 16], F32)
    GCOLS = sb("GCOLS", [128, depth, 3, 8], F32)
    BGC = sb("BGC", [128, depth, 16], F32)
    WC = sb("WC", [128, 2, 2, 512], BF16)
    SBUF_S = sb("SBUF_S", [128, 2, 2, 512], F32)
    WF = sb("WF", [128, 2, 1024], F32)
    WB = sb("WB", [128, 2, 1024], BF16)
    QKP = sb("QKP", [128, 2, 512], BF16)
    VB = sb("VB", [128, 2, 512], BF16)
    SG = sb("SG", [128, 3, 512], BF16)
    VBS = sb("VBS", [16, 512], BF16)
    OSA = sb("OSA", [16, 512], F32)
    QKT = sb("QKT", [128, 2, 4, 128], BF16)
    SCT = sb("SCT", [128, 2, 128], BF16)
    RR = sb("RR", [128, 2, 512], BF16)
    RTAB = sb("RTAB", [128, 8, 128], F32)
    RTABS = sb("RTABS", [128, 8, 16], F32)
    WTM = sb("WTM", [128, 4, 128], BF16)
    DG = sb("DG", [16, 4, 16], BF16)
    W00 = sb("W00", [16, 4], F32)
    BSH = sb("BSH", [1, 4, 128], BF16)
    BSL = sb("BSL", [1, 4, 128], BF16)
    BS0H = sb("BS0H", [1, 4, 16], BF16)
    BS0L = sb("BS0L", [1, 4, 16], BF16)
    SSQ = sb("SSQ", [128, 16], F32)
    RS = sb("RS", [128, 16], F32)
    ST1 = sb("ST1", [128, 16], F32)
    QM = sb("QM", [128, 16, 2, 16], F32)
    QS = sb("QS", [16, 256], F32)
    KS = sb("KS", [16, 256], BF16)
    KM = sb("KM", [16, 2, 256], BF16)

    SBUF_FREE = nc.sbuf_bytes_remaining
    PB_ = [ps("P%d" % i, [128, 512], F32) for i in range(4)]
    P45 = ps("P45", [128, 2, 512], F32)
    PT = [ps("PT0", [128, 8, 128], BF16)]
    PX = ps("PX", [128, 512], F32)

    def bank(i):
        return PB_[i] if i < 4 else P45[:, i - 4, :]

    def bk(i):
        return ("ps", i)

    sems = []

    def sem_alloc(name):
        s = es.enter_context(nc.semaphore(name))
        sems.append(s)
        return s

    dma_classes = {
        "w": [sem_alloc("dw%d" % i) for i in range(NSLOT)],
        "io": [sem_alloc("dio%d" % i) for i in range(4)],
        "par": [sem_alloc("dpar%d" % i) for i in range(6)],
        "ppar": [sem_alloc("dppar%d" % i) for i in range(2)],
        "si": [sem_alloc("dsi%d" % i) for i in range(2)],
        "so": [sem_alloc("dso%d" % i) for i in range(2)],
    }

    def chunk_list(t):
        cl = list(range(nch))
        if t == 0:
            cl.append(nch)
        return cl

    def ntok(c):
        return 128 if c < nch else ns

    def tok0(c):
        return c * 128

    def tokgroups(t):
        tgs = []
        c = 0
        while c < nch:
            n = min(4, nch - c)
            tgs.append((c * 128, n * 128, list(range(c, c + n))))
            c += n
        if t == 0:
            tgs.append((nch * 128, ns, [nch]))
        return tgs

    def dma(eng, cls, out, in_, reads, writes):
        P.op(eng, lambda e: e.dma_start(out=out, in_=in_), reads=reads, writes=writes, dma=cls)

    wstate = {"n": 0}

    def wload(parts):
        slot = wstate["n"] % NSLOT
        wstate["n"] += 1
        for (src, coff, ncols) in parts:
            dma("pool", "w", RING[:, slot, :, coff:coff + ncols], src.rearrange("(k p) n -> p k n", p=128),
                reads=[], writes=[("ring", slot)])
        return slot

    def win_unit(l, c0, n=512):
        return [(w_in[l, :, c0:c0 + n], 0, n)]

    def mm_group(out_ap, pairs, reads, writes, first=True, last=True):
        def fn(e):
            ins = None
            n = len(pairs)
            for i, (a, b) in enumerate(pairs):
                ins = e.matmul(out_ap, lhsT=a, rhs=b, start=(first and i == 0), stop=(last and i == n - 1))
            return ins
        P.op("pe", fn, reads=reads, writes=writes)

    def transposes(out_t, in_list, ident, reads, writes):
        def fn(e):
            ins = None
            for (o, i_) in in_list:
                ins = e.transpose(o, i_, ident)
            return ins
        P.op("pe", fn, reads=reads, writes=writes)

    def act(out, in_, func, reads, writes, **kw):
        P.op("act", lambda e: e.activation(out=out, in_=in_, func=func, **kw), reads=reads, writes=writes)

    def dve(fname, reads, writes, **kw):
        P.op("dve", lambda e: getattr(e, fname)(**kw), reads=reads, writes=writes)

    def pool_pow(ap, cells):
        n = ap.shape[-1]
        P.op("pool", lambda e: e.tensor_tensor(out=ap, in0=ap, in1=MHALF[0:ap.shape[0], 0:n], op=ALU.pow),
             reads=cells + [("mhalf",)], writes=cells)

    cnt = {"wf": 0, "wb": 0, "qkp": 0, "vb": 0, "qkt": 0, "sct": 0, "rr": 0, "pt": 0, "sg": 0, "cs": 0, "wc": 0, "ss": 0, "km": 0}

    def rot(name, n=2):
        v = cnt[name] % n
        cnt[name] += 1
        return v

    dma("sp", "par", CSS[:], css_d, [], [("css",)])
    dma("sp", "par", DQK[:], dqk_d, [], [("dqk",)])
    dma("sp", "par", MASK[:], mask_d, [], [("mask",)])
    dma("sp", "par", IDF[:], ident_d, [], [("idf",)])
    dma("sp", "par", GCOLS[:], gcols_d, [], [("gcols",)])
    dma("sp", "par", BGC[:], bg_d, [], [("bgc",)])
    dve("tensor_copy", [("idf",)], [("idb",)], out=IDB[:], in_=IDF[:])
    dve("memset", [], [("onesb",)], ap=ONESB[:], constant=1.0)
    dve("memset", [], [("mhalf",)], ap=MHALF[:], constant=-0.5)
    dve("memset", [], [("ssq",)], ap=SSQ[:], constant=1.0)
    dve("memset", [], [("st1",)], ap=ST1[:], constant=1.0)
    dve("memset", [], [("qm",)], ap=QM[:], constant=0.0)

    def phase_norm(t, l, gi):
        cl = chunk_list(t)
        for c in cl:
            n = ntok(c)
            jb = rot("wb")
            act(WB[0:n, jb, :], X[0:n, c, :], AF.Square, [("x", c), ("ssq",)], [("wb", jb), ("ssq", c)],
                accum_out=SSQ[0:n, c:c + 1])
        ncl = len(cl)
        dve("tensor_scalar", [("ssq", c) for c in cl] + [("ssq",)], [("rs",)], out=RS[:, 0:ncl], in0=SSQ[:, 0:ncl],
            scalar1=1.0 / D, scalar2=EPS, op0=ALU.mult, op1=ALU.add)
        pool_pow(RS[:, 0:ncl], [("rs",)])
        xbs = {}

        def do_xs(c):
            n = ntok(c)
            xb = rot("wb")
            xbs[c] = xb
            dve("tensor_scalar", [("x", c), ("rs",)], [("wb", xb)], out=WB[0:n, xb, :], in0=X[0:n, c, :],
                scalar1=RS[0:n, c:c + 1], scalar2=None, op0=ALU.mult)
        do_xs(cl[0])
        for i, c in enumerate(cl):
            n = ntok(c)
            if i + 1 < len(cl):
                do_xs(cl[i + 1])
            xb = xbs[c]
            transposes(None, [(PT[0][:, k, 0:n], WB[0:n, xb, k * 128:(k + 1) * 128]) for k in range(8)],
                       IDB[0:n, 0:n], [("wb", xb), ("idb",)], [("pt", 0)])
            dve("tensor_tensor", [("pt", 0), ("gcols",)], [("xnT", c)],
                out=XNT[:, :, tok0(c):tok0(c) + n], in0=PT[0][:, :, 0:n],
                in1=GCOLS[:, l, gi, :].unsqueeze(2).to_broadcast([128, 8, n]), op=ALU.mult)

    def load_layer_params(t, l):
        dma("pool", "ppar", WB[:, 1, :], lnb_d[l].partition_broadcast(128), [], [("wb", 1)])
        dma("sp", "par", WF[:, 0, 0:512].rearrange("p (g i) -> p g i", g=4), wt_d[l], [], [("wf", 0, 0)])
        dve("tensor_tensor", [("wf", 0, 0), ("mask",)], [("wtm",)], out=WTM[:],
            in0=WF[:, 0, 0:512].rearrange("p (g i) -> p g i", g=4),
            in1=MASK[:].unsqueeze(1).to_broadcast([128, 4, 128]), op=ALU.mult)
        dma("sp", "par", WF[0:1, 1, 0:512], bs_d[l].unsqueeze(0), [], [("wf", 1, 0)])
        dve("tensor_copy", [("wf", 1, 0)], [("bsh",)], out=BSH[:].rearrange("p g i -> p (g i)"), in_=WF[0:1, 1, 0:512])
        dve("tensor_tensor", [("wf", 1, 0), ("bsh",)], [("wf", 1, 1)], out=WF[0:1, 1, 512:1024], in0=WF[0:1, 1, 0:512],
            in1=BSH[:].rearrange("p g i -> p (g i)"), op=ALU.subtract)
        dve("tensor_copy", [("wf", 1, 1)], [("bsl",)], out=BSL[:].rearrange("p g i -> p (g i)"), in_=WF[0:1, 1, 512:1024])
        for kk in range(8):
            g = kk // 2
            mm_group(bank(kk % 2)[:, (kk // 2) * 128:(kk // 2) * 128 + 128],
                     [(WB[:, 1, kk * 128:(kk + 1) * 128], WTM[:, g, :]),
                      (ONESB[0:1, :], BSH[0:1, g, :]), (ONESB[0:1, :], BSL[0:1, g, :])],
                     [("wb", 1), ("wtm",), ("onesb",), ("bsh",), ("bsl",)], [bk(kk % 2)])
        for par in range(2):
            dve("tensor_copy", [bk(par)], [("rtab",)],
                out=RTAB[:].rearrange("p (a b) i -> p a b i", b=2)[:, :, par, :],
                in_=bank(par)[:].rearrange("p (a i) -> p a i", a=4))
        if t == 0:
            dma("sp", "par", W00[:], w00_d[l].partition_broadcast(16), [], [("w00",)])
            for g in range(4):
                dve("tensor_scalar", [("w00",), ("idf",)], [("dg",)], out=DG[:, g, :], in0=IDF[0:16, 0:16],
                    scalar1=W00[:, g:g + 1], scalar2=None, op0=ALU.mult)
            dve("tensor_copy", [("bsh",)], [("bs0h",)], out=BS0H[:], in_=BSH[:, :, 0:1].to_broadcast([1, 4, 16]))
            dve("tensor_copy", [("bsl",)], [("bs0l",)], out=BS0L[:], in_=BSL[:, :, 0:1].to_broadcast([1, 4, 16]))
            for kk in range(8):
                g = kk // 2
                mm_group(bank(2)[:, kk * 16:(kk + 1) * 16],
                         [(WB[0:16, 1, kk * 128:(kk + 1) * 128], DG[:, g, :]),
                          (ONESB[0:1, :], BS0H[0:1, g, :]), (ONESB[0:1, :], BS0L[0:1, g, :])],
                         [("wb", 1), ("dg",), ("onesb",), ("bs0h",), ("bs0l",)], [bk(2)])
            dve("tensor_copy", [bk(2)], [("rtabs",)], out=RTABS[:],
                in_=bank(2)[:, 0:128].rearrange("p (a i) -> p a i", a=8))

    def phase_G(t, l, slots_u, get_slots_v, after_u=None):
        tgs = tokgroups(t)
        pb = 0
        for ui in range(2):
            for (t0, n, cs) in tgs:
                for cc in range(4):
                    b = pb % 2
                    pb += 1
                    kk = ui * 4 + cc
                    mm_group(bank(b)[:, 0:n],
                             [(RING[:, slots_u[ui], k, cc * 128:(cc + 1) * 128], XNT[:, k, t0:t0 + n]) for k in range(8)],
                             [("ring", slots_u[ui])] + [("xnT", c) for c in cs], [bk(b)])
                    act(AT[:, kk, t0:t0 + n], bank(b)[:, 0:n], AF.Gelu_apprx_tanh, [bk(b)], [("aT", kk, c) for c in cs])
        slots_v = get_slots_v()
        if after_u is not None:
            after_u()
        def part1(c):
            n = ntok(c)
            vg = c % 2
            sb_ = (c % 2) * 8
            for hf in range(2):
                mm_group(bank(2 + hf)[0:n, :],
                         [(XNT[:, k, tok0(c):tok0(c) + n], RING[:, slots_v[hf], k, :]) for k in range(8)],
                         [("ring", slots_v[hf]), ("xnT", c)], [bk(2 + hf)])
                act(WF[0:n, vg, hf * 512:(hf + 1) * 512], bank(2 + hf)[0:n, :], AF.Gelu_apprx_tanh, [bk(2 + hf), ("st1",)],
                    [("wf", vg, hf), ("st1", sb_ + hf)], accum_out=ST1[0:n, sb_ + hf:sb_ + hf + 1])
            dve("tensor_scalar", [("st1", sb_), ("st1", sb_ + 1), ("st1",)], [("st1", sb_ + 2)], out=ST1[0:n, sb_ + 2:sb_ + 3],
                in0=ST1[0:n, sb_:sb_ + 1], scalar1=ST1[0:n, sb_ + 1:sb_ + 2], scalar2=-1.0 / D, op0=ALU.add, op1=ALU.mult)
            act(WB[0:n, 0, :], WF[0:n, vg, :], AF.Square, [("wf", vg, 0), ("wf", vg, 1), ("st1", sb_ + 2)],
                [("wb", 0), ("st1", sb_ + 3)], bias=ST1[0:n, sb_ + 2:sb_ + 3], accum_out=ST1[0:n, sb_ + 3:sb_ + 4])
            dve("tensor_scalar", [("st1", sb_ + 3)], [("st1", sb_ + 4)], out=ST1[0:n, sb_ + 4:sb_ + 5], in0=ST1[0:n, sb_ + 3:sb_ + 4],
                scalar1=1.0 / D, scalar2=EPS, op0=ALU.mult, op1=ALU.add)
            pool_pow(ST1[0:n, sb_ + 4:sb_ + 5], [("st1", sb_ + 4)])

        def part2(c):
            n = ntok(c)
            vg = c % 2
            sb_ = (c % 2) * 8
            vh = 1
            stc = [("st1", sb_ + 2), ("st1", sb_ + 4)]
            dve("tensor_scalar", [("wf", vg, 0), ("wf", vg, 1)] + stc, [("wb", vh)],
                out=WB[0:n, vh, :], in0=WF[0:n, vg, :], scalar1=ST1[0:n, sb_ + 2:sb_ + 3], scalar2=ST1[0:n, sb_ + 4:sb_ + 5],
                op0=ALU.add, op1=ALU.mult)
            if c == nch:
                dve("tensor_scalar", [("wf", vg, 0), ("wf", vg, 1)] + stc, [("wf", vg, 0), ("wf", vg, 1)],
                    out=WF[0:n, vg, :], in0=WF[0:n, vg, :], scalar1=ST1[0:n, sb_ + 2:sb_ + 3], scalar2=ST1[0:n, sb_ + 4:sb_ + 5],
                    op0=ALU.add, op1=ALU.mult)
                sc = 1 - vg
                dma("sp", "par", WF[0:n, sc, :], lng_d[l].partition_broadcast(n), [], [("wf", sc, 0), ("wf", sc, 1)])
                dve("tensor_tensor", [("wf", vg, 0), ("wf", vg, 1), ("wf", sc, 0), ("wf", sc, 1)],
                    [("wf", vg, 0), ("wf", vg, 1)], out=WF[0:n, vg, :], in0=WF[0:n, vg, :], in1=WF[0:n, sc, :], op=ALU.mult)
                dma("sp", "par", WF[0:n, sc, :], lnb_d[l].partition_broadcast(n), [], [("wf", sc, 0), ("wf", sc, 1)])
                dve("tensor_tensor", [("wf", vg, 0), ("wf", vg, 1), ("wf", sc, 0), ("wf", sc, 1)],
                    [("wf", vg, 0), ("wf", vg, 1)], out=WF[0:n, vg, :], in0=WF[0:n, vg, :], in1=WF[0:n, sc, :], op=ALU.add)
                dma("sp", "io", gmv[l], WF[0:n, vg, :], [("wf", vg, 0), ("wf", vg, 1)], [("gmv", l)])
            for kk in range(8):
                g = kk // 2
                rhs = WTM[:, g, :] if c < nch else DG[:, g, :]
                rcell = ("wtm",) if c < nch else ("dg",)
                mm_group(bank(4 + kk // 4)[:, (kk % 4) * 128:(kk % 4) * 128 + n],
                         [(WB[0:n, vh, kk * 128:(kk + 1) * 128], rhs)], [("wb", vh), rcell], [bk(4 + kk // 4)])
            tm = vg
            tmv = WF[:, tm, :].rearrange("p (a i) -> p a i", a=8)[:, :, 0:n]
            dve("tensor_tensor", [bk(4), bk(5), ("gcols",)], [("wf", tm, 0), ("wf", tm, 1)], out=tmv,
                in0=P45[:].rearrange("p b (a i) -> p (b a) i", a=4)[:, :, 0:n],
                in1=GCOLS[:, l, 2, :].unsqueeze(2).to_broadcast([128, 8, n]), op=ALU.mult)
            rt = RTAB[:] if c < nch else RTABS[:]
            P.op("pool", lambda e, tmv=tmv, rt=rt: e.tensor_tensor(out=tmv, in0=tmv, in1=rt, op=ALU.add),
                 reads=[("wf", tm, 0), ("wf", tm, 1), ("rtab",), ("rtabs",)], writes=[("wf", tm, 0), ("wf", tm, 1)])
            P.op("pool", lambda e, tmv=tmv, c=c, n=n: e.tensor_tensor(out=AT[:, :, tok0(c):tok0(c) + n], in0=tmv,
                                                                   in1=AT[:, :, tok0(c):tok0(c) + n], op=ALU.mult),
                 reads=[("wf", tm, 0), ("wf", tm, 1)] + [("aT", kk, c) for kk in range(8)],
                 writes=[("aT", kk, c) for kk in range(8)])

        cl = chunk_list(t)
        part1(cl[0])
        for i in range(1, len(cl)):
            part1(cl[i])
            part2(cl[i - 1])
        part2(cl[-1])

    def phase_P(t, l, src, src_cells, nk, gate_off, dst, dst_cells, groups):
        tgs = tokgroups(t)
        pb = 0
        for ch in range(2):
            wp_slots, wg_slot = groups[ch]()
            for (t0, n, cs) in tgs:
                for cc in range(4):
                    b = pb % 2
                    pb += 1
                    col = ch * 4 + cc
                    pairs = []
                    for k in range(nk):
                        pairs.append((RING[:, wp_slots[k // 8], k % 8, cc * 128:(cc + 1) * 128], src[:, k, t0:t0 + n]))
                    mm_group(bank(b)[:, 0:n], pairs,
                             [("ring", s) for s in wp_slots] + [src_cells(k, c) for k in range(nk) for c in cs], [bk(b)])
                    mm_group(bank(2 + b)[:, 0:n],
                             [(RING[:, wg_slot, k, cc * 128:(cc + 1) * 128], XNT[:, k, t0:t0 + n]) for k in range(8)],
                             [("ring", wg_slot)] + [("xnT", c) for c in cs], [bk(2 + b)])
                    sg = rot("wf")
                    act(WF[:, sg, 0:n], bank(2 + b)[:, 0:n], AF.Sigmoid, [bk(2 + b), ("bgc",)], [("wf", sg, 0)],
                        bias=BGC[:, l, gate_off + col:gate_off + col + 1])
                    dve("tensor_tensor", [("wf", sg, 0), bk(b)], [dst_cells(col, c) for c in cs],
                        out=dst[:, col, t0:t0 + n], in0=bank(b)[:, 0:n], in1=WF[:, sg, 0:n], op=ALU.mult)
        so = groups[2]()
        for c in chunk_list(t):
            n = ntok(c)
            for oh in range(2):
                b = 4 + oh
                mm_group(bank(b)[0:n, :],
                         [(dst[:, k, tok0(c):tok0(c) + n], RING[:, so[oh], k, :]) for k in range(8)],
                         [("ring", so[oh])] + [dst_cells(k, c) for k in range(8)], [bk(b)])
                dve("tensor_tensor", [bk(b), ("x", c)], [("x", c)], out=X[0:n, c, oh * 512:(oh + 1) * 512],
                    in0=bank(b)[0:n, :], in1=X[0:n, c, oh * 512:(oh + 1) * 512], op=ALU.add)

    def gn_elem(n, src, src_cells, sgi):
        jb = rot("wb")
        act(WB[0:n, jb, 0:512], src, AF.Square, src_cells + [("st1",)], [("wb", jb), ("st1", 5)],
            accum_out=ST1[0:n, 5:6])
        dve("tensor_scalar", [("st1", 5)], [("st1", 6)], out=ST1[0:n, 6:7], in0=ST1[0:n, 5:6],
            scalar1=1.0 / DV, scalar2=EPS, op0=ALU.mult, op1=ALU.add)
        pool_pow(ST1[0:n, 6:7], [("st1", 6)])
        rb = rot("rr")
        dve("scalar_tensor_tensor", src_cells + [("st1", 6), ("sg", sgi)], [("rr", rb)], out=RR[0:n, rb, :], in0=src,
            scalar=ST1[0:n, 6:7], in1=SG[0:n, sgi, :], op0=ALU.mult, op1=ALU.mult)
        return rb

    def gn_transpose(n, h, c, rb):
        transposes(None, [(PT[0][:, 4 + e, 0:n], RR[0:n, rb, e * 128:(e + 1) * 128]) for e in range(4)],
                   IDB[0:n, 0:n], [("rr", rb), ("idb",)], [("pt", 0)])
        act(BIG[:, h * 4:(h + 1) * 4, tok0(c):tok0(c) + n], PT[0][:, 4:8, 0:n], AF.Identity, [("pt", 0)],
            [("big", h * 4 + e, c) for e in range(4)])

    GB = {"b": 1}

    def proj3(c, n, which, slot):
        bi = {"qk": 0, "v": 1, "g": GB["b"]}[which]
        mm_group(bank(bi)[0:n, :], [(XNT[:, k, tok0(c):tok0(c) + n], RING[:, slot, k, :]) for k in range(8)],
                 [("ring", slot), ("xnT", c)], [bk(bi)])

    def phase_R(t, l, h, slots):
        sqk, sv, sgs = slots
        gam = float(GAM[h])
        have_state = (t > 0)
        GB["b"] = 1 if t == 0 else 2
        wcur = None
        if have_state:
            dma("sp", "si", SBUF_S[:, 0, :, :], nrp[l, h].rearrange("(a p) e -> p a e", p=128), [("nrp", l, h)], [("ss", 0)])
            w0 = rot("wc")
            act(WC[:, w0, :, :], SBUF_S[:, 0, :, :], AF.Identity, [("ss", 0)], [("wc", w0)])
            wcur = w0
        gen = None
        if t == 0:
            gen = sample_part(l, h, slots, gam)
            next(gen)

        def bg(k=1):
            nonlocal gen
            if os.environ.get('KBG', '1') == '0':
                return
            for _ in range(k):
                if gen is None:
                    return
                try:
                    next(gen)
                except StopIteration:
                    gen = None

        prev = None
        pend_tr = None
        first = True
        csbuf = {}

        def load_cs(cc):
            bb = rot("cs")
            csbuf[cc] = bb
            dma("sp", "par", CSB[:, bb, :, :], cs_d[t, :, cc, :, :], [], [("cs", bb)])
        load_cs(0)
        for s in range(nch + 1):
            c = s if s < nch else None
            n = 128
            if c is not None and c + 1 < nch:
                load_cs(c + 1)
            cur = None
            if c is not None:
                proj3(c, n, "qk", sqk)
                ra = rot("wf")
                rb_ = rot("wf")
                for qk in range(2):
                    src = bank(0)[0:n, qk * 256:(qk + 1) * 256].rearrange("p (a f) -> p a f", a=2)
                    for (ti, wfi) in ((0, ra), (1, rb_)):
                        dve("scalar_tensor_tensor", [bk(0), ("dqk",), ("cs", csbuf[c])], [("wf", wfi, 0)],
                            out=WF[0:n, wfi, qk * 256:(qk + 1) * 256].rearrange("p (a f) -> p a f", a=2),
                            in0=src, scalar=DQK[0:n, c, qk, h:h + 1],
                            in1=CSB[0:n, csbuf[c], ti, :].unsqueeze(1).to_broadcast([n, 2, 128]), op0=ALU.mult, op1=ALU.mult)
                qpn = rot("qkp")
                A4 = WF[0:n, ra, 0:512].rearrange("p (q a f) -> p q a f", q=2, a=2)
                B4 = WF[0:n, rb_, 0:512].rearrange("p (q a f) -> p q a f", q=2, a=2)
                O4 = QKP[0:n, qpn, :].rearrange("p (q a f) -> p q a f", q=2, a=2)
                rc = [("wf", ra, 0), ("wf", rb_, 0)]
                dve("tensor_tensor", rc, [("qkp", qpn, 0)], out=O4[:, :, 0, :], in0=A4[:, :, 0, :], in1=B4[:, :, 1, :], op=ALU.subtract)
                dve("tensor_tensor", rc, [("qkp", qpn, 1)], out=O4[:, :, 1, :], in0=B4[:, :, 0, :], in1=A4[:, :, 1, :], op=ALU.add)
                cur = {"c": c, "qp": qpn}
            bg()
            if prev is not None:
                qp, vb = prev["qp"], prev["vb"]
                transposes(None, [(PT[0][:, j, 0:n], QKP[0:n, qp, j * 128:(j + 1) * 128]) for j in range(4)],
                           IDB[0:n, 0:n], [("qkp", qp, 0), ("qkp", qp, 1), ("idb",)], [("pt", 0)])
                qt = rot("qkt")
                act(QKT[:, qt, :, 0:n], PT[0][:, 0:4, 0:n], AF.Identity, [("pt", 0)], [("qkt", qt)])
                bg()
            if c is not None:
                proj3(c, n, "v", sv)
                vbn = rot("vb")
                act(VB[0:n, vbn, :], bank(1)[0:n, :], AF.Identity, [bk(1)], [("vb", vbn)])
                cur["vb"] = vbn
            bg()
            if pend_tr is not None:
                gn_transpose(n, h, pend_tr[0], pend_tr[1])
                pend_tr = None
                bg()
            if prev is not None:
                mm_group(bank(1)[0:n, 0:n], [(QKT[:, qt, 2 + hf, 0:n], QKT[:, qt, hf, 0:n]) for hf in range(2)],
                         [("qkt", qt)], [bk(1)])
                sc = rot("sct")
                dve("tensor_tensor", [bk(1), ("mask",)], [("sct", sc)], out=SCT[0:n, sc, 0:n], in0=bank(1)[0:n, 0:n],
                    in1=MASK[0:n, 0:n], op=ALU.mult)
                bg()
            if c is not None:
                proj3(c, n, "g", sgs)
                sgn = rot("sg")
                act(SG[0:n, sgn, :], bank(GB["b"])[0:n, :], AF.Silu, [bk(GB["b"])], [("sg", sgn)])
                cur["sg"] = sgn
            bg()
            if prev is not None:
                pairs = [(SCT[0:n, sc, 0:n], VB[0:n, vb, :])]
                rds = [("sct", sc), ("vb", vb)]
                if have_state or not first:
                    pairs += [(QKT[:, qt, hf, 0:n], WC[:, wcur, hf, :]) for hf in range(2)]
                    rds += [("qkt", qt), ("wc", wcur)]
                mm_group(bank(3)[0:n, :], pairs, rds, [bk(3)])
                for hf in range(2):
                    def fn(e, hf=hf, qp=qp, vb=vb, first=first):
                        return e.matmul(bank(4 + hf)[:, :], lhsT=QKP[0:128, qp, 256 + hf * 128:256 + (hf + 1) * 128],
                                        rhs=VB[0:128, vb, :], start=first, stop=True, skip_group_check=True)
                    P.op("pe", fn, reads=[("qkp", qp, 0), ("qkp", qp, 1), ("vb", vb)], writes=[bk(4 + hf)])
                first = False
                if prev["c"] < nch - 1:
                    wn = rot("wc")
                    if have_state:
                        dve("tensor_tensor", [bk(4), bk(5), ("ss", 0)], [("wc", wn)], out=WC[:, wn, :, :], in0=P45[:],
                            in1=SBUF_S[:, 0, :, :], op=ALU.add)
                    else:
                        act(WC[:, wn, :, :], P45[:], AF.Identity, [bk(4), bk(5)], [("wc", wn)])
                    wcur = wn
            bg()
            if prev is not None:
                rbuf = gn_elem(n, bank(3)[0:n, :], [bk(3)], prev["sg"])
                pend_tr = (prev["c"], rbuf)
            bg()
            prev = cur
        if pend_tr is not None:
            gn_transpose(128, h, pend_tr[0], pend_tr[1])
        sn = rot("wf")
        sview = WF[:, sn, :].rearrange("p (a e) -> p a e", a=2)
        if have_state:
            dve("tensor_tensor", [bk(4), bk(5), ("ss", 0)], [("wf", sn, 0), ("wf", sn, 1)], out=sview, in0=P45[:],
                in1=SBUF_S[:, 0, :, :], op=ALU.add)
            act(sview, sview, AF.Identity, [("wf", sn, 0), ("wf", sn, 1)], [("wf", sn, 0), ("wf", sn, 1)], scale=float(GEND[h]))
        else:
            act(sview, P45[:], AF.Identity, [bk(4), bk(5)], [("wf", sn, 0), ("wf", sn, 1)], scale=float(GEND[h]))
        dma("sp", "so", nrp[l, h].rearrange("(a p) e -> p a e", p=128), sview, [("wf", sn, 0), ("wf", sn, 1)], [("nrp", l, h)])
        if gen is not None:
            for _ in gen:
                pass

    def sample_part(l, h, slots, gam):
        sqk, sv, sgs = slots
        c = nch
        n = ns
        proj3(c, n, "qk", sqk)
        proj3(c, n, "v", sv)
        act(VBS[0:n, :], bank(1)[0:n, :], AF.Identity, [bk(1)], [("vbs",)])
        proj3(c, n, "g", sgs)
        act(SG[0:n, 2, :], bank(1)[0:n, :], AF.Silu, [bk(1)], [("sg", 2)])
        ra = rot("wf")
        rb_ = rot("wf")
        for qk in range(2):
            src = bank(0)[0:n, qk * 256:(qk + 1) * 256].rearrange("p (a f) -> p a f", a=2)
            for (ti, wfi) in ((0, ra), (1, rb_)):
                dve("scalar_tensor_tensor", [bk(0), ("css",)], [("wf", wfi, 0)],
                    out=WF[0:n, wfi, qk * 256:(qk + 1) * 256].rearrange("p (a f) -> p a f", a=2),
                    in0=src, scalar=(1.0 if qk == 0 else DK ** -0.5),
                    in1=CSS[0:n, ti, :].unsqueeze(1).to_broadcast([n, 2, 128]), op0=ALU.mult, op1=ALU.mult)
        A4 = WF[0:n, ra, 0:512].rearrange("p (q a f) -> p q a f", q=2, a=2)
        B4 = WF[0:n, rb_, 0:512].rearrange("p (q a f) -> p q a f", q=2, a=2)
        rc = [("wf", ra, 0), ("wf", rb_, 0)]
        QS2 = QS[:].rearrange("p (a f) -> p a f", a=2)
        KS2 = KS[:].rearrange("p (a f) -> p a f", a=2)
        dve("tensor_tensor", rc, [("qs", 0)], out=QS2[:, 0, :], in0=A4[:, 0, 0, :], in1=B4[:, 0, 1, :], op=ALU.subtract)
        dve("tensor_tensor", rc, [("qs", 1)], out=QS2[:, 1, :], in0=B4[:, 0, 0, :], in1=A4[:, 0, 1, :], op=ALU.add)
        dve("tensor_tensor", rc, [("ks", 0)], out=KS2[:, 0, :], in0=A4[:, 1, 0, :], in1=B4[:, 1, 1, :], op=ALU.subtract)
        dve("tensor_tensor", rc, [("ks", 1)], out=KS2[:, 1, :], in0=B4[:, 1, 0, :], in1=A4[:, 1, 1, :], op=ALU.add)
        transposes(None, [(bank(1)[:, hf * 16:hf * 16 + n], QS[0:n, hf * 128:(hf + 1) * 128]) for hf in range(2)],
                   IDF[0:n, 0:n], [("qs", 0), ("qs", 1), ("idf",)], [bk(1)])
        for b in range(n):
            dve("tensor_copy", [bk(1), ("qm",)], [("qm", b)], out=QM[:, b, :, b:b + 1],
                in_=bank(1)[:, 0:32].rearrange("p (a j) -> p a j", a=2)[:, :, b:b + 1])
        qkt_ = rot("wf")
        dve("tensor_tensor", [("qs", 0), ("qs", 1), ("ks", 0), ("ks", 1)], [("wf", qkt_, 0)], out=WF[0:n, qkt_, 0:256],
            in0=QS[0:n, :], in1=KS[0:n, :], op=ALU.mult)
        dve("reduce_sum", [("wf", qkt_, 0), ("st1",)], [("st1", 14)], out=ST1[0:n, 14:15], in_=WF[0:n, qkt_, 0:256],
            axis=mybir.AxisListType.X)
        dma("sp", "si", SBUF_S[:, 0, :, :], st[l, 0, h].rearrange("(a p) e -> p a e", p=128), [], [("ss", 0)])
        yield
        for b in range(n):
            km = rot("km")
            dve("tensor_scalar", [("ks", 0), ("ks", 1), ("idf",)], [("km", km)], out=KM[0:n, km, :], in0=KS[0:n, :],
                scalar1=IDF[0:n, b:b + 1], scalar2=None, op0=ALU.mult)
            ss = b % 2
            if b + 1 < n:
                dma("sp", "si", SBUF_S[:, 1 - ss, :, :], st[l, b + 1, h].rearrange("(a p) e -> p a e", p=128), [],
                    [("ss", 1 - ss)])
            for hf in range(2):
                def fn(e, b=b, hf=hf, ss=ss):
                    return e.matmul(PB_[2][0:ns, :], lhsT=QM[:, b, hf, :], rhs=SBUF_S[:, ss, hf, :],
                                    start=(b == 0 and hf == 0), stop=(b == ns - 1 and hf == 1), skip_group_check=True)
                P.op("pe", fn, reads=[("qm", b), ("ss", ss)], writes=[bk(2)])
            yield
            for hf in range(2):
                mm_group(PX[:, :], [(KM[0:n, km, hf * 128:(hf + 1) * 128], VBS[0:n, :])],
                         [("km", km), ("vbs",)], [bk(6)])
                dve("scalar_tensor_tensor", [("ss", ss), bk(6)], [("ss", ss)], out=SBUF_S[:, ss, hf, :],
                    in0=SBUF_S[:, ss, hf, :], scalar=gam, in1=PX[:, :], op0=ALU.mult, op1=ALU.add)
                if hf == 1:
                    dma("sp", "so", nrs[l, b, h].rearrange("(a p) e -> p a e", p=128), SBUF_S[:, ss, :, :], [("ss", ss)],
                        [("nrs", l, b, h)])
                yield
        tq = rot("wf")
        dve("tensor_scalar", [("vbs",), ("st1", 14)], [("wf", tq, 0)], out=WF[0:n, tq, 0:512], in0=VBS[0:n, :],
            scalar1=ST1[0:n, 14:15], scalar2=None, op0=ALU.mult)
        dve("scalar_tensor_tensor", [bk(2), ("wf", tq, 0)], [("osa",)], out=OSA[:], in0=PB_[2][0:ns, :], scalar=gam,
            in1=WF[0:n, tq, 0:512], op0=ALU.mult, op1=ALU.add)
        rbuf = gn_elem(n, OSA[:], [("osa",)], 2)
        gn_transpose(n, h, c, rbuf)

    def phase_F(t, l, groups):
        tgs = tokgroups(t)
        pb = 0
        for g in range(4):
            su = groups[2 * g]()
            buf = g % 2
            for (t0, n, cs) in tgs:
                for ui in range(2):
                    for cc in range(4):
                        b = pb % 2
                        pb += 1
                        kk = ui * 4 + cc
                        mm_group(bank(b)[:, 0:n],
                                 [(RING[:, su[ui], k, cc * 128:(cc + 1) * 128], XNT[:, k, t0:t0 + n]) for k in range(8)],
                                 [("ring", su[ui])] + [("xnT", c) for c in cs], [bk(b)])
                        rl = rot("wf")
                        act(WF[:, rl, 0:n], bank(b)[:, 0:n], AF.Relu, [bk(b)], [("wf", rl, 0)])
                        dve("tensor_tensor", [("wf", rl, 0)], [("big", buf * 8 + kk, c) for c in cs],
                            out=BIG[:, buf * 8 + kk, t0:t0 + n], in0=WF[:, rl, 0:n], in1=WF[:, rl, 0:n], op=ALU.mult)
            sd = groups[2 * g + 1]()
            for c in chunk_list(t):
                n = ntok(c)
                for oh in range(2):
                    b = 2 + (pb % 2)
                    pb += 1
                    mm_group(bank(b)[0:n, :],
                             [(BIG[:, buf * 8 + k, tok0(c):tok0(c) + n], RING[:, sd[oh], k, :]) for k in range(8)],
                             [("ring", sd[oh])] + [("big", buf * 8 + k, c) for k in range(8)], [bk(b)])
                    dve("tensor_tensor", [bk(b), ("x", c)], [("x", c)], out=X[0:n, c, oh * 512:(oh + 1) * 512],
                        in0=bank(b)[0:n, :], in1=X[0:n, c, oh * 512:(oh + 1) * 512], op=ALU.add)

    def weight_groups(l):
        G = []
        G.append([win_unit(l, 0), win_unit(l, 512)])
        G.append([win_unit(l, 1024), win_unit(l, 1536)])
        for ch in range(2):
            G.append([[(w_pgm[l, :, ch * 512:(ch + 1) * 512], 0, 512)], win_unit(l, 8192 + ch * 512)])
        G.append([[(w_out[l, :, 0:512], 0, 512)], [(w_out[l, :, 512:1024], 0, 512)]])
        for h in range(H):
            G.append([[(w_in[l, :, 2048 + h * 256:2048 + (h + 1) * 256], 0, 256),
                       (w_in[l, :, 3072 + h * 256:3072 + (h + 1) * 256], 256, 256)],
                      win_unit(l, 4096 + h * 512), win_unit(l, 6144 + h * 512)])
        for ch in range(2):
            G.append([[(w_pret[l, 0:1024, ch * 512:(ch + 1) * 512], 0, 512)],
                      [(w_pret[l, 1024:2048, ch * 512:(ch + 1) * 512], 0, 512)], win_unit(l, 9216 + ch * 512)])
        G.append([[(w_out[l, :, 0:512], 0, 512)], [(w_out[l, :, 512:1024], 0, 512)]])
        for g in range(4):
            G.append([[(w_up[l, :, g * 1024:g * 1024 + 512], 0, 512)], [(w_up[l, :, g * 1024 + 512:(g + 1) * 1024], 0, 512)]])
            G.append([[(w_down[l, g * 1024:(g + 1) * 1024, 0:512], 0, 512)], [(w_down[l, g * 1024:(g + 1) * 1024, 512:1024], 0, 512)]])
        return G

    allgroups = []
    for t in range(ntiles):
        for l in range(depth):
            allgroups += weight_groups(l)
    gstate = {"next_load": 0, "loaded": {}, "next_use": 0}

    def issue_group_load():
        gi = gstate["next_load"]
        if gi >= len(allgroups):
            return
        gstate["next_load"] += 1
        base = (gi % 2) * 3
        slots = []
        for ui, parts in enumerate(allgroups[gi]):
            slot = base + ui
            for (src, coff, ncols) in parts:
                dma("pool", "w", RING[:, slot, :, coff:coff + ncols], src.rearrange("(k p) n -> p k n", p=128),
                    reads=[], writes=[("ring", slot)])
            slots.append(slot)
        gstate["loaded"][gi] = slots

    def use_group():
        gi = gstate["next_use"]
        gstate["next_use"] += 1
        while gstate["next_load"] <= gi + 1 and gstate["next_load"] < len(allgroups):
            issue_group_load()
        return gstate["loaded"][gi]

    for t in range(ntiles):
        dma("sp", "io", X[:, 0:nch, :], xp[t * nch * 128:(t + 1) * nch * 128, :].rearrange("(c p) d -> p c d", p=128),
            [], [("x", c) for c in range(nch)])
        if t == 0:
            dma("sp", "io", X[0:ns, nch, :], xs, [], [("x", nch)])
        for l in range(depth):
            phase_norm(t, l, 0)
            su = use_group()
            phase_G(t, l, su, use_group, after_u=lambda: load_layer_params(t, l))

            def pa_group():
                s = use_group()
                return [s[0]], s[1]
            phase_P(t, l, AT, lambda k, c: ("aT", k, c), 8, 0, BIG, lambda k, c: ("big", k, c),
                    [pa_group, pa_group, use_group])
            for h in range(H):
                phase_R(t, l, h, use_group())

            def pb_group():
                s = use_group()
                return [s[0], s[1]], s[2]
            phase_P(t, l, BIG, lambda k, c: ("big", k, c), 16, 8, AT, lambda k, c: ("aT", k, c),
                    [pb_group, pb_group, use_group])
            phase_norm(t, l, 1)
            phase_F(t, l, [use_group] * 8)
        cl = chunk_list(t)
        for c in cl:
            n = ntok(c)
            jb = rot("wb")
            act(WB[0:n, jb, :], X[0:n, c, :], AF.Square, [("x", c), ("ssq",)], [("wb", jb), ("ssq", c)],
                accum_out=SSQ[0:n, c:c + 1])
        ncl = len(cl)
        dve("tensor_scalar", [("ssq", c) for c in cl] + [("ssq",)], [("rs",)], out=RS[:, 0:ncl], in0=SSQ[:, 0:ncl],
            scalar1=1.0 / D, scalar2=EPS, op0=ALU.mult, op1=ALU.add)
        pool_pow(RS[:, 0:ncl], [("rs",)])
        gf = rot("wf")
        dma("sp", "par", WF[:, gf, :], gfin_d.partition_broadcast(128), [], [("wf", gf, 0), ("wf", gf, 1)])
        for c in cl:
            n = ntok(c)
            dve("scalar_tensor_tensor", [("x", c), ("rs",), ("wf", gf, 0), ("wf", gf, 1)], [("x", c)], out=X[0:n, c, :],
                in0=X[0:n, c, :], scalar=RS[0:n, c:c + 1], in1=WF[0:n, gf, :], op0=ALU.mult, op1=ALU.mult)
        dma("sp", "io", yp[t * nch * 128:(t + 1) * nch * 128, :].rearrange("(c p) d -> p c d", p=128), X[:, 0:nch, :],
            [("x", c) for c in range(nch)], [("yp", t)])
        if t == 0:
            dma("sp", "io", ys, X[0:ns, nch, :], [("x", nch)], [("ys",)])

    outs = [("yp", t) for t in range(ntiles)] + [("ys",)] + [("gmv", l) for l in range(depth)]
    outs += [("nrp", l, h) for l in range(depth) for h in range(H)]
    outs += [("nrs", l, b, h) for l in range(depth) for b in range(ns) for h in range(H)]
    P.op("sp", lambda e: None, reads=outs, writes=[])

    block = es.enter_context(nc.Block())
    P.emit(nc, {"sp": block.sync, "act": block.scalar, "dve": block.vector, "pool": block.gpsimd, "pe": block.tensor},
           sem_alloc, dma_classes)
    es.close()
    P.sbuf_free = SBUF_FREE
    return nc, P


def _prep_common(cfg, inputs):
    depth = cfg.depth
    tabs, _, _ = _host_tables(cfg)
    f = lambda a: np.ascontiguousarray(np.asarray(a, dtype=np.float32))
    g3 = np.stack([f(inputs["norm_mix_g"])[:depth], f(inputs["norm_ffn_g"])[:depth], f(inputs["gm_ln_g"])[:depth]], axis=1)
    gcols = np.ascontiguousarray(g3.reshape(depth, 3, 8, 128).transpose(3, 0, 1, 2))
    bgc = np.ascontiguousarray(f(inputs["b_gate"])[:depth].reshape(depth, 16, 128).transpose(2, 0, 1))
    wt = np.ascontiguousarray(f(inputs["gm_w_s"])[:depth].transpose(0, 3, 1, 2))
    w00 = np.ascontiguousarray(f(inputs["gm_w_s"])[:depth, :, 0, 0])
    common = dict(
        w_in=f(inputs["w_in"])[:depth], w_pgm=f(inputs["w_proj_gm"])[:depth], w_pret=f(inputs["w_proj_ret"])[:depth],
        w_out=f(inputs["w_out"])[:depth], w_up=f(inputs["w_up"])[:depth], w_down=f(inputs["w_down"])[:depth],
        gcols=gcols, gfin=f(inputs["norm_final_g"]), bgcols=bgc, lng=f(inputs["gm_ln_g"])[:depth],
        lnb=f(inputs["gm_ln_b"])[:depth], wt=wt, w00=w00, bs=f(inputs["gm_b_s"])[:depth].reshape(depth, 512),
        cs=tabs["cs"], cs_s=tabs["cs_s"], dqk=tabs["dqk"], mask=tabs["mask"], ident=tabs["ident"],
    )
    return common


_CACHE = {}


def run(cfg, inputs, ncores, trace=False):
    key = (cfg.depth, cfg.nch, cfg.ntiles, cfg.ns)
    if key not in _CACHE:
        _CACHE[key] = build_nc(cfg)
    nc, _ = _CACHE[key]
    common = _prep_common(cfg, inputs)
    xp = np.asarray(inputs["x_prompt"], dtype=np.float32)
    xs = np.asarray(inputs["x_sample"], dtype=np.float32)
    st = np.asarray(inputs["state_ret"], dtype=np.float32)
    ns = cfg.ns
    in_maps = []
    for b in range(ncores):
        m = dict(common)
        m["xp"] = np.ascontiguousarray(xp[b])
        m["xs"] = np.ascontiguousarray(xs[b * ns:(b + 1) * ns, 0, :])
        m["st"] = np.ascontiguousarray(st[:cfg.depth, b * ns:(b + 1) * ns])
        in_maps.append(m)
    res = run_bass_kernel_spmd(nc, in_maps, core_ids=list(range(ncores)), trace=trace)
    rs = res.results
    y_prompt = np.stack([rs[b]["yp"] for b in range(ncores)], axis=0)
    y_sample = np.concatenate([rs[b]["ys"] for b in range(ncores)], axis=0)[:, None, :]
    nrp = np.stack([rs[b]["nrp"] for b in range(ncores)], axis=1)
    nrs = np.concatenate([rs[b]["nrs"] for b in range(ncores)], axis=1)
    gmv = np.concatenate([rs[b]["gmv"] for b in range(ncores)], axis=1)[:, :, None, :]
    out = (y_prompt.astype(np.float32), y_sample.astype(np.float32), nrp.astype(np.float32), nrs.astype(np.float32),
           gmv.astype(np.float32))
    return out, res


def kernel(**inputs):
    cfg = Cfg(depth=4, nch=8, ntiles=2, ns=16)
    out, _ = run(cfg, inputs, 8)
    return out
```

```python
import os
import numpy as np
from contextlib import ExitStack
import concourse.bass as bass
import concourse.mybir as mybir
from concourse.bass_utils import run_bass_kernel_spmd

F32 = mybir.dt.float32
BF16 = mybir.dt.bfloat16
AF = mybir.ActivationFunctionType
ALU = mybir.AluOpType

D = 1024
H = 4
DK = 256
DV = 512
DFF = 4096
INW = 10240
EPS = 1e-6
NSLOT = 6
PAST_LEN = 16384


class Cfg:
    def __init__(self, depth=4, nch=8, ntiles=2, ns=16):
        self.depth, self.nch, self.ntiles, self.ns = depth, nch, ntiles, ns
        self.seq = nch * ntiles * 128
        self.ntok = nch * 128 + ns


class Prog:
    COMPUTE = ("pe", "act", "dve", "pool")

    def __init__(self):
        self.ops = []
        self.cells = {}

    def op(self, eng, fn, reads=(), writes=(), dma=None):
        oid = len(self.ops)
        deps = set()
        for k in reads:
            c = self.cells.get(k)
            if c is not None and c[0] is not None:
                deps.add(c[0])
        for k in writes:
            c = self.cells.get(k)
            if c is not None:
                if c[0] is not None:
                    deps.add(c[0])
                deps.update(c[1].values())
                deps.update(c[2])
        for k in reads:
            c = self.cells.get(k)
            if c is None:
                c = self.cells[k] = [None, {}, []]
            if dma is None:
                c[1][eng] = oid
            else:
                c[2].append(oid)
        for k in writes:
            self.cells[k] = [oid, {}, []]
        deps.discard(oid)
        self.ops.append(dict(eng=eng, fn=fn, deps=deps, dma=dma))
        return oid

    def emit(self, nc, block_engines, sem_alloc, dma_classes):
        ops = self.ops
        needed = set()
        for o in ops:
            needed.update(o["deps"])
        ROLL = 30000
        cnt = {e: 0 for e in self.COMPUTE}
        epoch = {e: 0 for e in self.COMPUTE}
        esems = {e: [sem_alloc("c_%s_0" % e)] for e in self.COMPUTE}
        dcnt = {}
        for i, o in enumerate(ops):
            o["pre"] = None
            if o["dma"] is not None:
                cls = o["dma"]
                sems = dma_classes[cls]
                n = dcnt.get(cls, 0)
                dcnt[cls] = n + 1
                s = sems[n % len(sems)]
                k = n // len(sems)
                o["pre"] = (s, 16 * k) if k > 0 else None
                o["ev"] = (s, 16 * (k + 1))
                o["inc"] = (s, 16)
            elif o["eng"] in self.COMPUTE and i in needed:
                e = o["eng"]
                if cnt[e] >= ROLL:
                    epoch[e] += 1
                    cnt[e] = 0
                    esems[e].append(sem_alloc("c_%s_%d" % (e, epoch[e])))
                cnt[e] += 1
                s = esems[e][epoch[e]]
                o["ev"] = (s, cnt[e])
                o["inc"] = (s, 1)
            else:
                o["ev"] = None
                o["inc"] = None
        per = {}
        for i, o in enumerate(ops):
            per.setdefault(o["eng"], []).append(i)
        self.nwaits = 0

        def run(engname, e):
            seen = {}
            for i in per.get(engname, []):
                o = ops[i]
                want = {}
                for d in o["deps"]:
                    od = ops[d]
                    if od["dma"] is None and od["eng"] == "pe" and engname == "pe":
                        continue
                    ev = od["ev"]
                    if ev is None:
                        continue
                    s, v = ev
                    if want.get(id(s), (None, -1))[1] < v:
                        want[id(s)] = (s, v)
                if o["pre"] is not None:
                    s, v = o["pre"]
                    if want.get(id(s), (None, -1))[1] < v:
                        want[id(s)] = (s, v)
                for sid, (s, v) in want.items():
                    if seen.get(sid, -1) >= v:
                        continue
                    seen[sid] = v
                    e.wait_ge(s, v)
                    self.nwaits += 1
                ins = o["fn"](e)
                if o["inc"] is not None:
                    assert ins is not None
                    ins.then_inc(o["inc"][0], o["inc"][1])

        for engname, deco in block_engines.items():
            deco((lambda en: (lambda e: run(en, e)))(engname))


def _host_tables(cfg):
    nch, ntiles = cfg.nch, cfg.ntiles
    inv_freq = (1.0 / (np.float32(10000.0) ** np.linspace(0.0, 1.0, DK // 2, dtype=np.float32))).astype(np.float32)
    pos = np.arange(cfg.seq, dtype=np.int32).astype(np.float32)
    ang = (pos[:, None] * inv_freq[None, :]).astype(np.float32)
    cos = np.cos(ang).astype(np.float32).reshape(ntiles, nch, 128, 128).transpose(0, 2, 1, 3)
    sin = np.sin(ang).astype(np.float32).reshape(ntiles, nch, 128, 128).transpose(0, 2, 1, 3)
    angs = (np.float32(PAST_LEN) * inv_freq).astype(np.float32)
    cs_s = np.stack([np.cos(angs), np.sin(angs)]).astype(np.float32)
    cs_s = np.broadcast_to(cs_s[None], (16, 2, 128)).copy()
    g = 1.0 - 2.0 ** (-5.0 - np.arange(H, dtype=np.float64))
    p = (np.arange(nch)[None, :, None] * 128 + np.arange(128)[:, None, None] + 1).astype(np.float64)
    dq = (g[None, None, :] ** p).astype(np.float32)
    dk = ((g[None, None, :] ** (-p)) * (DK ** -0.5)).astype(np.float32)
    dqk = np.stack([dq, dk], axis=2).copy()
    j = np.arange(128)[:, None]
    i = np.arange(128)[None, :]
    mask = (i >= j).astype(np.float32)
    ident = np.eye(128, dtype=np.float32)
    gam = g
    gend = g ** (128 * nch)
    cs = np.ascontiguousarray(np.stack([cos, sin], axis=3))
    return dict(cs=cs, cs_s=cs_s, dqk=dqk, mask=mask,
                ident=ident), gam, gend


def build_nc(cfg):
    depth, nch, ntiles, ns = cfg.depth, cfg.nch, cfg.ntiles, cfg.ns
    NT = cfg.ntok
    _, GAM, GEND = _host_tables(cfg)
    nc = bass.Bass("TRN2", target_bir_lowering=False)

    def din(name, shape):
        return nc.dram_tensor(name, list(shape), F32, kind="ExternalInput").ap()

    def dout(name, shape):
        return nc.dram_tensor(name, list(shape), F32, kind="ExternalOutput").ap()

    xp = din("xp", [cfg.seq, D])
    xs = din("xs", [ns, D])
    st = din("st", [depth, ns, H, DK, DV])
    w_in = din("w_in", [depth, D, INW])
    w_pgm = din("w_pgm", [depth, D, D])
    w_pret = din("w_pret", [depth, 2 * D, D])
    w_out = din("w_out", [depth, D, D])
    w_up = din("w_up", [depth, D, DFF])
    w_down = din("w_down", [depth, DFF, D])
    gcols_d = din("gcols", [128, depth, 3, 8])
    gfin_d = din("gfin", [D])
    bg_d = din("bgcols", [128, depth, 16])
    lng_d = din("lng", [depth, D])
    lnb_d = din("lnb", [depth, D])
    wt_d = din("wt", [depth, 128, 4, 128])
    w00_d = din("w00", [depth, 4])
    bs_d = din("bs", [depth, 4 * 128])
    cs_d = din("cs", [ntiles, 128, nch, 2, 128])
    css_d = din("cs_s", [16, 2, 128])
    dqk_d = din("dqk", [128, nch, 2, H])
    mask_d = din("mask", [128, 128])
    ident_d = din("ident", [128, 128])

    yp = dout("yp", [cfg.seq, D])
    ys = dout("ys", [ns, D])
    nrp = dout("nrp", [depth, H, DK, DV])
    nrs = dout("nrs", [depth, ns, H, DK, DV])
    gmv = dout("gmv", [depth, ns, D])

    P = Prog()
    es = ExitStack()

    def sb(name, shape, dt):
        return es.enter_context(nc.sbuf_tensor(name, list(shape), dt))

    def ps(name, shape, dt):
        return es.enter_context(nc.psum_tensor(name, list(shape), dt))

    X = sb("X", [128, nch + 1, D], F32)
    XNT = sb("XNT", [128, 8, NT], BF16)
    AT = sb("AT", [128, 8, NT], BF16)
    BIG = sb("BIG", [128, 16, NT], BF16)
    RING = sb("RING", [128, NSLOT, 8, 512], BF16)
    CSB = sb("CSB", [128, 2, 2, 128], F32)
    CSS = sb("CSS", [16, 2, 128], F32)
    DQK = sb("DQK", [128, nch, 2, H], F32)
    MASK = sb("MASK", [128, 128], F32)
    IDF = sb("IDF", [128, 128], F32)
    IDB = sb("IDB", [128, 128], BF16)
    ONESB = sb("ONESB", [1, 128], BF16)
    MHALF = sb("MHALF", [128, 16], F32)
    GCOLS = sb("GCOLS", [128, depth, 3, 8], F32)
    BGC = sb("BGC", [128, depth, 16], F32)
    WC = sb("WC", [128, 2, 2, 512], BF16)
    SBUF_S = sb("SBUF_S", [128, 2, 2, 512], F32)
    WF = sb("WF", [128, 2, 1024], F32)
    WB = sb("WB", [128, 2, 1024], BF16)
    QKP = sb("QKP", [128, 2, 512], BF16)
    VB = sb("VB", [128, 2, 512], BF16)
    SG = sb("SG", [128, 3, 512], BF16)
    VBS = sb("VBS", [16, 512], BF16)
    OSA = sb("OSA", [16, 512], F32)
    QKT = sb("QKT", [128, 2, 4, 128], BF16)
    SCT = sb("SCT", [128, 2, 128], BF16)
    RR = sb("RR", [128, 2, 512], BF16)
    RTAB = sb("RTAB", [128, 8, 128], F32)
    RTABS = sb("RTABS", [128, 8, 16], F32)
    WTM = sb("WTM", [128, 4, 128], BF16)
    DG = sb("DG", [16, 4, 16], BF16)
    W00 = sb("W00", [16, 4], F32)
    BSH = sb("BSH", [1, 4, 128], BF16)
    BSL = sb("BSL", [1, 4, 128], BF16)
    BS0H = sb("BS0H", [1, 4, 16], BF16)
    BS0L = sb("BS0L", [1, 4, 16], BF16)
    SSQ = sb("SSQ", [128, 16], F32)
    RS = sb("RS", [128, 16], F32)
    ST1 = sb("ST1", [128, 16], F32)
    QM = sb("QM", [128, 16, 2, 16], F32)
    QS = sb("QS", [16, 256], F32)
    KS = sb("KS", [16, 256], BF16)
    KM = sb("KM", [16, 2, 256], BF16)

    SBUF_FREE = nc.sbuf_bytes_remaining
    PB_ = [ps("P%d" % i, [128, 512], F32) for i in range(4)]
    P45 = ps("P45", [128, 2, 512], F32)
    PT = [ps("PT0", [128, 8, 128], BF16)]
    PX = ps("PX", [128, 512], F32)

    def bank(i):
        return PB_[i] if i < 4 else P45[:, i - 4, :]

    def bk(i):
        return ("ps", i)

    sems = []

    def sem_alloc(name):
        s = es.enter_context(nc.semaphore(name))
        sems.append(s)
        return s

    dma_classes = {
        "w": [sem_alloc("dw%d" % i) for i in range(NSLOT)],
        "io": [sem_alloc("dio%d" % i) for i in range(4)],
        "par": [sem_alloc("dpar%d" % i) for i in range(6)],
        "ppar": [sem_alloc("dppar%d" % i) for i in range(2)],
        "si": [sem_alloc("dsi%d" % i) for i in range(2)],
        "so": [sem_alloc("dso%d" % i) for i in range(2)],
    }

    def chunk_list(t):
        cl = list(range(nch))
        if t == 0:
            cl.append(nch)
        return cl

    def ntok(c):
        return 128 if c < nch else ns

    def tok0(c):
        return c * 128

    def tokgroups(t):
        tgs = []
        c = 0
        while c < nch:
            n = min(4, nch - c)
            tgs.append((c * 128, n * 128, list(range(c, c + n))))
            c += n
        if t == 0:
            tgs.append((nch * 128, ns, [nch]))
        return tgs

    def dma(eng, cls, out, in_, reads, writes):
        P.op(eng, lambda e: e.dma_start(out=out, in_=in_), reads=reads, writes=writes, dma=cls)

    wstate = {"n": 0}

    def wload(parts):
        slot = wstate["n"] % NSLOT
        wstate["n"] += 1
        for (src, coff, ncols) in parts:
            dma("pool", "w", RING[:, slot, :, coff:coff + ncols], src.rearrange("(k p) n -> p k n", p=128),
                reads=[], writes=[("ring", slot)])
        return slot

    def win_unit(l, c0, n=512):
        return [(w_in[l, :, c0:c0 + n], 0, n)]

    def mm_group(out_ap, pairs, reads, writes, first=True, last=True):
        def fn(e):
            ins = None
            n = len(pairs)
            for i, (a, b) in enumerate(pairs):
                ins = e.matmul(out_ap, lhsT=a, rhs=b, start=(first and i == 0), stop=(last and i == n - 1))
            return ins
        P.op("pe", fn, reads=reads, writes=writes)

    def transposes(out_t, in_list, ident, reads, writes):
        def fn(e):
            ins = None
            for (o, i_) in in_list:
                ins = e.transpose(o, i_, ident)
            return ins
        P.op("pe", fn, reads=reads, writes=writes)

    def act(out, in_, func, reads, writes, **kw):
        P.op("act", lambda e: e.activation(out=out, in_=in_, func=func, **kw), reads=reads, writes=writes)

    def dve(fname, reads, writes, **kw):
        P.op("dve", lambda e: getattr(e, fname)(**kw), reads=reads, writes=writes)

    def pool_pow(ap, cells):
        n = ap.shape[-1]
        P.op("pool", lambda e: e.tensor_tensor(out=ap, in0=ap, in1=MHALF[0:ap.shape[0], 0:n], op=ALU.pow),
             reads=cells + [("mhalf",)], writes=cells)

    cnt = {"wf": 0, "wb": 0, "qkp": 0, "vb": 0, "qkt": 0, "sct": 0, "rr": 0, "pt": 0, "sg": 0, "cs": 0, "wc": 0, "ss": 0, "km": 0}

    def rot(name, n=2):
        v = cnt[name] % n
        cnt[name] += 1
        return v

    dma("sp", "par", CSS[:], css_d, [], [("css",)])
    dma("sp", "par", DQK[:], dqk_d, [], [("dqk",)])
    dma("sp", "par", MASK[:], mask_d, [], [("mask",)])
    dma("sp", "par", IDF[:], ident_d, [], [("idf",)])
    dma("sp", "par", GCOLS[:], gcols_d, [], [("gcols",)])
    dma("sp", "par", BGC[:], bg_d, [], [("bgc",)])
    dve("tensor_copy", [("idf",)], [("idb",)], out=IDB[:], in_=IDF[:])
    dve("memset", [], [("onesb",)], ap=ONESB[:], constant=1.0)
    dve("memset", [], [("mhalf",)], ap=MHALF[:], constant=-0.5)
    dve("memset", [], [("ssq",)], ap=SSQ[:], constant=1.0)
    dve("memset", [], [("st1",)], ap=ST1[:], constant=1.0)
    dve("memset", [], [("qm",)], ap=QM[:], constant=0.0)

    def phase_norm(t, l, gi):
        cl = chunk_list(t)
        for c in cl:
            n = ntok(c)
            jb = rot("wb")
            act(WB[0:n, jb, :], X[0:n, c, :], AF.Square, [("x", c), ("ssq",)], [("wb", jb), ("ssq", c)],
                accum_out=SSQ[0:n, c:c + 1])
        ncl = len(cl)
        dve("tensor_scalar", [("ssq", c) for c in cl] + [("ssq",)], [("rs",)], out=RS[:, 0:ncl], in0=SSQ[:, 0:ncl],
            scalar1=1.0 / D, scalar2=EPS, op0=ALU.mult, op1=ALU.add)
        pool_pow(RS[:, 0:ncl], [("rs",)])
        xbs = {}

        def do_xs(c):
            n = ntok(c)
            xb = rot("wb")
            xbs[c] = xb
            dve("tensor_scalar", [("x", c), ("rs",)], [("wb", xb)], out=WB[0:n, xb, :], in0=X[0:n, c, :],
                scalar1=RS[0:n, c:c + 1], scalar2=None, op0=ALU.mult)
        do_xs(cl[0])
        for i, c in enumerate(cl):
            n = ntok(c)
            if i + 1 < len(cl):
                do_xs(cl[i + 1])
            xb = xbs[c]
            transposes(None, [(PT[0][:, k, 0:n], WB[0:n, xb, k * 128:(k + 1) * 128]) for k in range(8)],
                       IDB[0:n, 0:n], [("wb", xb), ("idb",)], [("pt", 0)])
            dve("tensor_tensor", [("pt", 0), ("gcols",)], [("xnT", c)],
                out=XNT[:, :, tok0(c):tok0(c) + n], in0=PT[0][:, :, 0:n],
                in1=GCOLS[:, l, gi, :].unsqueeze(2).to_broadcast([128, 8, n]), op=ALU.mult)

    def load_layer_params(t, l):
        dma("pool", "ppar", WB[:, 1, :], lnb_d[l].partition_broadcast(128), [], [("wb", 1)])
        dma("sp", "par", WF[:, 0, 0:512].rearrange("p (g i) -> p g i", g=4), wt_d[l], [], [("wf", 0, 0)])
        dve("tensor_tensor", [("wf", 0, 0), ("mask",)], [("wtm",)], out=WTM[:],
            in0=WF[:, 0, 0:512].rearrange("p (g i) -> p g i", g=4),
            in1=MASK[:].unsqueeze(1).to_broadcast([128, 4, 128]), op=ALU.mult)
        dma("sp", "par", WF[0:1, 1, 0:512], bs_d[l].unsqueeze(0), [], [("wf", 1, 0)])
        dve("tensor_copy", [("wf", 1, 0)], [("bsh",)], out=BSH[:].rearrange("p g i -> p (g i)"), in_=WF[0:1, 1, 0:512])
        dve("tensor_tensor", [("wf", 1, 0), ("bsh",)], [("wf", 1, 1)], out=WF[0:1, 1, 512:1024], in0=WF[0:1, 1, 0:512],
            in1=BSH[:].rearrange("p g i -> p (g i)"), op=ALU.subtract)
        dve("tensor_copy", [("wf", 1, 1)], [("bsl",)], out=BSL[:].rearrange("p g i -> p (g i)"), in_=WF[0:1, 1, 512:1024])

    def layer_params_compute(t, l):
        for kk in range(8):
            g = kk // 2
            mm_group(bank(kk % 2)[:, (kk // 2) * 128:(kk // 2) * 128 + 128],
                     [(WB[:, 1, kk * 128:(kk + 1) * 128], WTM[:, g, :]),
                      (ONESB[0:1, :], BSH[0:1, g, :]), (ONESB[0:1, :], BSL[0:1, g, :])],
                     [("wb", 1), ("wtm",), ("onesb",), ("bsh",), ("bsl",)], [bk(kk % 2)])
        for par in range(2):
            dve("tensor_copy", [bk(par)], [("rtab",)],
                out=RTAB[:].rearrange("p (a b) i -> p a b i", b=2)[:, :, par, :],
                in_=bank(par)[:].rearrange("p (a i) -> p a i", a=4))
        if t == 0:
            dma("sp", "par", W00[:], w00_d[l].partition_broadcast(16), [], [("w00",)])
            for g in range(4):
                dve("tensor_scalar", [("w00",), ("idf",)], [("dg",)], out=DG[:, g, :], in0=IDF[0:16, 0:16],
                    scalar1=W00[:, g:g + 1], scalar2=None, op0=ALU.mult)
            dve("tensor_copy", [("bsh",)], [("bs0h",)], out=BS0H[:], in_=BSH[:, :, 0:1].to_broadcast([1, 4, 16]))
            dve("tensor_copy", [("bsl",)], [("bs0l",)], out=BS0L[:], in_=BSL[:, :, 0:1].to_broadcast([1, 4, 16]))
            for kk in range(8):
                g = kk // 2
                mm_group(bank(2)[:, kk * 16:(kk + 1) * 16],
                         [(WB[0:16, 1, kk * 128:(kk + 1) * 128], DG[:, g, :]),
                          (ONESB[0:1, :], BS0H[0:1, g, :]), (ONESB[0:1, :], BS0L[0:1, g, :])],
                         [("wb", 1), ("dg",), ("onesb",), ("bs0h",), ("bs0l",)], [bk(2)])
            dve("tensor_copy", [bk(2)], [("rtabs",)], out=RTABS[:],
                in_=bank(2)[:, 0:128].rearrange("p (a i) -> p a i", a=8))

    def phase_G(t, l, slots_u, get_slots_v, after_u=None):
        tgs = tokgroups(t)
        pb = 0
        for ui in range(2):
            for (t0, n, cs) in tgs:
                for cc in range(4):
                    b = pb % 2
                    pb += 1
                    kk = ui * 4 + cc
                    mm_group(bank(b)[:, 0:n],
                             [(RING[:, slots_u[ui], k, cc * 128:(cc + 1) * 128], XNT[:, k, t0:t0 + n]) for k in range(8)],
                             [("ring", slots_u[ui])] + [("xnT", c) for c in cs], [bk(b)])
                    act(AT[:, kk, t0:t0 + n], bank(b)[:, 0:n], AF.Gelu_apprx_tanh, [bk(b)], [("aT", kk, c) for c in cs])
        slots_v = get_slots_v()
        if after_u is not None:
            after_u()
        def part1(c):
            n = ntok(c)
            vg = c % 2
            sb_ = (c % 2) * 8
            for hf in range(2):
                mm_group(bank(2 + hf)[0:n, :],
                         [(XNT[:, k, tok0(c):tok0(c) + n], RING[:, slots_v[hf], k, :]) for k in range(8)],
                         [("ring", slots_v[hf]), ("xnT", c)], [bk(2 + hf)])
                act(WF[0:n, vg, hf * 512:(hf + 1) * 512], bank(2 + hf)[0:n, :], AF.Gelu_apprx_tanh, [bk(2 + hf), ("st1",)],
                    [("wf", vg, hf), ("st1", sb_ + hf)], accum_out=ST1[0:n, sb_ + hf:sb_ + hf + 1])
            act(WB[0:n, 0, :], WF[0:n, vg, :], AF.Square, [("wf", vg, 0), ("wf", vg, 1), ("st1",)],
                [("wb", 0), ("st1", sb_ + 3)], accum_out=ST1[0:n, sb_ + 3:sb_ + 4])
            dve("tensor_scalar", [("st1", sb_), ("st1", sb_ + 1), ("st1",)], [("st1", sb_ + 2)], out=ST1[0:n, sb_ + 2:sb_ + 3],
                in0=ST1[0:n, sb_:sb_ + 1], scalar1=ST1[0:n, sb_ + 1:sb_ + 2], scalar2=-1.0 / D, op0=ALU.add, op1=ALU.mult)
            dve("tensor_scalar", [("st1", sb_ + 2)], [("st1", sb_ + 7)], out=ST1[0:n, sb_ + 7:sb_ + 8],
                in0=ST1[0:n, sb_ + 2:sb_ + 3], scalar1=ST1[0:n, sb_ + 2:sb_ + 3], scalar2=EPS, op0=ALU.mult, op1=ALU.subtract)
            dve("scalar_tensor_tensor", [("st1", sb_ + 3), ("st1", sb_ + 7)], [("st1", sb_ + 4)], out=ST1[0:n, sb_ + 4:sb_ + 5],
                in0=ST1[0:n, sb_ + 3:sb_ + 4], scalar=1.0 / D, in1=ST1[0:n, sb_ + 7:sb_ + 8], op0=ALU.mult, op1=ALU.subtract)
            pool_pow(ST1[0:n, sb_ + 4:sb_ + 5], [("st1", sb_ + 4)])

        def part2(c):
            n = ntok(c)
            vg = c % 2
            sb_ = (c % 2) * 8
            vh = 1
            stc = [("st1", sb_ + 2), ("st1", sb_ + 4)]
            dve("tensor_scalar", [("wf", vg, 0), ("wf", vg, 1)] + stc, [("wb", vh)],
                out=WB[0:n, vh, :], in0=WF[0:n, vg, :], scalar1=ST1[0:n, sb_ + 2:sb_ + 3], scalar2=ST1[0:n, sb_ + 4:sb_ + 5],
                op0=ALU.add, op1=ALU.mult)
            if c == nch:
                dve("tensor_scalar", [("wf", vg, 0), ("wf", vg, 1)] + stc, [("wf", vg, 0), ("wf", vg, 1)],
                    out=WF[0:n, vg, :], in0=WF[0:n, vg, :], scalar1=ST1[0:n, sb_ + 2:sb_ + 3], scalar2=ST1[0:n, sb_ + 4:sb_ + 5],
                    op0=ALU.add, op1=ALU.mult)
                sc = 1 - vg
                dma("sp", "par", WF[0:n, sc, :], lng_d[l].partition_broadcast(n), [], [("wf", sc, 0), ("wf", sc, 1)])
                dve("tensor_tensor", [("wf", vg, 0), ("wf", vg, 1), ("wf", sc, 0), ("wf", sc, 1)],
                    [("wf", vg, 0), ("wf", vg, 1)], out=WF[0:n, vg, :], in0=WF[0:n, vg, :], in1=WF[0:n, sc, :], op=ALU.mult)
                dma("sp", "par", WF[0:n, sc, :], lnb_d[l].partition_broadcast(n), [], [("wf", sc, 0), ("wf", sc, 1)])
                dve("tensor_tensor", [("wf", vg, 0), ("wf", vg, 1), ("wf", sc, 0), ("wf", sc, 1)],
                    [("wf", vg, 0), ("wf", vg, 1)], out=WF[0:n, vg, :], in0=WF[0:n, vg, :], in1=WF[0:n, sc, :], op=ALU.add)
                dma("sp", "io", gmv[l], WF[0:n, vg, :], [("wf", vg, 0), ("wf", vg, 1)], [("gmv", l)])
            for kk in range(8):
                g = kk // 2
                rhs = WTM[:, g, :] if c < nch else DG[:, g, :]
                rcell = ("wtm",) if c < nch else ("dg",)
                mm_group(bank(4 + kk // 4)[:, (kk % 4) * 128:(kk % 4) * 128 + n],
                         [(WB[0:n, vh, kk * 128:(kk + 1) * 128], rhs)], [("wb", vh), rcell], [bk(4 + kk // 4)])
            tm = vg
            tmv = WF[:, tm, :].rearrange("p (a i) -> p a i", a=8)[:, :, 0:n]
            dve("tensor_tensor", [bk(4), bk(5), ("gcols",)], [("wf", tm, 0), ("wf", tm, 1)], out=tmv,
                in0=P45[:].rearrange("p b (a i) -> p (b a) i", a=4)[:, :, 0:n],
                in1=GCOLS[:, l, 2, :].unsqueeze(2).to_broadcast([128, 8, n]), op=ALU.mult)
            rt = RTAB[:] if c < nch else RTABS[:]
            P.op("pool", lambda e, tmv=tmv, rt=rt: e.tensor_tensor(out=tmv, in0=tmv, in1=rt, op=ALU.add),
                 reads=[("wf", tm, 0), ("wf", tm, 1), ("rtab",), ("rtabs",)], writes=[("wf", tm, 0), ("wf", tm, 1)])
            P.op("pool", lambda e, tmv=tmv, c=c, n=n: e.tensor_tensor(out=AT[:, :, tok0(c):tok0(c) + n], in0=tmv,
                                                                   in1=AT[:, :, tok0(c):tok0(c) + n], op=ALU.mult),
                 reads=[("wf", tm, 0), ("wf", tm, 1)] + [("aT", kk, c) for kk in range(8)],
                 writes=[("aT", kk, c) for kk in range(8)])

        cl = chunk_list(t)
        part1(cl[0])
        for i in range(1, len(cl)):
            part1(cl[i])
            part2(cl[i - 1])
        part2(cl[-1])

    def phase_P(t, l, src, src_cells, nk, gate_off, dst, dst_cells, groups):
        tgs = tokgroups(t)
        pb = 0
        for ch in range(2):
            wp_slots, wg_slot = groups[ch]()
            for (t0, n, cs) in tgs:
                for cc in range(4):
                    b = pb % 2
                    pb += 1
                    col = ch * 4 + cc
                    pairs = []
                    for k in range(nk):
                        pairs.append((RING[:, wp_slots[k // 8], k % 8, cc * 128:(cc + 1) * 128], src[:, k, t0:t0 + n]))
                    mm_group(bank(b)[:, 0:n], pairs,
                             [("ring", s) for s in wp_slots] + [src_cells(k, c) for k in range(nk) for c in cs], [bk(b)])
                    mm_group(bank(2 + b)[:, 0:n],
                             [(RING[:, wg_slot, k, cc * 128:(cc + 1) * 128], XNT[:, k, t0:t0 + n]) for k in range(8)],
                             [("ring", wg_slot)] + [("xnT", c) for c in cs], [bk(2 + b)])
                    sg = rot("wf")
                    act(WF[:, sg, 0:n], bank(2 + b)[:, 0:n], AF.Sigmoid, [bk(2 + b), ("bgc",)], [("wf", sg, 0)],
                        bias=BGC[:, l, gate_off + col:gate_off + col + 1])
                    dve("tensor_tensor", [("wf", sg, 0), bk(b)], [dst_cells(col, c) for c in cs],
                        out=dst[:, col, t0:t0 + n], in0=bank(b)[:, 0:n], in1=WF[:, sg, 0:n], op=ALU.mult)
        so = groups[2]()
        for c in chunk_list(t):
            n = ntok(c)
            for oh in range(2):
                b = 4 + oh
                mm_group(bank(b)[0:n, :],
                         [(dst[:, k, tok0(c):tok0(c) + n], RING[:, so[oh], k, :]) for k in range(8)],
                         [("ring", so[oh])] + [dst_cells(k, c) for k in range(8)], [bk(b)])
                dve("tensor_tensor", [bk(b), ("x", c)], [("x", c)], out=X[0:n, c, oh * 512:(oh + 1) * 512],
                    in0=bank(b)[0:n, :], in1=X[0:n, c, oh * 512:(oh + 1) * 512], op=ALU.add)

    def gn_elem(n, src, src_cells, sgi):
        jb = rot("wb")
        act(WB[0:n, jb, 0:512], src, AF.Square, src_cells + [("st1",)], [("wb", jb), ("st1", 5)],
            accum_out=ST1[0:n, 5:6])
        dve("tensor_scalar", [("st1", 5)], [("st1", 6)], out=ST1[0:n, 6:7], in0=ST1[0:n, 5:6],
            scalar1=1.0 / DV, scalar2=EPS, op0=ALU.mult, op1=ALU.add)
        pool_pow(ST1[0:n, 6:7], [("st1", 6)])
        rb = rot("rr")
        dve("scalar_tensor_tensor", src_cells + [("st1", 6), ("sg", sgi)], [("rr", rb)], out=RR[0:n, rb, :], in0=src,
            scalar=ST1[0:n, 6:7], in1=SG[0:n, sgi, :], op0=ALU.mult, op1=ALU.mult)
        return rb

    def gn_transpose(n, h, c, rb):
        transposes(None, [(PT[0][:, 4 + e, 0:n], RR[0:n, rb, e * 128:(e + 1) * 128]) for e in range(4)],
                   IDB[0:n, 0:n], [("rr", rb), ("idb",)], [("pt", 0)])
        act(BIG[:, h * 4:(h + 1) * 4, tok0(c):tok0(c) + n], PT[0][:, 4:8, 0:n], AF.Identity, [("pt", 0)],
            [("big", h * 4 + e, c) for e in range(4)])

    GB = {"b": 1}

    def proj3(c, n, which, slot):
        bi = {"qk": 0, "v": 1, "g": GB["b"]}[which]
        mm_group(bank(bi)[0:n, :], [(XNT[:, k, tok0(c):tok0(c) + n], RING[:, slot, k, :]) for k in range(8)],
                 [("ring", slot), ("xnT", c)], [bk(bi)])

    def phase_R(t, l, h, slots):
        sqk, sv, sgs = slots
        gam = float(GAM[h])
        have_state = (t > 0)
        GB["b"] = 1 if t == 0 else 2
        wcur = None
        if have_state:
            dma("sp", "si", SBUF_S[:, 0, :, :], nrp[l, h].rearrange("(a p) e -> p a e", p=128), [("nrp", l, h)], [("ss", 0)])
            w0 = rot("wc")
            act(WC[:, w0, :, :], SBUF_S[:, 0, :, :], AF.Identity, [("ss", 0)], [("wc", w0)])
            wcur = w0
        gen = None
        if t == 0:
            gen = sample_part(l, h, slots, gam)
            next(gen)

        def bg(k=1):
            nonlocal gen
            if os.environ.get('KBG', '1') == '0':
                return
            for _ in range(k):
                if gen is None:
                    return
                try:
                    next(gen)
                except StopIteration:
                    gen = None

        prev = None
        pend_tr = None
        first = True
        csbuf = {}

        def load_cs(cc):
            bb = rot("cs")
            csbuf[cc] = bb
            dma("sp", "par", CSB[:, bb, :, :], cs_d[t, :, cc, :, :], [], [("cs", bb)])
        load_cs(0)
        for s in range(nch + 1):
            c = s if s < nch else None
            n = 128
            if c is not None and c + 1 < nch:
                load_cs(c + 1)
            cur = None
            if c is not None:
                proj3(c, n, "qk", sqk)
                ra = rot("wf")
                rb_ = rot("wf")
                for qk in range(2):
                    src = bank(0)[0:n, qk * 256:(qk + 1) * 256].rearrange("p (a f) -> p a f", a=2)
                    for (ti, wfi) in ((0, ra), (1, rb_)):
                        dve("scalar_tensor_tensor", [bk(0), ("dqk",), ("cs", csbuf[c])], [("wf", wfi, 0)],
                            out=WF[0:n, wfi, qk * 256:(qk + 1) * 256].rearrange("p (a f) -> p a f", a=2),
                            in0=src, scalar=DQK[0:n, c, qk, h:h + 1],
                            in1=CSB[0:n, csbuf[c], ti, :].unsqueeze(1).to_broadcast([n, 2, 128]), op0=ALU.mult, op1=ALU.mult)
                qpn = rot("qkp")
                A4 = WF[0:n, ra, 0:512].rearrange("p (q a f) -> p q a f", q=2, a=2)
                B4 = WF[0:n, rb_, 0:512].rearrange("p (q a f) -> p q a f", q=2, a=2)
                O4 = QKP[0:n, qpn, :].rearrange("p (q a f) -> p q a f", q=2, a=2)
                rc = [("wf", ra, 0), ("wf", rb_, 0)]
                dve("tensor_tensor", rc, [("qkp", qpn, 0)], out=O4[:, :, 0, :], in0=A4[:, :, 0, :], in1=B4[:, :, 1, :], op=ALU.subtract)
                dve("tensor_tensor", rc, [("qkp", qpn, 1)], out=O4[:, :, 1, :], in0=B4[:, :, 0, :], in1=A4[:, :, 1, :], op=ALU.add)
                cur = {"c": c, "qp": qpn}
            bg()
            if prev is not None:
                qp, vb = prev["qp"], prev["vb"]
                transposes(None, [(PT[0][:, j, 0:n], QKP[0:n, qp, j * 128:(j + 1) * 128]) for j in range(4)],
                           IDB[0:n, 0:n], [("qkp", qp, 0), ("qkp", qp, 1), ("idb",)], [("pt", 0)])
                qt = rot("qkt")
                act(QKT[:, qt, :, 0:n], PT[0][:, 0:4, 0:n], AF.Identity, [("pt", 0)], [("qkt", qt)])
                bg()
            if c is not None:
                proj3(c, n, "v", sv)
                vbn = rot("vb")
                act(VB[0:n, vbn, :], bank(1)[0:n, :], AF.Identity, [bk(1)], [("vb", vbn)])
                cur["vb"] = vbn
            bg()
            if pend_tr is not None:
                gn_transpose(n, h, pend_tr[0], pend_tr[1])
                pend_tr = None
                bg()
            if prev is not None:
                mm_group(bank(1)[0:n, 0:n], [(QKT[:, qt, 2 + hf, 0:n], QKT[:, qt, hf, 0:n]) for hf in range(2)],
                         [("qkt", qt)], [bk(1)])
                sc = rot("sct")
                dve("tensor_tensor", [bk(1), ("mask",)], [("sct", sc)], out=SCT[0:n, sc, 0:n], in0=bank(1)[0:n, 0:n],
                    in1=MASK[0:n, 0:n], op=ALU.mult)
                bg()
            if c is not None:
                proj3(c, n, "g", sgs)
                sgn = rot("sg")
                act(SG[0:n, sgn, :], bank(GB["b"])[0:n, :], AF.Silu, [bk(GB["b"])], [("sg", sgn)])
                cur["sg"] = sgn
            bg()
            if prev is not None:
                pairs = [(SCT[0:n, sc, 0:n], VB[0:n, vb, :])]
                rds = [("sct", sc), ("vb", vb)]
                if have_state or not first:
                    pairs += [(QKT[:, qt, hf, 0:n], WC[:, wcur, hf, :]) for hf in range(2)]
                    rds += [("qkt", qt), ("wc", wcur)]
                mm_group(bank(3)[0:n, :], pairs, rds, [bk(3)])
                for hf in range(2):
                    def fn(e, hf=hf, qp=qp, vb=vb, first=first):
                        return e.matmul(bank(4 + hf)[:, :], lhsT=QKP[0:128, qp, 256 + hf * 128:256 + (hf + 1) * 128],
                                        rhs=VB[0:128, vb, :], start=first, stop=True, skip_group_check=True)
                    P.op("pe", fn, reads=[("qkp", qp, 0), ("qkp", qp, 1), ("vb", vb)], writes=[bk(4 + hf)])
                first = False
                if prev["c"] < nch - 1:
                    wn = rot("wc")
                    if have_state:
                        dve("tensor_tensor", [bk(4), bk(5), ("ss", 0)], [("wc", wn)], out=WC[:, wn, :, :], in0=P45[:],
                            in1=SBUF_S[:, 0, :, :], op=ALU.add)
                    else:
                        act(WC[:, wn, :, :], P45[:], AF.Identity, [bk(4), bk(5)], [("wc", wn)])
                    wcur = wn
            bg()
            if prev is not None:
                rbuf = gn_elem(n, bank(3)[0:n, :], [bk(3)], prev["sg"])
                pend_tr = (prev["c"], rbuf)
            bg()
            prev = cur
        if pend_tr is not None:
            gn_transpose(128, h, pend_tr[0], pend_tr[1])
        sn = rot("wf")
        sview = WF[:, sn, :].rearrange("p (a e) -> p a e", a=2)
        if have_state:
            dve("tensor_tensor", [bk(4), bk(5), ("ss", 0)], [("wf", sn, 0), ("wf", sn, 1)], out=sview, in0=P45[:],
                in1=SBUF_S[:, 0, :, :], op=ALU.add)
            act(sview, sview, AF.Identity, [("wf", sn, 0), ("wf", sn, 1)], [("wf", sn, 0), ("wf", sn, 1)], scale=float(GEND[h]))
        else:
            act(sview, P45[:], AF.Identity, [bk(4), bk(5)], [("wf", sn, 0), ("wf", sn, 1)], scale=float(GEND[h]))
        dma("sp", "so", nrp[l, h].rearrange("(a p) e -> p a e", p=128), sview, [("wf", sn, 0), ("wf", sn, 1)], [("nrp", l, h)])
        if gen is not None:
            for _ in gen:
                pass

    def sample_part(l, h, slots, gam):
        sqk, sv, sgs = slots
        c = nch
        n = ns
        proj3(c, n, "qk", sqk)
        proj3(c, n, "v", sv)
        act(VBS[0:n, :], bank(1)[0:n, :], AF.Identity, [bk(1)], [("vbs",)])
        proj3(c, n, "g", sgs)
        act(SG[0:n, 2, :], bank(1)[0:n, :], AF.Silu, [bk(1)], [("sg", 2)])
        ra = rot("wf")
        rb_ = rot("wf")
        for qk in range(2):
            src = bank(0)[0:n, qk * 256:(qk + 1) * 256].rearrange("p (a f) -> p a f", a=2)
            for (ti, wfi) in ((0, ra), (1, rb_)):
                dve("scalar_tensor_tensor", [bk(0), ("css",)], [("wf", wfi, 0)],
                    out=WF[0:n, wfi, qk * 256:(qk + 1) * 256].rearrange("p (a f) -> p a f", a=2),
                    in0=src, scalar=(1.0 if qk == 0 else DK ** -0.5),
                    in1=CSS[0:n, ti, :].unsqueeze(1).to_broadcast([n, 2, 128]), op0=ALU.mult, op1=ALU.mult)
        A4 = WF[0:n, ra, 0:512].rearrange("p (q a f) -> p q a f", q=2, a=2)
        B4 = WF[0:n, rb_, 0:512].rearrange("p (q a f) -> p q a f", q=2, a=2)
        rc = [("wf", ra, 0), ("wf", rb_, 0)]
        QS2 = QS[:].rearrange("p (a f) -> p a f", a=2)
        KS2 = KS[:].rearrange("p (a f) -> p a f", a=2)
        dve("tensor_tensor", rc, [("qs", 0)], out=QS2[:, 0, :], in0=A4[:, 0, 0, :], in1=B4[:, 0, 1, :], op=ALU.subtract)
        dve("tensor_tensor", rc, [("qs", 1)], out=QS2[:, 1, :], in0=B4[:, 0, 0, :], in1=A4[:, 0, 1, :], op=ALU.add)
        dve("tensor_tensor", rc, [("ks", 0)], out=KS2[:, 0, :], in0=A4[:, 1, 0, :], in1=B4[:, 1, 1, :], op=ALU.subtract)
        dve("tensor_tensor", rc, [("ks", 1)], out=KS2[:, 1, :], in0=B4[:, 1, 0, :], in1=A4[:, 1, 1, :], op=ALU.add)
        transposes(None, [(bank(1)[:, hf * 16:hf * 16 + n], QS[0:n, hf * 128:(hf + 1) * 128]) for hf in range(2)],
                   IDF[0:n, 0:n], [("qs", 0), ("qs", 1), ("idf",)], [bk(1)])
        for b in range(n):
            dve("tensor_copy", [bk(1), ("qm",)], [("qm", b)], out=QM[:, b, :, b:b + 1],
                in_=bank(1)[:, 0:32].rearrange("p (a j) -> p a j", a=2)[:, :, b:b + 1])
        qkt_ = rot("wf")
        dve("tensor_tensor", [("qs", 0), ("qs", 1), ("ks", 0), ("ks", 1)], [("wf", qkt_, 0)], out=WF[0:n, qkt_, 0:256],
            in0=QS[0:n, :], in1=KS[0:n, :], op=ALU.mult)
        dve("reduce_sum", [("wf", qkt_, 0), ("st1",)], [("st1", 14)], out=ST1[0:n, 14:15], in_=WF[0:n, qkt_, 0:256],
            axis=mybir.AxisListType.X)
        dma("sp", "si", SBUF_S[:, 0, :, :], st[l, 0, h].rearrange("(a p) e -> p a e", p=128), [], [("ss", 0)])
        yield
        for b in range(n):
            km = rot("km")
            dve("tensor_scalar", [("ks", 0), ("ks", 1), ("idf",)], [("km", km)], out=KM[0:n, km, :], in0=KS[0:n, :],
                scalar1=IDF[0:n, b:b + 1], scalar2=None, op0=ALU.mult)
            ss = b % 2
            if b + 1 < n:
                dma("sp", "si", SBUF_S[:, 1 - ss, :, :], st[l, b + 1, h].rearrange("(a p) e -> p a e", p=128), [],
                    [("ss", 1 - ss)])
            for hf in range(2):
                def fn(e, b=b, hf=hf, ss=ss):
                    return e.matmul(PB_[2][0:ns, :], lhsT=QM[:, b, hf, :], rhs=SBUF_S[:, ss, hf, :],
                                    start=(b == 0 and hf == 0), stop=(b == ns - 1 and hf == 1), skip_group_check=True)
                P.op("pe", fn, reads=[("qm", b), ("ss", ss)], writes=[bk(2)])
            yield
            for hf in range(2):
                mm_group(PX[:, :], [(KM[0:n, km, hf * 128:(hf + 1) * 128], VBS[0:n, :])],
                         [("km", km), ("vbs",)], [bk(6)])
                dve("scalar_tensor_tensor", [("ss", ss), bk(6)], [("ss", ss)], out=SBUF_S[:, ss, hf, :],
                    in0=SBUF_S[:, ss, hf, :], scalar=gam, in1=PX[:, :], op0=ALU.mult, op1=ALU.add)
                if hf == 1:
                    dma("sp", "so", nrs[l, b, h].rearrange("(a p) e -> p a e", p=128), SBUF_S[:, ss, :, :], [("ss", ss)],
                        [("nrs", l, b, h)])
                yield
        tq = rot("wf")
        dve("tensor_scalar", [("vbs",), ("st1", 14)], [("wf", tq, 0)], out=WF[0:n, tq, 0:512], in0=VBS[0:n, :],
            scalar1=ST1[0:n, 14:15], scalar2=None, op0=ALU.mult)
        dve("scalar_tensor_tensor", [bk(2), ("wf", tq, 0)], [("osa",)], out=OSA[:], in0=PB_[2][0:ns, :], scalar=gam,
            in1=WF[0:n, tq, 0:512], op0=ALU.mult, op1=ALU.add)
        rbuf = gn_elem(n, OSA[:], [("osa",)], 2)
        gn_transpose(n, h, c, rbuf)

    def phase_F(t, l, groups):
        tgs = tokgroups(t)
        pb = 0
        for g in range(4):
            su = groups[2 * g]()
            buf = g % 2
            for (t0, n, cs) in tgs:
                for ui in range(2):
                    for cc in range(4):
                        b = pb % 2
                        pb += 1
                        kk = ui * 4 + cc
                        mm_group(bank(b)[:, 0:n],
                                 [(RING[:, su[ui], k, cc * 128:(cc + 1) * 128], XNT[:, k, t0:t0 + n]) for k in range(8)],
                                 [("ring", su[ui])] + [("xnT", c) for c in cs], [bk(b)])
                        rl = rot("wf")
                        act(WF[:, rl, 0:n], bank(b)[:, 0:n], AF.Relu, [bk(b)], [("wf", rl, 0)])
                        dve("tensor_tensor", [("wf", rl, 0)], [("big", buf * 8 + kk, c) for c in cs],
                            out=BIG[:, buf * 8 + kk, t0:t0 + n], in0=WF[:, rl, 0:n], in1=WF[:, rl, 0:n], op=ALU.mult)
            sd = groups[2 * g + 1]()
            for c in chunk_list(t):
                n = ntok(c)
                for oh in range(2):
                    b = 2 + (pb % 2)
                    pb += 1
                    mm_group(bank(b)[0:n, :],
                             [(BIG[:, buf * 8 + k, tok0(c):tok0(c) + n], RING[:, sd[oh], k, :]) for k in range(8)],
                             [("ring", sd[oh])] + [("big", buf * 8 + k, c) for k in range(8)], [bk(b)])
                    dve("tensor_tensor", [bk(b), ("x", c)], [("x", c)], out=X[0:n, c, oh * 512:(oh + 1) * 512],
                        in0=bank(b)[0:n, :], in1=X[0:n, c, oh * 512:(oh + 1) * 512], op=ALU.add)

    def weight_groups(l):
        G = []
        G.append([win_unit(l, 0), win_unit(l, 512)])
        G.append([win_unit(l, 1024), win_unit(l, 1536)])
        for ch in range(2):
            G.append([[(w_pgm[l, :, ch * 512:(ch + 1) * 512], 0, 512)], win_unit(l, 8192 + ch * 512)])
        G.append([[(w_out[l, :, 0:512], 0, 512)], [(w_out[l, :, 512:1024], 0, 512)]])
        for h in range(H):
            G.append([[(w_in[l, :, 2048 + h * 256:2048 + (h + 1) * 256], 0, 256),
                       (w_in[l, :, 3072 + h * 256:3072 + (h + 1) * 256], 256, 256)],
                      win_unit(l, 4096 + h * 512), win_unit(l, 6144 + h * 512)])
        for ch in range(2):
            G.append([[(w_pret[l, 0:1024, ch * 512:(ch + 1) * 512], 0, 512)],
                      [(w_pret[l, 1024:2048, ch * 512:(ch + 1) * 512], 0, 512)], win_unit(l, 9216 + ch * 512)])
        G.append([[(w_out[l, :, 0:512], 0, 512)], [(w_out[l, :, 512:1024], 0, 512)]])
        for g in range(4):
            G.append([[(w_up[l, :, g * 1024:g * 1024 + 512], 0, 512)], [(w_up[l, :, g * 1024 + 512:(g + 1) * 1024], 0, 512)]])
            G.append([[(w_down[l, g * 1024:(g + 1) * 1024, 0:512], 0, 512)], [(w_down[l, g * 1024:(g + 1) * 1024, 512:1024], 0, 512)]])
        return G

    allgroups = []
    for t in range(ntiles):
        for l in range(depth):
            allgroups += weight_groups(l)
    gstate = {"next_load": 0, "loaded": {}, "next_use": 0}

    def issue_group_load():
        gi = gstate["next_load"]
        if gi >= len(allgroups):
            return
        gstate["next_load"] += 1
        base = (gi % 2) * 3
        slots = []
        for ui, parts in enumerate(allgroups[gi]):
            slot = base + ui
            for (src, coff, ncols) in parts:
                dma("pool", "w", RING[:, slot, :, coff:coff + ncols], src.rearrange("(k p) n -> p k n", p=128),
                    reads=[], writes=[("ring", slot)])
            slots.append(slot)
        gstate["loaded"][gi] = slots

    def use_group():
        gi = gstate["next_use"]
        gstate["next_use"] += 1
        while gstate["next_load"] <= gi + 1 and gstate["next_load"] < len(allgroups):
            issue_group_load()
        return gstate["loaded"][gi]

    for t in range(ntiles):
        dma("sp", "io", X[:, 0:nch, :], xp[t * nch * 128:(t + 1) * nch * 128, :].rearrange("(c p) d -> p c d", p=128),
            [], [("x", c) for c in range(nch)])
        if t == 0:
            dma("sp", "io", X[0:ns, nch, :], xs, [], [("x", nch)])
        for l in range(depth):
            phase_norm(t, l, 0)
            su = use_group()
            load_layer_params(t, l)
            phase_G(t, l, su, use_group, after_u=lambda: layer_params_compute(t, l))

            def pa_group():
                s = use_group()
                return [s[0]], s[1]
            phase_P(t, l, AT, lambda k, c: ("aT", k, c), 8, 0, BIG, lambda k, c: ("big", k, c),
                    [pa_group, pa_group, use_group])
            for h in range(H):
                phase_R(t, l, h, use_group())

            def pb_group():
                s = use_group()
                return [s[0], s[1]], s[2]
            phase_P(t, l, BIG, lambda k, c: ("big", k, c), 16, 8, AT, lambda k, c: ("aT", k, c),
                    [pb_group, pb_group, use_group])
            phase_norm(t, l, 1)
            phase_F(t, l, [use_group] * 8)
        cl = chunk_list(t)
        for c in cl:
            n = ntok(c)
            jb = rot("wb")
            act(WB[0:n, jb, :], X[0:n, c, :], AF.Square, [("x", c), ("ssq",)], [("wb", jb), ("ssq", c)],
                accum_out=SSQ[0:n, c:c + 1])
        ncl = len(cl)
        dve("tensor_scalar", [("ssq", c) for c in cl] + [("ssq",)], [("rs",)], out=RS[:, 0:ncl], in0=SSQ[:, 0:ncl],
            scalar1=1.0 / D, scalar2=EPS, op0=ALU.mult, op1=ALU.add)
        pool_pow(RS[:, 0:ncl], [("rs",)])
        gf = rot("wf")
        dma("sp", "par", WF[:, gf, :], gfin_d.partition_broadcast(128), [], [("wf", gf, 0), ("wf", gf, 1)])
        for c in cl:
            n = ntok(c)
            dve("scalar_tensor_tensor", [("x", c), ("rs",), ("wf", gf, 0), ("wf", gf, 1)], [("x", c)], out=X[0:n, c, :],
                in0=X[0:n, c, :], scalar=RS[0:n, c:c + 1], in1=WF[0:n, gf, :], op0=ALU.mult, op1=ALU.mult)
        dma("sp", "io", yp[t * nch * 128:(t + 1) * nch * 128, :].rearrange("(c p) d -> p c d", p=128), X[:, 0:nch, :],
            [("x", c) for c in range(nch)], [("yp", t)])
        if t == 0:
            dma("sp", "io", ys, X[0:ns, nch, :], [("x", nch)], [("ys",)])

    outs = [("yp", t) for t in range(ntiles)] + [("ys",)] + [("gmv", l) for l in range(depth)]
    outs += [("nrp", l, h) for l in range(depth) for h in range(H)]
    outs += [("nrs", l, b, h) for l in range(depth) for b in range(ns) for h in range(H)]
    P.op("sp", lambda e: None, reads=outs, writes=[])

    block = es.enter_context(nc.Block())
    P.emit(nc, {"sp": block.sync, "act": block.scalar, "dve": block.vector, "pool": block.gpsimd, "pe": block.tensor},
           sem_alloc, dma_classes)
    es.close()
    P.sbuf_free = SBUF_FREE
    return nc, P


def _prep_common(cfg, inputs):
    depth = cfg.depth
    tabs, _, _ = _host_tables(cfg)
    f = lambda a: np.ascontiguousarray(np.asarray(a, dtype=np.float32))
    g3 = np.stack([f(inputs["norm_mix_g"])[:depth], f(inputs["norm_ffn_g"])[:depth], f(inputs["gm_ln_g"])[:depth]], axis=1)
    gcols = np.ascontiguousarray(g3.reshape(depth, 3, 8, 128).transpose(3, 0, 1, 2))
    bgc = np.ascontiguousarray(f(inputs["b_gate"])[:depth].reshape(depth, 16, 128).transpose(2, 0, 1))
    wt = np.ascontiguousarray(f(inputs["gm_w_s"])[:depth].transpose(0, 3, 1, 2))
    w00 = np.ascontiguousarray(f(inputs["gm_w_s"])[:depth, :, 0, 0])
    common = dict(
        w_in=f(inputs["w_in"])[:depth], w_pgm=f(inputs["w_proj_gm"])[:depth], w_pret=f(inputs["w_proj_ret"])[:depth],
        w_out=f(inputs["w_out"])[:depth], w_up=f(inputs["w_up"])[:depth], w_down=f(inputs["w_down"])[:depth],
        gcols=gcols, gfin=f(inputs["norm_final_g"]), bgcols=bgc, lng=f(inputs["gm_ln_g"])[:depth],
        lnb=f(inputs["gm_ln_b"])[:depth], wt=wt, w00=w00, bs=f(inputs["gm_b_s"])[:depth].reshape(depth, 512),
        cs=tabs["cs"], cs_s=tabs["cs_s"], dqk=tabs["dqk"], mask=tabs["mask"], ident=tabs["ident"],
    )
    return common


_CACHE = {}


def run(cfg, inputs, ncores, trace=False):
    key = (cfg.depth, cfg.nch, cfg.ntiles, cfg.ns)
    if key not in _CACHE:
        _CACHE[key] = build_nc(cfg)
    nc, _ = _CACHE[key]
    common = _prep_common(cfg, inputs)
    xp = np.asarray(inputs["x_prompt"], dtype=np.float32)
    xs = np.asarray(inputs["x_sample"], dtype=np.float32)
    st = np.asarray(inputs["state_ret"], dtype=np.float32)
    ns = cfg.ns
    in_maps = []
    for b in range(ncores):
        m = dict(common)
        m["xp"] = np.ascontiguousarray(xp[b])
        m["xs"] = np.ascontiguousarray(xs[b * ns:(b + 1) * ns, 0, :])
        m["st"] = np.ascontiguousarray(st[:cfg.depth, b * ns:(b + 1) * ns])
        in_maps.append(m)
    res = run_bass_kernel_spmd(nc, in_maps, core_ids=list(range(ncores)), trace=trace)
    rs = res.results
    y_prompt = np.stack([rs[b]["yp"] for b in range(ncores)], axis=0)
    y_sample = np.concatenate([rs[b]["ys"] for b in range(ncores)], axis=0)[:, None, :]
    nrp = np.stack([rs[b]["nrp"] for b in range(ncores)], axis=1)
    nrs = np.concatenate([rs[b]["nrs"] for b in range(ncores)], axis=1)
    gmv = np.concatenate([rs[b]["gmv"] for b in range(ncores)], axis=1)[:, :, None, :]
    out = (y_prompt.astype(np.float32), y_sample.astype(np.float32), nrp.astype(np.float32), nrs.astype(np.float32),
           gmv.astype(np.float32))
    return out, res


def kernel(**inputs):
    cfg = Cfg(depth=4, nch=8, ntiles=2, ns=16)
    out, _ = run(cfg, inputs, 8)
    return out
```

```python
import os
import numpy as np
from contextlib import ExitStack
import concourse.bass as bass
import concourse.mybir as mybir
from concourse.bass_utils import run_bass_kernel_spmd

F32 = mybir.dt.float32
BF16 = mybir.dt.bfloat16
AF = mybir.ActivationFunctionType
ALU = mybir.AluOpType

D = 1024
H = 4
DK = 256
DV = 512
DFF = 4096
INW = 10240
EPS = 1e-6
NSLOT = 6
PAST_LEN = 16384


class Cfg:
    def __init__(self, depth=4, nch=8, ntiles=2, ns=16):
        self.depth, self.nch, self.ntiles, self.ns = depth, nch, ntiles, ns
        self.seq = nch * ntiles * 128
        self.ntok = nch * 128 + ns


class Prog:
    COMPUTE = ("pe", "act", "dve", "pool")

    def __init__(self):
        self.ops = []
        self.cells = {}

    def op(self, eng, fn, reads=(), writes=(), dma=None):
        oid = len(self.ops)
        deps = set()
        for k in reads:
            c = self.cells.get(k)
            if c is not None and c[0] is not None:
                deps.add(c[0])
        for k in writes:
            c = self.cells.get(k)
            if c is not None:
                if c[0] is not None:
                    deps.add(c[0])
                deps.update(c[1].values())
                deps.update(c[2])
        for k in reads:
            c = self.cells.get(k)
            if c is None:
                c = self.cells[k] = [None, {}, []]
            if dma is None:
                c[1][eng] = oid
            else:
                c[2].append(oid)
        for k in writes:
            self.cells[k] = [oid, {}, []]
        deps.discard(oid)
        self.ops.append(dict(eng=eng, fn=fn, deps=deps, dma=dma))
        return oid

    def emit(self, nc, block_engines, sem_alloc, dma_classes):
        ops = self.ops
        needed = set()
        for o in ops:
            needed.update(o["deps"])
        ROLL = 30000
        cnt = {e: 0 for e in self.COMPUTE}
        epoch = {e: 0 for e in self.COMPUTE}
        esems = {e: [sem_alloc("c_%s_0" % e)] for e in self.COMPUTE}
        dcnt = {}
        for i, o in enumerate(ops):
            o["pre"] = None
            if o["dma"] is not None:
                cls = o["dma"]
                sems = dma_classes[cls]
                n = dcnt.get(cls, 0)
                dcnt[cls] = n + 1
                s = sems[n % len(sems)]
                k = n // len(sems)
                o["pre"] = (s, 16 * k) if k > 0 else None
                o["ev"] = (s, 16 * (k + 1))
                o["inc"] = (s, 16)
            elif o["eng"] in self.COMPUTE and i in needed:
                e = o["eng"]
                if cnt[e] >= ROLL:
                    epoch[e] += 1
                    cnt[e] = 0
                    esems[e].append(sem_alloc("c_%s_%d" % (e, epoch[e])))
                cnt[e] += 1
                s = esems[e][epoch[e]]
                o["ev"] = (s, cnt[e])
                o["inc"] = (s, 1)
            else:
                o["ev"] = None
                o["inc"] = None
        per = {}
        for i, o in enumerate(ops):
            per.setdefault(o["eng"], []).append(i)
        self.nwaits = 0

        def run(engname, e):
            seen = {}
            for i in per.get(engname, []):
                o = ops[i]
                want = {}
                for d in o["deps"]:
                    od = ops[d]
                    if od["dma"] is None and od["eng"] == "pe" and engname == "pe":
                        continue
                    ev = od["ev"]
                    if ev is None:
                        continue
                    s, v = ev
                    if want.get(id(s), (None, -1))[1] < v:
                        want[id(s)] = (s, v)
                if o["pre"] is not None:
                    s, v = o["pre"]
                    if want.get(id(s), (None, -1))[1] < v:
                        want[id(s)] = (s, v)
                for sid, (s, v) in want.items():
                    if seen.get(sid, -1) >= v:
                        continue
                    seen[sid] = v
                    e.wait_ge(s, v)
                    self.nwaits += 1
                ins = o["fn"](e)
                if o["inc"] is not None:
                    assert ins is not None
                    ins.then_inc(o["inc"][0], o["inc"][1])

        for engname, deco in block_engines.items():
            deco((lambda en: (lambda e: run(en, e)))(engname))


def _host_tables(cfg):
    nch, ntiles = cfg.nch, cfg.ntiles
    inv_freq = (1.0 / (np.float32(10000.0) ** np.linspace(0.0, 1.0, DK // 2, dtype=np.float32))).astype(np.float32)
    pos = np.arange(cfg.seq, dtype=np.int32).astype(np.float32)
    ang = (pos[:, None] * inv_freq[None, :]).astype(np.float32)
    cos = np.cos(ang).astype(np.float32).reshape(ntiles, nch, 128, 128).transpose(0, 2, 1, 3)
    sin = np.sin(ang).astype(np.float32).reshape(ntiles, nch, 128, 128).transpose(0, 2, 1, 3)
    angs = (np.float32(PAST_LEN) * inv_freq).astype(np.float32)
    cs_s = np.stack([np.cos(angs), np.sin(angs)]).astype(np.float32)
    cs_s = np.broadcast_to(cs_s[None], (16, 2, 128)).copy()
    g = 1.0 - 2.0 ** (-5.0 - np.arange(H, dtype=np.float64))
    p = (np.arange(nch)[None, :, None] * 128 + np.arange(128)[:, None, None] + 1).astype(np.float64)
    dq = (g[None, None, :] ** p).astype(np.float32)
    dk = ((g[None, None, :] ** (-p)) * (DK ** -0.5)).astype(np.float32)
    dqk = np.stack([dq, dk], axis=2).copy()
    j = np.arange(128)[:, None]
    i = np.arange(128)[None, :]
    mask = (i >= j).astype(np.float32)
    ident = np.eye(128, dtype=np.float32)
    gam = g
    gend = g ** (128 * nch)
    cs = np.ascontiguousarray(np.stack([cos, sin], axis=3))
    return dict(cs=cs, cs_s=cs_s, dqk=dqk, mask=mask,
                ident=ident), gam, gend


def build_nc(cfg):
    depth, nch, ntiles, ns = cfg.depth, cfg.nch, cfg.ntiles, cfg.ns
    NT = cfg.ntok
    _, GAM, GEND = _host_tables(cfg)
    nc = bass.Bass("TRN2", target_bir_lowering=False)

    def din(name, shape):
        return nc.dram_tensor(name, list(shape), F32, kind="ExternalInput").ap()

    def dout(name, shape):
        return nc.dram_tensor(name, list(shape), F32, kind="ExternalOutput").ap()

    xp = din("xp", [cfg.seq, D])
    xs = din("xs", [ns, D])
    st = din("st", [depth, ns, H, DK, DV])
    w_in = din("w_in", [depth, D, INW])
    w_pgm = din("w_pgm", [depth, D, D])
    w_pret = din("w_pret", [depth, 2 * D, D])
    w_out = din("w_out", [depth, D, D])
    w_up = din("w_up", [depth, D, DFF])
    w_down = din("w_down", [depth, DFF, D])
    gcols_d = din("gcols", [128, depth, 3, 8])
    gfin_d = din("gfin", [D])
    bg_d = din("bgcols", [128, depth, 16])
    lng_d = din("lng", [depth, D])
    lnb_d = din("lnb", [depth, D])
    wt_d = din("wt", [depth, 128, 4, 128])
    w00_d = din("w00", [depth, 4])
    bs_d = din("bs", [depth, 4 * 128])
    cs_d = din("cs", [ntiles, 128, nch, 2, 128])
    css_d = din("cs_s", [16, 2, 128])
    dqk_d = din("dqk", [128, nch, 2, H])
    mask_d = din("mask", [128, 128])
    ident_d = din("ident", [128, 128])

    yp = dout("yp", [cfg.seq, D])
    ys = dout("ys", [ns, D])
    nrp = dout("nrp", [depth, H, DK, DV])
    nrs = dout("nrs", [depth, ns, H, DK, DV])
    gmv = dout("gmv", [depth, ns, D])

    P = Prog()
    es = ExitStack()

    def sb(name, shape, dt):
        return es.enter_context(nc.sbuf_tensor(name, list(shape), dt))

    def ps(name, shape, dt):
        return es.enter_context(nc.psum_tensor(name, list(shape), dt))

    X = sb("X", [128, nch + 1, D], F32)
    XNT = sb("XNT", [128, 8, NT], BF16)
    AT = sb("AT", [128, 8, NT], BF16)
    BIG = sb("BIG", [128, 16, NT], BF16)
    RING = sb("RING", [128, NSLOT, 8, 512], BF16)
    CSB = sb("CSB", [128, 2, 2, 128], F32)
    CSS = sb("CSS", [16, 2, 128], F32)
    DQK = sb("DQK", [128, nch, 2, H], F32)
    MASK = sb("MASK", [128, 128], F32)
    IDF = sb("IDF", [128, 128], F32)
    IDB = sb("IDB", [128, 128], BF16)
    ONESB = sb("ONESB", [1, 128], BF16)
    MHALF = sb("MHALF", [128, 16], F32)
    GCOLS = sb("GCOLS", [128, depth, 3, 8], F32)
    BGC = sb("BGC", [128, depth, 16], F32)
    WC = sb("WC", [128, 2, 2, 512], BF16)
    SBUF_S = sb("SBUF_S", [128, 2, 2, 512], F32)
    WF = sb("WF", [128, 2, 1024], F32)
    WB = sb("WB", [128, 2, 1024], BF16)
    QKP = sb("QKP", [128, 2, 512], BF16)
    VB = sb("VB", [128, 2, 512], BF16)
    SG = sb("SG", [128, 3, 512], BF16)
    VBS = sb("VBS", [16, 512], BF16)
    OSA = sb("OSA", [16, 512], F32)
    QKT = sb("QKT", [128, 2, 4, 128], BF16)
    SCT = sb("SCT", [128, 2, 128], BF16)
    RR = sb("RR", [128, 2, 512], BF16)
    RTAB = sb("RTAB", [128, 8, 128], F32)
    RTABS = sb("RTABS", [128, 8, 16], F32)
    WTM = sb("WTM", [128, 4, 128], BF16)
    DG = sb("DG", [16, 4, 16], BF16)
    W00 = sb("W00", [16, 4], F32)
    BSH = sb("BSH", [1, 4, 128], BF16)
    BSL = sb("BSL", [1, 4, 128], BF16)
    BS0H = sb("BS0H", [1, 4, 16], BF16)
    BS0L = sb("BS0L", [1, 4, 16], BF16)
    SSQ = sb("SSQ", [128, 16], F32)
    RS = sb("RS", [128, 16], F32)
    ST1 = sb("ST1", [128, 16], F32)
    QM = sb("QM", [128, 16, 2, 16], F32)
    QS = sb("QS", [16, 256], F32)
    KS = sb("KS", [16, 256], BF16)
    KM = sb("KM", [16, 2, 256], BF16)

    SBUF_FREE = nc.sbuf_bytes_remaining
    PB_ = [ps("P%d" % i, [128, 512], F32) for i in range(4)]
    P45 = ps("P45", [128, 2, 512], F32)
    PT = [ps("PT0", [128, 8, 128], BF16)]
    PX = ps("PX", [128, 512], F32)

    def bank(i):
        return PB_[i] if i < 4 else P45[:, i - 4, :]

    def bk(i):
        return ("ps", i)

    sems = []

    def sem_alloc(name):
        s = es.enter_context(nc.semaphore(name))
        sems.append(s)
        return s

    dma_classes = {
        "w": [sem_alloc("dw%d" % i) for i in range(NSLOT)],
        "io": [sem_alloc("dio%d" % i) for i in range(4)],
        "par": [sem_alloc("dpar%d" % i) for i in range(6)],
        "ppar": [sem_alloc("dppar%d" % i) for i in range(2)],
        "si": [sem_alloc("dsi%d" % i) for i in range(4)],
        "so": [sem_alloc("dso%d" % i) for i in range(4)],
    }

    def chunk_list(t):
        cl = list(range(nch))
        if t == 0:
            cl.append(nch)
        return cl

    def ntok(c):
        return 128 if c < nch else ns

    def tok0(c):
        return c * 128

    def tokgroups(t):
        tgs = []
        c = 0
        while c < nch:
            n = min(4, nch - c)
            tgs.append((c * 128, n * 128, list(range(c, c + n))))
            c += n
        if t == 0:
            tgs.append((nch * 128, ns, [nch]))
        return tgs

    def dma(eng, cls, out, in_, reads, writes):
        P.op(eng, lambda e: e.dma_start(out=out, in_=in_), reads=reads, writes=writes, dma=cls)

    wstate = {"n": 0}

    def wload(parts):
        slot = wstate["n"] % NSLOT
        wstate["n"] += 1
        for (src, coff, ncols) in parts:
            dma("pool", "w", RING[:, slot, :, coff:coff + ncols], src.rearrange("(k p) n -> p k n", p=128),
                reads=[], writes=[("ring", slot)])
        return slot

    def win_unit(l, c0, n=512):
        return [(w_in[l, :, c0:c0 + n], 0, n)]

    def mm_group(out_ap, pairs, reads, writes, first=True, last=True):
        def fn(e):
            ins = None
            n = len(pairs)
            for i, (a, b) in enumerate(pairs):
                ins = e.matmul(out_ap, lhsT=a, rhs=b, start=(first and i == 0), stop=(last and i == n - 1))
            return ins
        P.op("pe", fn, reads=reads, writes=writes)

    def transposes(out_t, in_list, ident, reads, writes):
        def fn(e):
            ins = None
            for (o, i_) in in_list:
                ins = e.transpose(o, i_, ident)
            return ins
        P.op("pe", fn, reads=reads, writes=writes)

    def act(out, in_, func, reads, writes, **kw):
        P.op("act", lambda e: e.activation(out=out, in_=in_, func=func, **kw), reads=reads, writes=writes)

    def dve(fname, reads, writes, **kw):
        P.op("dve", lambda e: getattr(e, fname)(**kw), reads=reads, writes=writes)

    def pool_pow(ap, cells):
        n = ap.shape[-1]
        P.op("pool", lambda e: e.tensor_tensor(out=ap, in0=ap, in1=MHALF[0:ap.shape[0], 0:n], op=ALU.pow),
             reads=cells + [("mhalf",)], writes=cells)

    cnt = {"wf": 0, "wb": 0, "qkp": 0, "vb": 0, "qkt": 0, "sct": 0, "rr": 0, "pt": 0, "sg": 0, "cs": 0, "wc": 0, "ss": 0, "km": 0}

    def rot(name, n=2):
        v = cnt[name] % n
        cnt[name] += 1
        return v

    dma("sp", "par", CSS[:], css_d, [], [("css",)])
    dma("sp", "par", DQK[:], dqk_d, [], [("dqk",)])
    dma("sp", "par", MASK[:], mask_d, [], [("mask",)])
    dma("sp", "par", IDF[:], ident_d, [], [("idf",)])
    dma("sp", "par", GCOLS[:], gcols_d, [], [("gcols",)])
    dma("sp", "par", BGC[:], bg_d, [], [("bgc",)])
    dve("tensor_copy", [("idf",)], [("idb",)], out=IDB[:], in_=IDF[:])
    dve("memset", [], [("onesb",)], ap=ONESB[:], constant=1.0)
    dve("memset", [], [("mhalf",)], ap=MHALF[:], constant=-0.5)
    dve("memset", [], [("ssq",)], ap=SSQ[:], constant=1.0)
    dve("memset", [], [("st1",)], ap=ST1[:], constant=1.0)
    dve("memset", [], [("qm",)], ap=QM[:], constant=0.0)

    def phase_norm(t, l, gi):
        cl = chunk_list(t)
        for c in cl:
            n = ntok(c)
            jb = rot("wb")
            act(WB[0:n, jb, :], X[0:n, c, :], AF.Square, [("x", c), ("ssq",)], [("wb", jb), ("ssq", c)],
                accum_out=SSQ[0:n, c:c + 1])
        ncl = len(cl)
        dve("tensor_scalar", [("ssq", c) for c in cl] + [("ssq",)], [("rs",)], out=RS[:, 0:ncl], in0=SSQ[:, 0:ncl],
            scalar1=1.0 / D, scalar2=EPS, op0=ALU.mult, op1=ALU.add)
        pool_pow(RS[:, 0:ncl], [("rs",)])
        xbs = {}

        def do_xs(c):
            n = ntok(c)
            xb = rot("wb")
            xbs[c] = xb
            dve("tensor_scalar", [("x", c), ("rs",)], [("wb", xb)], out=WB[0:n, xb, :], in0=X[0:n, c, :],
                scalar1=RS[0:n, c:c + 1], scalar2=None, op0=ALU.mult)
        do_xs(cl[0])
        for i, c in enumerate(cl):
            n = ntok(c)
            if i + 1 < len(cl):
                do_xs(cl[i + 1])
            xb = xbs[c]
            transposes(None, [(PT[0][:, k, 0:n], WB[0:n, xb, k * 128:(k + 1) * 128]) for k in range(8)],
                       IDB[0:n, 0:n], [("wb", xb), ("idb",)], [("pt", 0)])
            dve("tensor_tensor", [("pt", 0), ("gcols",)], [("xnT", c)],
                out=XNT[:, :, tok0(c):tok0(c) + n], in0=PT[0][:, :, 0:n],
                in1=GCOLS[:, l, gi, :].unsqueeze(2).to_broadcast([128, 8, n]), op=ALU.mult)

    def load_layer_params(t, l):
        dma("pool", "ppar", WB[:, 1, :], lnb_d[l].partition_broadcast(128), [], [("wb", 1)])
        dma("sp", "par", WF[:, 0, 0:512].rearrange("p (g i) -> p g i", g=4), wt_d[l], [], [("wf", 0, 0)])
        dve("tensor_tensor", [("wf", 0, 0), ("mask",)], [("wtm",)], out=WTM[:],
            in0=WF[:, 0, 0:512].rearrange("p (g i) -> p g i", g=4),
            in1=MASK[:].unsqueeze(1).to_broadcast([128, 4, 128]), op=ALU.mult)
        dma("sp", "par", WF[0:1, 1, 0:512], bs_d[l].unsqueeze(0), [], [("wf", 1, 0)])
        dve("tensor_copy", [("wf", 1, 0)], [("bsh",)], out=BSH[:].rearrange("p g i -> p (g i)"), in_=WF[0:1, 1, 0:512])
        dve("tensor_tensor", [("wf", 1, 0), ("bsh",)], [("wf", 1, 1)], out=WF[0:1, 1, 512:1024], in0=WF[0:1, 1, 0:512],
            in1=BSH[:].rearrange("p g i -> p (g i)"), op=ALU.subtract)
        dve("tensor_copy", [("wf", 1, 1)], [("bsl",)], out=BSL[:].rearrange("p g i -> p (g i)"), in_=WF[0:1, 1, 512:1024])

    def layer_params_compute(t, l):
        for kk in range(8):
            g = kk // 2
            mm_group(bank(kk % 2)[:, (kk // 2) * 128:(kk // 2) * 128 + 128],
                     [(WB[:, 1, kk * 128:(kk + 1) * 128], WTM[:, g, :]),
                      (ONESB[0:1, :], BSH[0:1, g, :]), (ONESB[0:1, :], BSL[0:1, g, :])],
                     [("wb", 1), ("wtm",), ("onesb",), ("bsh",), ("bsl",)], [bk(kk % 2)])
        for par in range(2):
            dve("tensor_copy", [bk(par)], [("rtab",)],
                out=RTAB[:].rearrange("p (a b) i -> p a b i", b=2)[:, :, par, :],
                in_=bank(par)[:].rearrange("p (a i) -> p a i", a=4))
        if t == 0:
            dma("sp", "par", W00[:], w00_d[l].partition_broadcast(16), [], [("w00",)])
            for g in range(4):
                dve("tensor_scalar", [("w00",), ("idf",)], [("dg",)], out=DG[:, g, :], in0=IDF[0:16, 0:16],
                    scalar1=W00[:, g:g + 1], scalar2=None, op0=ALU.mult)
            dve("tensor_copy", [("bsh",)], [("bs0h",)], out=BS0H[:], in_=BSH[:, :, 0:1].to_broadcast([1, 4, 16]))
            dve("tensor_copy", [("bsl",)], [("bs0l",)], out=BS0L[:], in_=BSL[:, :, 0:1].to_broadcast([1, 4, 16]))
            for kk in range(8):
                g = kk // 2
                mm_group(bank(2)[:, kk * 16:(kk + 1) * 16],
                         [(WB[0:16, 1, kk * 128:(kk + 1) * 128], DG[:, g, :]),
                          (ONESB[0:1, :], BS0H[0:1, g, :]), (ONESB[0:1, :], BS0L[0:1, g, :])],
                         [("wb", 1), ("dg",), ("onesb",), ("bs0h",), ("bs0l",)], [bk(2)])
            dve("tensor_copy", [bk(2)], [("rtabs",)], out=RTABS[:],
                in_=bank(2)[:, 0:128].rearrange("p (a i) -> p a i", a=8))

    def phase_G(t, l, slots_u, get_slots_v, after_u=None):
        tgs = tokgroups(t)
        pb = 0
        for ui in range(2):
            for (t0, n, cs) in tgs:
                for cc in range(4):
                    b = pb % 2
                    pb += 1
                    kk = ui * 4 + cc
                    mm_group(bank(b)[:, 0:n],
                             [(RING[:, slots_u[ui], k, cc * 128:(cc + 1) * 128], XNT[:, k, t0:t0 + n]) for k in range(8)],
                             [("ring", slots_u[ui])] + [("xnT", c) for c in cs], [bk(b)])
                    act(AT[:, kk, t0:t0 + n], bank(b)[:, 0:n], AF.Gelu_apprx_tanh, [bk(b)], [("aT", kk, c) for c in cs])
        slots_v = get_slots_v()
        if after_u is not None:
            after_u()
        def part1(c):
            n = ntok(c)
            vg = c % 2
            sb_ = (c % 2) * 8
            for hf in range(2):
                mm_group(bank(2 + hf)[0:n, :],
                         [(XNT[:, k, tok0(c):tok0(c) + n], RING[:, slots_v[hf], k, :]) for k in range(8)],
                         [("ring", slots_v[hf]), ("xnT", c)], [bk(2 + hf)])
                act(WF[0:n, vg, hf * 512:(hf + 1) * 512], bank(2 + hf)[0:n, :], AF.Gelu_apprx_tanh, [bk(2 + hf), ("st1",)],
                    [("wf", vg, hf), ("st1", sb_ + hf)], accum_out=ST1[0:n, sb_ + hf:sb_ + hf + 1])
            act(WB[0:n, 0, :], WF[0:n, vg, :], AF.Square, [("wf", vg, 0), ("wf", vg, 1), ("st1",)],
                [("wb", 0), ("st1", sb_ + 3)], accum_out=ST1[0:n, sb_ + 3:sb_ + 4])
            dve("tensor_scalar", [("st1", sb_), ("st1", sb_ + 1), ("st1",)], [("st1", sb_ + 2)], out=ST1[0:n, sb_ + 2:sb_ + 3],
                in0=ST1[0:n, sb_:sb_ + 1], scalar1=ST1[0:n, sb_ + 1:sb_ + 2], scalar2=-1.0 / D, op0=ALU.add, op1=ALU.mult)
            dve("tensor_scalar", [("st1", sb_ + 2)], [("st1", sb_ + 7)], out=ST1[0:n, sb_ + 7:sb_ + 8],
                in0=ST1[0:n, sb_ + 2:sb_ + 3], scalar1=ST1[0:n, sb_ + 2:sb_ + 3], scalar2=EPS, op0=ALU.mult, op1=ALU.subtract)
            dve("scalar_tensor_tensor", [("st1", sb_ + 3), ("st1", sb_ + 7)], [("st1", sb_ + 4)], out=ST1[0:n, sb_ + 4:sb_ + 5],
                in0=ST1[0:n, sb_ + 3:sb_ + 4], scalar=1.0 / D, in1=ST1[0:n, sb_ + 7:sb_ + 8], op0=ALU.mult, op1=ALU.subtract)
            pool_pow(ST1[0:n, sb_ + 4:sb_ + 5], [("st1", sb_ + 4)])

        def part2(c):
            n = ntok(c)
            vg = c % 2
            sb_ = (c % 2) * 8
            vh = 1
            stc = [("st1", sb_ + 2), ("st1", sb_ + 4)]
            dve("tensor_scalar", [("wf", vg, 0), ("wf", vg, 1)] + stc, [("wb", vh)],
                out=WB[0:n, vh, :], in0=WF[0:n, vg, :], scalar1=ST1[0:n, sb_ + 2:sb_ + 3], scalar2=ST1[0:n, sb_ + 4:sb_ + 5],
                op0=ALU.add, op1=ALU.mult)
            if c == nch:
                dve("tensor_scalar", [("wf", vg, 0), ("wf", vg, 1)] + stc, [("wf", vg, 0), ("wf", vg, 1)],
                    out=WF[0:n, vg, :], in0=WF[0:n, vg, :], scalar1=ST1[0:n, sb_ + 2:sb_ + 3], scalar2=ST1[0:n, sb_ + 4:sb_ + 5],
                    op0=ALU.add, op1=ALU.mult)
                sc = 1 - vg
                dma("sp", "par", WF[0:n, sc, :], lng_d[l].partition_broadcast(n), [], [("wf", sc, 0), ("wf", sc, 1)])
                dve("tensor_tensor", [("wf", vg, 0), ("wf", vg, 1), ("wf", sc, 0), ("wf", sc, 1)],
                    [("wf", vg, 0), ("wf", vg, 1)], out=WF[0:n, vg, :], in0=WF[0:n, vg, :], in1=WF[0:n, sc, :], op=ALU.mult)
                dma("sp", "par", WF[0:n, sc, :], lnb_d[l].partition_broadcast(n), [], [("wf", sc, 0), ("wf", sc, 1)])
                dve("tensor_tensor", [("wf", vg, 0), ("wf", vg, 1), ("wf", sc, 0), ("wf", sc, 1)],
                    [("wf", vg, 0), ("wf", vg, 1)], out=WF[0:n, vg, :], in0=WF[0:n, vg, :], in1=WF[0:n, sc, :], op=ALU.add)
                dma("sp", "io", gmv[l], WF[0:n, vg, :], [("wf", vg, 0), ("wf", vg, 1)], [("gmv", l)])
            for kk in range(8):
                g = kk // 2
                rhs = WTM[:, g, :] if c < nch else DG[:, g, :]
                rcell = ("wtm",) if c < nch else ("dg",)
                mm_group(bank(4 + kk // 4)[:, (kk % 4) * 128:(kk % 4) * 128 + n],
                         [(WB[0:n, vh, kk * 128:(kk + 1) * 128], rhs)], [("wb", vh), rcell], [bk(4 + kk // 4)])
            tm = vg
            tmv = WF[:, tm, :].rearrange("p (a i) -> p a i", a=8)[:, :, 0:n]
            dve("tensor_tensor", [bk(4), bk(5), ("gcols",)], [("wf", tm, 0), ("wf", tm, 1)], out=tmv,
                in0=P45[:].rearrange("p b (a i) -> p (b a) i", a=4)[:, :, 0:n],
                in1=GCOLS[:, l, 2, :].unsqueeze(2).to_broadcast([128, 8, n]), op=ALU.mult)
            rt = RTAB[:] if c < nch else RTABS[:]
            P.op("pool", lambda e, tmv=tmv, rt=rt: e.tensor_tensor(out=tmv, in0=tmv, in1=rt, op=ALU.add),
                 reads=[("wf", tm, 0), ("wf", tm, 1), ("rtab",), ("rtabs",)], writes=[("wf", tm, 0), ("wf", tm, 1)])
            P.op("pool", lambda e, tmv=tmv, c=c, n=n: e.tensor_tensor(out=AT[:, :, tok0(c):tok0(c) + n], in0=tmv,
                                                                   in1=AT[:, :, tok0(c):tok0(c) + n], op=ALU.mult),
                 reads=[("wf", tm, 0), ("wf", tm, 1)] + [("aT", kk, c) for kk in range(8)],
                 writes=[("aT", kk, c) for kk in range(8)])

        cl = chunk_list(t)
        part1(cl[0])
        for i in range(1, len(cl)):
            part1(cl[i])
            part2(cl[i - 1])
        part2(cl[-1])

    def phase_P(t, l, src, src_cells, nk, gate_off, dst, dst_cells, groups):
        tgs = tokgroups(t)
        pb = 0
        for ch in range(2):
            wp_slots, wg_slot = groups[ch]()
            for (t0, n, cs) in tgs:
                for cc in range(4):
                    b = pb % 2
                    pb += 1
                    col = ch * 4 + cc
                    pairs = []
                    for k in range(nk):
                        pairs.append((RING[:, wp_slots[k // 8], k % 8, cc * 128:(cc + 1) * 128], src[:, k, t0:t0 + n]))
                    mm_group(bank(b)[:, 0:n], pairs,
                             [("ring", s) for s in wp_slots] + [src_cells(k, c) for k in range(nk) for c in cs], [bk(b)])
                    mm_group(bank(2 + b)[:, 0:n],
                             [(RING[:, wg_slot, k, cc * 128:(cc + 1) * 128], XNT[:, k, t0:t0 + n]) for k in range(8)],
                             [("ring", wg_slot)] + [("xnT", c) for c in cs], [bk(2 + b)])
                    sg = rot("wf")
                    act(WF[:, sg, 0:n], bank(2 + b)[:, 0:n], AF.Sigmoid, [bk(2 + b), ("bgc",)], [("wf", sg, 0)],
                        bias=BGC[:, l, gate_off + col:gate_off + col + 1])
                    dve("tensor_tensor", [("wf", sg, 0), bk(b)], [dst_cells(col, c) for c in cs],
                        out=dst[:, col, t0:t0 + n], in0=bank(b)[:, 0:n], in1=WF[:, sg, 0:n], op=ALU.mult)
        so = groups[2]()
        for c in chunk_list(t):
            n = ntok(c)
            for oh in range(2):
                b = 4 + oh
                mm_group(bank(b)[0:n, :],
                         [(dst[:, k, tok0(c):tok0(c) + n], RING[:, so[oh], k, :]) for k in range(8)],
                         [("ring", so[oh])] + [dst_cells(k, c) for k in range(8)], [bk(b)])
                dve("tensor_tensor", [bk(b), ("x", c)], [("x", c)], out=X[0:n, c, oh * 512:(oh + 1) * 512],
                    in0=bank(b)[0:n, :], in1=X[0:n, c, oh * 512:(oh + 1) * 512], op=ALU.add)

    def gn_elem(n, src, src_cells, sgi):
        jb = rot("wb")
        act(WB[0:n, jb, 0:512], src, AF.Square, src_cells + [("st1",)], [("wb", jb), ("st1", 5)],
            accum_out=ST1[0:n, 5:6])
        dve("tensor_scalar", [("st1", 5)], [("st1", 6)], out=ST1[0:n, 6:7], in0=ST1[0:n, 5:6],
            scalar1=1.0 / DV, scalar2=EPS, op0=ALU.mult, op1=ALU.add)
        pool_pow(ST1[0:n, 6:7], [("st1", 6)])
        rb = rot("rr")
        dve("scalar_tensor_tensor", src_cells + [("st1", 6), ("sg", sgi)], [("rr", rb)], out=RR[0:n, rb, :], in0=src,
            scalar=ST1[0:n, 6:7], in1=SG[0:n, sgi, :], op0=ALU.mult, op1=ALU.mult)
        return rb

    def gn_transpose(n, h, c, rb):
        transposes(None, [(PT[0][:, 4 + e, 0:n], RR[0:n, rb, e * 128:(e + 1) * 128]) for e in range(4)],
                   IDB[0:n, 0:n], [("rr", rb), ("idb",)], [("pt", 0)])
        act(BIG[:, h * 4:(h + 1) * 4, tok0(c):tok0(c) + n], PT[0][:, 4:8, 0:n], AF.Identity, [("pt", 0)],
            [("big", h * 4 + e, c) for e in range(4)])

    GB = {"b": 1}

    def proj3(c, n, which, slot):
        bi = {"qk": 0, "v": 1, "g": GB["b"]}[which]
        mm_group(bank(bi)[0:n, :], [(XNT[:, k, tok0(c):tok0(c) + n], RING[:, slot, k, :]) for k in range(8)],
                 [("ring", slot), ("xnT", c)], [bk(bi)])

    def phase_R(t, l, h, slots):
        sqk, sv, sgs = slots
        gam = float(GAM[h])
        have_state = (t > 0)
        GB["b"] = 1 if t == 0 else 2
        wcur = None
        if have_state:
            dma("sp", "si", SBUF_S[:, 0, :, :], nrp[l, h].rearrange("(a p) e -> p a e", p=128), [("nrp", l, h)], [("ss", 0), ("ss", 0, 0), ("ss", 0, 1)])
            w0 = rot("wc")
            act(WC[:, w0, :, :], SBUF_S[:, 0, :, :], AF.Identity, [("ss", 0)], [("wc", w0)])
            wcur = w0
        gen = None
        if t == 0:
            gen = sample_part(l, h, slots, gam)
            next(gen)

        def bg(k=1):
            nonlocal gen
            if os.environ.get('KBG', '1') == '0':
                return
            for _ in range(k):
                if gen is None:
                    return
                try:
                    next(gen)
                except StopIteration:
                    gen = None

        prev = None
        pend_tr = None
        first = True
        csbuf = {}

        def load_cs(cc):
            bb = rot("cs")
            csbuf[cc] = bb
            dma("sp", "par", CSB[:, bb, :, :], cs_d[t, :, cc, :, :], [], [("cs", bb)])
        load_cs(0)
        for s in range(nch + 1):
            c = s if s < nch else None
            n = 128
            if c is not None and c + 1 < nch:
                load_cs(c + 1)
            cur = None
            if c is not None:
                proj3(c, n, "qk", sqk)
                ra = rot("wf")
                rb_ = rot("wf")
                for qk in range(2):
                    src = bank(0)[0:n, qk * 256:(qk + 1) * 256].rearrange("p (a f) -> p a f", a=2)
                    for (ti, wfi) in ((0, ra), (1, rb_)):
                        dve("scalar_tensor_tensor", [bk(0), ("dqk",), ("cs", csbuf[c])], [("wf", wfi, 0)],
                            out=WF[0:n, wfi, qk * 256:(qk + 1) * 256].rearrange("p (a f) -> p a f", a=2),
                            in0=src, scalar=DQK[0:n, c, qk, h:h + 1],
                            in1=CSB[0:n, csbuf[c], ti, :].unsqueeze(1).to_broadcast([n, 2, 128]), op0=ALU.mult, op1=ALU.mult)
                qpn = rot("qkp")
                A4 = WF[0:n, ra, 0:512].rearrange("p (q a f) -> p q a f", q=2, a=2)
                B4 = WF[0:n, rb_, 0:512].rearrange("p (q a f) -> p q a f", q=2, a=2)
                O4 = QKP[0:n, qpn, :].rearrange("p (q a f) -> p q a f", q=2, a=2)
                rc = [("wf", ra, 0), ("wf", rb_, 0)]
                dve("tensor_tensor", rc, [("qkp", qpn, 0)], out=O4[:, :, 0, :], in0=A4[:, :, 0, :], in1=B4[:, :, 1, :], op=ALU.subtract)
                dve("tensor_tensor", rc, [("qkp", qpn, 1)], out=O4[:, :, 1, :], in0=B4[:, :, 0, :], in1=A4[:, :, 1, :], op=ALU.add)
                cur = {"c": c, "qp": qpn}
            bg()
            if prev is not None:
                qp, vb = prev["qp"], prev["vb"]
                transposes(None, [(PT[0][:, j, 0:n], QKP[0:n, qp, j * 128:(j + 1) * 128]) for j in range(4)],
                           IDB[0:n, 0:n], [("qkp", qp, 0), ("qkp", qp, 1), ("idb",)], [("pt", 0)])
                qt = rot("qkt")
                act(QKT[:, qt, :, 0:n], PT[0][:, 0:4, 0:n], AF.Identity, [("pt", 0)], [("qkt", qt)])
                bg()
            if c is not None:
                proj3(c, n, "v", sv)
                vbn = rot("vb")
                act(VB[0:n, vbn, :], bank(1)[0:n, :], AF.Identity, [bk(1)], [("vb", vbn)])
                cur["vb"] = vbn
            bg()
            if pend_tr is not None:
                gn_transpose(n, h, pend_tr[0], pend_tr[1])
                pend_tr = None
                bg()
            if prev is not None:
                mm_group(bank(1)[0:n, 0:n], [(QKT[:, qt, 2 + hf, 0:n], QKT[:, qt, hf, 0:n]) for hf in range(2)],
                         [("qkt", qt)], [bk(1)])
                sc = rot("sct")
                dve("tensor_tensor", [bk(1), ("mask",)], [("sct", sc)], out=SCT[0:n, sc, 0:n], in0=bank(1)[0:n, 0:n],
                    in1=MASK[0:n, 0:n], op=ALU.mult)
                bg()
            if c is not None:
                proj3(c, n, "g", sgs)
                sgn = rot("sg")
                act(SG[0:n, sgn, :], bank(GB["b"])[0:n, :], AF.Silu, [bk(GB["b"])], [("sg", sgn)])
                cur["sg"] = sgn
            bg()
            if prev is not None:
                pairs = [(SCT[0:n, sc, 0:n], VB[0:n, vb, :])]
                rds = [("sct", sc), ("vb", vb)]
                if have_state or not first:
                    pairs += [(QKT[:, qt, hf, 0:n], WC[:, wcur, hf, :]) for hf in range(2)]
                    rds += [("qkt", qt), ("wc", wcur)]
                mm_group(bank(3)[0:n, :], pairs, rds, [bk(3)])
                for hf in range(2):
                    def fn(e, hf=hf, qp=qp, vb=vb, first=first):
                        return e.matmul(bank(4 + hf)[:, :], lhsT=QKP[0:128, qp, 256 + hf * 128:256 + (hf + 1) * 128],
                                        rhs=VB[0:128, vb, :], start=first, stop=True, skip_group_check=True)
                    P.op("pe", fn, reads=[("qkp", qp, 0), ("qkp", qp, 1), ("vb", vb)], writes=[bk(4 + hf)])
                first = False
                if prev["c"] < nch - 1:
                    wn = rot("wc")
                    if have_state:
                        dve("tensor_tensor", [bk(4), bk(5), ("ss", 0)], [("wc", wn)], out=WC[:, wn, :, :], in0=P45[:],
                            in1=SBUF_S[:, 0, :, :], op=ALU.add)
                    else:
                        act(WC[:, wn, :, :], P45[:], AF.Identity, [bk(4), bk(5)], [("wc", wn)])
                    wcur = wn
            bg()
            if prev is not None:
                rbuf = gn_elem(n, bank(3)[0:n, :], [bk(3)], prev["sg"])
                pend_tr = (prev["c"], rbuf)
            bg()
            prev = cur
        if pend_tr is not None:
            gn_transpose(128, h, pend_tr[0], pend_tr[1])
        sn = rot("wf")
        sview = WF[:, sn, :].rearrange("p (a e) -> p a e", a=2)
        if have_state:
            dve("tensor_tensor", [bk(4), bk(5), ("ss", 0)], [("wf", sn, 0), ("wf", sn, 1)], out=sview, in0=P45[:],
                in1=SBUF_S[:, 0, :, :], op=ALU.add)
            act(sview, sview, AF.Identity, [("wf", sn, 0), ("wf", sn, 1)], [("wf", sn, 0), ("wf", sn, 1)], scale=float(GEND[h]))
        else:
            act(sview, P45[:], AF.Identity, [bk(4), bk(5)], [("wf", sn, 0), ("wf", sn, 1)], scale=float(GEND[h]))
        dma("sp", "so", nrp[l, h].rearrange("(a p) e -> p a e", p=128), sview, [("wf", sn, 0), ("wf", sn, 1)], [("nrp", l, h)])
        if gen is not None:
            for _ in gen:
                pass

    def sample_part(l, h, slots, gam):
        sqk, sv, sgs = slots
        c = nch
        n = ns
        proj3(c, n, "qk", sqk)
        proj3(c, n, "v", sv)
        act(VBS[0:n, :], bank(1)[0:n, :], AF.Identity, [bk(1)], [("vbs",)])
        proj3(c, n, "g", sgs)
        act(SG[0:n, 2, :], bank(1)[0:n, :], AF.Silu, [bk(1)], [("sg", 2)])
        ra = rot("wf")
        rb_ = rot("wf")
        for qk in range(2):
            src = bank(0)[0:n, qk * 256:(qk + 1) * 256].rearrange("p (a f) -> p a f", a=2)
            for (ti, wfi) in ((0, ra), (1, rb_)):
                dve("scalar_tensor_tensor", [bk(0), ("css",)], [("wf", wfi, 0)],
                    out=WF[0:n, wfi, qk * 256:(qk + 1) * 256].rearrange("p (a f) -> p a f", a=2),
                    in0=src, scalar=(1.0 if qk == 0 else DK ** -0.5),
                    in1=CSS[0:n, ti, :].unsqueeze(1).to_broadcast([n, 2, 128]), op0=ALU.mult, op1=ALU.mult)
        A4 = WF[0:n, ra, 0:512].rearrange("p (q a f) -> p q a f", q=2, a=2)
        B4 = WF[0:n, rb_, 0:512].rearrange("p (q a f) -> p q a f", q=2, a=2)
        rc = [("wf", ra, 0), ("wf", rb_, 0)]
        QS2 = QS[:].rearrange("p (a f) -> p a f", a=2)
        KS2 = KS[:].rearrange("p (a f) -> p a f", a=2)
        dve("tensor_tensor", rc, [("qs", 0)], out=QS2[:, 0, :], in0=A4[:, 0, 0, :], in1=B4[:, 0, 1, :], op=ALU.subtract)
        dve("tensor_tensor", rc, [("qs", 1)], out=QS2[:, 1, :], in0=B4[:, 0, 0, :], in1=A4[:, 0, 1, :], op=ALU.add)
        dve("tensor_tensor", rc, [("ks", 0)], out=KS2[:, 0, :], in0=A4[:, 1, 0, :], in1=B4[:, 1, 1, :], op=ALU.subtract)
        dve("tensor_tensor", rc, [("ks", 1)], out=KS2[:, 1, :], in0=B4[:, 1, 0, :], in1=A4[:, 1, 1, :], op=ALU.add)
        transposes(None, [(bank(1)[:, hf * 16:hf * 16 + n], QS[0:n, hf * 128:(hf + 1) * 128]) for hf in range(2)],
                   IDF[0:n, 0:n], [("qs", 0), ("qs", 1), ("idf",)], [bk(1)])
        for b in range(n):
            dve("tensor_copy", [bk(1), ("qm",)], [("qm", b)], out=QM[:, b, :, b:b + 1],
                in_=bank(1)[:, 0:32].rearrange("p (a j) -> p a j", a=2)[:, :, b:b + 1])
        qkt_ = rot("wf")
        dve("tensor_tensor", [("qs", 0), ("qs", 1), ("ks", 0), ("ks", 1)], [("wf", qkt_, 0)], out=WF[0:n, qkt_, 0:256],
            in0=QS[0:n, :], in1=KS[0:n, :], op=ALU.mult)
        dve("reduce_sum", [("wf", qkt_, 0), ("st1",)], [("st1", 14)], out=ST1[0:n, 14:15], in_=WF[0:n, qkt_, 0:256],
            axis=mybir.AxisListType.X)
        def s_in(bb, slot):
            for hf in range(2):
                dma("sp", "si", SBUF_S[:, slot, hf, :], st[l, bb, h, hf * 128:(hf + 1) * 128, :], [], [("ss", slot, hf)])
        s_in(0, 0)
        yield
        for b in range(n):
            km = rot("km")
            dve("tensor_scalar", [("ks", 0), ("ks", 1), ("idf",)], [("km", km)], out=KM[0:n, km, :], in0=KS[0:n, :],
                scalar1=IDF[0:n, b:b + 1], scalar2=None, op0=ALU.mult)
            ss = b % 2
            if b + 1 < n:
                s_in(b + 1, 1 - ss)
            for hf in range(2):
                def fn(e, b=b, hf=hf, ss=ss):
                    return e.matmul(PB_[2][0:ns, :], lhsT=QM[:, b, hf, :], rhs=SBUF_S[:, ss, hf, :],
                                    start=(b == 0 and hf == 0), stop=(b == ns - 1 and hf == 1), skip_group_check=True)
                P.op("pe", fn, reads=[("qm", b), ("ss", ss, hf)], writes=[bk(2)])
            yield
            for hf in range(2):
                mm_group(PX[:, :], [(KM[0:n, km, hf * 128:(hf + 1) * 128], VBS[0:n, :])],
                         [("km", km), ("vbs",)], [bk(6)])
                dve("scalar_tensor_tensor", [("ss", ss, hf), bk(6)], [("ss", ss, hf)], out=SBUF_S[:, ss, hf, :],
                    in0=SBUF_S[:, ss, hf, :], scalar=gam, in1=PX[:, :], op0=ALU.mult, op1=ALU.add)
                dma("sp", "so", nrs[l, b, h, hf * 128:(hf + 1) * 128, :], SBUF_S[:, ss, hf, :], [("ss", ss, hf)],
                    [("nrs", l, b, h, hf)])
                yield
        tq = rot("wf")
        dve("tensor_scalar", [("vbs",), ("st1", 14)], [("wf", tq, 0)], out=WF[0:n, tq, 0:512], in0=VBS[0:n, :],
            scalar1=ST1[0:n, 14:15], scalar2=None, op0=ALU.mult)
        dve("scalar_tensor_tensor", [bk(2), ("wf", tq, 0)], [("osa",)], out=OSA[:], in0=PB_[2][0:ns, :], scalar=gam,
            in1=WF[0:n, tq, 0:512], op0=ALU.mult, op1=ALU.add)
        rbuf = gn_elem(n, OSA[:], [("osa",)], 2)
        gn_transpose(n, h, c, rbuf)

    def phase_F(t, l, groups):
        tgs = tokgroups(t)
        pb = 0
        for g in range(4):
            su = groups[2 * g]()
            buf = g % 2
            for (t0, n, cs) in tgs:
                for ui in range(2):
                    for cc in range(4):
                        b = pb % 2
                        pb += 1
                        kk = ui * 4 + cc
                        mm_group(bank(b)[:, 0:n],
                                 [(RING[:, su[ui], k, cc * 128:(cc + 1) * 128], XNT[:, k, t0:t0 + n]) for k in range(8)],
                                 [("ring", su[ui])] + [("xnT", c) for c in cs], [bk(b)])
                        rl = rot("wf")
                        act(WF[:, rl, 0:n], bank(b)[:, 0:n], AF.Relu, [bk(b)], [("wf", rl, 0)])
                        dve("tensor_tensor", [("wf", rl, 0)], [("big", buf * 8 + kk, c) for c in cs],
                            out=BIG[:, buf * 8 + kk, t0:t0 + n], in0=WF[:, rl, 0:n], in1=WF[:, rl, 0:n], op=ALU.mult)
            sd = groups[2 * g + 1]()
            for c in chunk_list(t):
                n = ntok(c)
                for oh in range(2):
                    b = 2 + (pb % 2)
                    pb += 1
                    mm_group(bank(b)[0:n, :],
                             [(BIG[:, buf * 8 + k, tok0(c):tok0(c) + n], RING[:, sd[oh], k, :]) for k in range(8)],
                             [("ring", sd[oh])] + [("big", buf * 8 + k, c) for k in range(8)], [bk(b)])
                    dve("tensor_tensor", [bk(b), ("x", c)], [("x", c)], out=X[0:n, c, oh * 512:(oh + 1) * 512],
                        in0=bank(b)[0:n, :], in1=X[0:n, c, oh * 512:(oh + 1) * 512], op=ALU.add)

    def weight_groups(l):
        G = []
        G.append([win_unit(l, 0), win_unit(l, 512)])
        G.append([win_unit(l, 1024), win_unit(l, 1536)])
        for ch in range(2):
            G.append([[(w_pgm[l, :, ch * 512:(ch + 1) * 512], 0, 512)], win_unit(l, 8192 + ch * 512)])
        G.append([[(w_out[l, :, 0:512], 0, 512)], [(w_out[l, :, 512:1024], 0, 512)]])
        for h in range(H):
            G.append([[(w_in[l, :, 2048 + h * 256:2048 + (h + 1) * 256], 0, 256),
                       (w_in[l, :, 3072 + h * 256:3072 + (h + 1) * 256], 256, 256)],
                      win_unit(l, 4096 + h * 512), win_unit(l, 6144 + h * 512)])
        for ch in range(2):
            G.append([[(w_pret[l, 0:1024, ch * 512:(ch + 1) * 512], 0, 512)],
                      [(w_pret[l, 1024:2048, ch * 512:(ch + 1) * 512], 0, 512)], win_unit(l, 9216 + ch * 512)])
        G.append([[(w_out[l, :, 0:512], 0, 512)], [(w_out[l, :, 512:1024], 0, 512)]])
        for g in range(4):
            G.append([[(w_up[l, :, g * 1024:g * 1024 + 512], 0, 512)], [(w_up[l, :, g * 1024 + 512:(g + 1) * 1024], 0, 512)]])
            G.append([[(w_down[l, g * 1024:(g + 1) * 1024, 0:512], 0, 512)], [(w_down[l, g * 1024:(g + 1) * 1024, 512:1024], 0, 512)]])
        return G

    allgroups = []
    for t in range(ntiles):
        for l in range(depth):
            allgroups += weight_groups(l)
    gstate = {"next_load": 0, "loaded": {}, "next_use": 0}

    def issue_group_load():
        gi = gstate["next_load"]
        if gi >= len(allgroups):
            return
        gstate["next_load"] += 1
        base = (gi % 2) * 3
        slots = []
        for ui, parts in enumerate(allgroups[gi]):
            slot = base + ui
            for (src, coff, ncols) in parts:
                dma("pool", "w", RING[:, slot, :, coff:coff + ncols], src.rearrange("(k p) n -> p k n", p=128),
                    reads=[], writes=[("ring", slot)])
            slots.append(slot)
        gstate["loaded"][gi] = slots

    def use_group():
        gi = gstate["next_use"]
        gstate["next_use"] += 1
        while gstate["next_load"] <= gi + 1 and gstate["next_load"] < len(allgroups):
            issue_group_load()
        return gstate["loaded"][gi]

    for t in range(ntiles):
        dma("sp", "io", X[:, 0:nch, :], xp[t * nch * 128:(t + 1) * nch * 128, :].rearrange("(c p) d -> p c d", p=128),
            [], [("x", c) for c in range(nch)])
        if t == 0:
            dma("sp", "io", X[0:ns, nch, :], xs, [], [("x", nch)])
        for l in range(depth):
            phase_norm(t, l, 0)
            su = use_group()
            load_layer_params(t, l)
            phase_G(t, l, su, use_group, after_u=lambda: layer_params_compute(t, l))

            def pa_group():
                s = use_group()
                return [s[0]], s[1]
            phase_P(t, l, AT, lambda k, c: ("aT", k, c), 8, 0, BIG, lambda k, c: ("big", k, c),
                    [pa_group, pa_group, use_group])
            for h in range(H):
                phase_R(t, l, h, use_group())

            def pb_group():
                s = use_group()
                return [s[0], s[1]], s[2]
            phase_P(t, l, BIG, lambda k, c: ("big", k, c), 16, 8, AT, lambda k, c: ("aT", k, c),
                    [pb_group, pb_group, use_group])
            phase_norm(t, l, 1)
            phase_F(t, l, [use_group] * 8)
        cl = chunk_list(t)
        for c in cl:
            n = ntok(c)
            jb = rot("wb")
            act(WB[0:n, jb, :], X[0:n, c, :], AF.Square, [("x", c), ("ssq",)], [("wb", jb), ("ssq", c)],
                accum_out=SSQ[0:n, c:c + 1])
        ncl = len(cl)
        dve("tensor_scalar", [("ssq", c) for c in cl] + [("ssq",)], [("rs",)], out=RS[:, 0:ncl], in0=SSQ[:, 0:ncl],
            scalar1=1.0 / D, scalar2=EPS, op0=ALU.mult, op1=ALU.add)
        pool_pow(RS[:, 0:ncl], [("rs",)])
        gf = rot("wf")
        dma("sp", "par", WF[:, gf, :], gfin_d.partition_broadcast(128), [], [("wf", gf, 0), ("wf", gf, 1)])
        for c in cl:
            n = ntok(c)
            dve("scalar_tensor_tensor", [("x", c), ("rs",), ("wf", gf, 0), ("wf", gf, 1)], [("x", c)], out=X[0:n, c, :],
                in0=X[0:n, c, :], scalar=RS[0:n, c:c + 1], in1=WF[0:n, gf, :], op0=ALU.mult, op1=ALU.mult)
        dma("sp", "io", yp[t * nch * 128:(t + 1) * nch * 128, :].rearrange("(c p) d -> p c d", p=128), X[:, 0:nch, :],
            [("x", c) for c in range(nch)], [("yp", t)])
        if t == 0:
            dma("sp", "io", ys, X[0:ns, nch, :], [("x", nch)], [("ys",)])

    outs = [("yp", t) for t in range(ntiles)] + [("ys",)] + [("gmv", l) for l in range(depth)]
    outs += [("nrp", l, h) for l in range(depth) for h in range(H)]
    outs += [("nrs", l, b, h, hf) for l in range(depth) for b in range(ns) for h in range(H) for hf in range(2)]
    P.op("sp", lambda e: None, reads=outs, writes=[])

    block = es.enter_context(nc.Block())
    P.emit(nc, {"sp": block.sync, "act": block.scalar, "dve": block.vector, "pool": block.gpsimd, "pe": block.tensor},
           sem_alloc, dma_classes)
    es.close()
    P.sbuf_free = SBUF_FREE
    return nc, P


def _prep_common(cfg, inputs):
    depth = cfg.depth
    tabs, _, _ = _host_tables(cfg)
    f = lambda a: np.ascontiguousarray(np.asarray(a, dtype=np.float32))
    g3 = np.stack([f(inputs["norm_mix_g"])[:depth], f(inputs["norm_ffn_g"])[:depth], f(inputs["gm_ln_g"])[:depth]], axis=1)
    gcols = np.ascontiguousarray(g3.reshape(depth, 3, 8, 128).transpose(3, 0, 1, 2))
    bgc = np.ascontiguousarray(f(inputs["b_gate"])[:depth].reshape(depth, 16, 128).transpose(2, 0, 1))
    wt = np.ascontiguousarray(f(inputs["gm_w_s"])[:depth].transpose(0, 3, 1, 2))
    w00 = np.ascontiguousarray(f(inputs["gm_w_s"])[:depth, :, 0, 0])
    common = dict(
        w_in=f(inputs["w_in"])[:depth], w_pgm=f(inputs["w_proj_gm"])[:depth], w_pret=f(inputs["w_proj_ret"])[:depth],
        w_out=f(inputs["w_out"])[:depth], w_up=f(inputs["w_up"])[:depth], w_down=f(inputs["w_down"])[:depth],
        gcols=gcols, gfin=f(inputs["norm_final_g"]), bgcols=bgc, lng=f(inputs["gm_ln_g"])[:depth],
        lnb=f(inputs["gm_ln_b"])[:depth], wt=wt, w00=w00, bs=f(inputs["gm_b_s"])[:depth].reshape(depth, 512),
        cs=tabs["cs"], cs_s=tabs["cs_s"], dqk=tabs["dqk"], mask=tabs["mask"], ident=tabs["ident"],
    )
    return common


_CACHE = {}


def run(cfg, inputs, ncores, trace=False):
    key = (cfg.depth, cfg.nch, cfg.ntiles, cfg.ns)
    if key not in _CACHE:
        _CACHE[key] = build_nc(cfg)
    nc, _ = _CACHE[key]
    common = _prep_common(cfg, inputs)
    xp = np.asarray(inputs["x_prompt"], dtype=np.float32)
    xs = np.asarray(inputs["x_sample"], dtype=np.float32)
    st = np.asarray(inputs["state_ret"], dtype=np.float32)
    ns = cfg.ns
    in_maps = []
    for b in range(ncores):
        m = dict(common)
        m["xp"] = np.ascontiguousarray(xp[b])
        m["xs"] = np.ascontiguousarray(xs[b * ns:(b + 1) * ns, 0, :])
        m["st"] = np.ascontiguousarray(st[:cfg.depth, b * ns:(b + 1) * ns])
        in_maps.append(m)
    res = run_bass_kernel_spmd(nc, in_maps, core_ids=list(range(ncores)), trace=trace)
    rs = res.results
    y_prompt = np.stack([rs[b]["yp"] for b in range(ncores)], axis=0)
    y_sample = np.concatenate([rs[b]["ys"] for b in range(ncores)], axis=0)[:, None, :]
    nrp = np.stack([rs[b]["nrp"] for b in range(ncores)], axis=1)
    nrs = np.concatenate([rs[b]["nrs"] for b in range(ncores)], axis=1)
    gmv = np.concatenate([rs[b]["gmv"] for b in range(ncores)], axis=1)[:, :, None, :]
    out = (y_prompt.astype(np.float32), y_sample.astype(np.float32), nrp.astype(np.float32), nrs.astype(np.float32),
           gmv.astype(np.float32))
    return out, res


def kernel(**inputs):
    cfg = Cfg(depth=4, nch=8, ntiles=2, ns=16)
    out, _ = run(cfg, inputs, 8)
    return out
```
